# Optimizing a Trainium2 kernel written in Bass

```python
import jax, jax.numpy as jnp
from jax import lax
import numpy as np

D_MODEL = 1024
BATCH = 16
SEQ = 4096
DEPTH = 2

CHUNK = 64
MEM_LEN = 256
RET_HEADS = 8
RET_DK = 64
RET_DV = 128
RET_QK = RET_HEADS * RET_DK
RET_V = RET_HEADS * RET_DV
ROPE_BASE = 10000.0
ATT_HEADS = 8
ATT_DH = 64
ATT_W = ATT_HEADS * ATT_DH
LEFT_CHUNKS = 8
MAX_REL = 256
MEM_HEADS = 4
MEM_DH = D_MODEL // MEM_HEADS
N_GROUPS = 4
EXPERTS_PER_GROUP = 8
N_EXPERTS = N_GROUPS * EXPERTS_PER_GROUP
TOP_K = 2
D_EXPERT = 512
ROUTE_BLOCK = 256
LN_EPS = 1e-5
DEEPNORM_ALPHA = (2.0 * DEPTH) ** 0.25
DEEPNORM_BETA = (8.0 * DEPTH) ** -0.25
IN_WIDTHS = (RET_QK, RET_QK, RET_V, RET_V, ATT_W, ATT_W, ATT_W, D_MODEL, D_MODEL)
IN_TOTAL = sum(IN_WIDTHS)

kernel_name = "hybrid_retention_chunkattn_hmoe_deepnorm"


def layer_norm(x, g, b):
    xf = x.astype(jnp.float32)
    mu = jnp.mean(xf, axis=-1, keepdims=True)
    var = jnp.mean(jnp.square(xf - mu), axis=-1, keepdims=True)
    return ((xf - mu) * lax.rsqrt(var + LN_EPS) * g.astype(jnp.float32) + b.astype(jnp.float32)).astype(x.dtype)


def head_group_norm(y):
    yf = y.astype(jnp.float32)
    mu = jnp.mean(yf, axis=-1, keepdims=True)
    var = jnp.mean(jnp.square(yf - mu), axis=-1, keepdims=True)
    return ((yf - mu) * lax.rsqrt(var + LN_EPS)).astype(y.dtype)


def rotary(t, pos):
    half = t.shape[-1] // 2
    inv_freq = 1.0 / (ROPE_BASE ** jnp.linspace(0.0, 1.0, half, dtype=jnp.float32))
    ang = pos.astype(jnp.float32)[:, :, None, None] * inv_freq
    cos, sin = jnp.cos(ang), jnp.sin(ang)
    t1 = t[..., :half].astype(jnp.float32)
    t2 = t[..., half:].astype(jnp.float32)
    return jnp.concatenate([t1 * cos - t2 * sin, t1 * sin + t2 * cos], axis=-1).astype(t.dtype)


def retention(q, k, v, pos):
    B, S = q.shape[:2]
    nc = S // CHUNK
    dt = q.dtype
    q = rotary(q, pos)
    k = rotary(k, pos) * (RET_DK ** -0.5)
    log_g = jnp.log1p(-jnp.exp2(-5.0 - jnp.arange(RET_HEADS, dtype=jnp.float32)))
    i = jnp.arange(CHUNK, dtype=jnp.float32)
    intra_decay = jnp.exp(log_g[:, None, None] * jnp.abs(i[:, None] - i[None, :])).astype(dt)
    q_decay = jnp.exp(log_g[:, None] * (i + 1.0)).astype(dt)
    k_decay = jnp.exp(log_g[:, None] * (CHUNK - 1.0 - i)).astype(dt)
    chunk_decay = jnp.exp(log_g * CHUNK).astype(dt)
    qc = q.reshape(B, nc, CHUNK, RET_HEADS, RET_DK)
    kc = k.reshape(B, nc, CHUNK, RET_HEADS, RET_DK)
    vc = v.reshape(B, nc, CHUNK, RET_HEADS, RET_DV)
    scores = jnp.einsum('bnihd,bnjhd->bnhij', qc, kc) * intra_decay
    intra = jnp.einsum('bnhij,bnjhe->bnihe', scores, vc)

    def step(state, inp):
        q_n, k_n, v_n = inp
        cross = jnp.einsum('bihd,hi,bhde->bihe', q_n, q_decay, state)
        state = state * chunk_decay[None, :, None, None] + jnp.einsum('bjhd,hj,bjhe->bhde', k_n, k_decay, v_n)
        return state, cross

    state0 = jnp.zeros((B, RET_HEADS, RET_DK, RET_DV), dt)
    _, cross = lax.scan(step, state0, (qc.swapaxes(0, 1), kc.swapaxes(0, 1), vc.swapaxes(0, 1)))
    out = intra + cross.swapaxes(0, 1)
    return out.reshape(B, S, RET_HEADS, RET_DV)


def chunked_attention(q, k, v, rel_bias):
    B, S, H, dh = q.shape
    nc = S // CHUNK
    pad = LEFT_CHUNKS * CHUNK
    band = pad + CHUNK
    kp = jnp.pad(k, ((0, 0), (pad, 0), (0, 0), (0, 0)))
    vp = jnp.pad(v, ((0, 0), (pad, 0), (0, 0), (0, 0)))
    qi = jnp.arange(CHUNK)
    kj = jnp.arange(band)
    dist = (pad + qi)[:, None] - kj[None, :]
    bias = rel_bias[:, jnp.clip(dist, -MAX_REL, MAX_REL) + MAX_REL].astype(jnp.float32)
    scale = dh ** -0.5

    def one_chunk(c):
        start = c * CHUNK
        q_c = lax.dynamic_slice_in_dim(q, start, CHUNK, axis=1)
        k_b = lax.dynamic_slice_in_dim(kp, start, band, axis=1)
        v_b = lax.dynamic_slice_in_dim(vp, start, band, axis=1)
        s = jnp.einsum('bihd,bjhd->bhij', q_c, k_b).astype(jnp.float32) * scale + bias
        valid = (start - pad + kj) >= 0
        s = jnp.where(valid[None, None, None, :], s, -1e30)
        p = jax.nn.softmax(s, axis=-1).astype(v.dtype)
        return jnp.einsum('bhij,bjhd->bihd', p, v_b)

    out = lax.map(one_chunk, jnp.arange(nc))
    return out.swapaxes(0, 1).reshape(B, S, H * dh)


def hybrid_mixer(x, pos, w_in, rel_bias, w_proj_ret, w_proj_att, w_out):
    B, S, _ = x.shape
    z = x @ w_in
    splits = np.cumsum(IN_WIDTHS)[:-1].tolist()
    q_r, k_r, v_r, g_r, q_a, k_a, v_a, gate_r, gate_a = jnp.split(z, splits, axis=-1)
    ret = retention(q_r.reshape(B, S, RET_HEADS, RET_DK), k_r.reshape(B, S, RET_HEADS, RET_DK),
                    v_r.reshape(B, S, RET_HEADS, RET_DV), pos)
    y_r = jax.nn.silu(g_r) * head_group_norm(ret).reshape(B, S, RET_V)
    y_a = chunked_attention(q_a.reshape(B, S, ATT_HEADS, ATT_DH), k_a.reshape(B, S, ATT_HEADS, ATT_DH),
                            v_a.reshape(B, S, ATT_HEADS, ATT_DH), rel_bias)
    merged = jax.nn.sigmoid(gate_r) * (y_r @ w_proj_ret) + jax.nn.sigmoid(gate_a) * (y_a @ w_proj_att)
    return merged @ w_out


def memory_attention(x, mem, w_q, w_kv, w_o):
    B, S, _ = x.shape
    q = (x @ w_q).reshape(B, S, MEM_HEADS, MEM_DH)
    k, v = jnp.split(mem @ w_kv, 2, axis=-1)
    k = k.reshape(B, -1, MEM_HEADS, MEM_DH)
    v = v.reshape(B, -1, MEM_HEADS, MEM_DH)
    s = jnp.einsum('bshd,bmhd->bhsm', q, k).astype(jnp.float32) * (MEM_DH ** -0.5)
    p = jax.nn.softmax(s, axis=-1).astype(v.dtype)
    o = jnp.einsum('bhsm,bmhd->bshd', p, v).reshape(B, S, D_MODEL)
    return o @ w_o


def hierarchical_moe(x, w_group, b_group, w_route, b_route, w_gate, w_up, w_down):
    B, S, D = x.shape
    T = B * S
    xt = x.reshape(T, D)
    g_prob = jax.nn.softmax((xt @ w_group).astype(jnp.float32) + b_group.astype(jnp.float32), axis=-1)
    g_w, g_idx = lax.top_k(g_prob, 1)
    g_w, g_idx = g_w[:, 0], g_idx[:, 0]
    e_all = (xt @ w_route).astype(jnp.float32).reshape(T, N_GROUPS, EXPERTS_PER_GROUP) + b_route.astype(jnp.float32)
    e_logits = jnp.take_along_axis(e_all, g_idx[:, None, None], axis=1)[:, 0]
    e_w, e_idx = lax.top_k(jax.nn.softmax(e_logits, axis=-1), TOP_K)
    e_w = e_w / jnp.sum(e_w, axis=-1, keepdims=True)
    weights = g_w[:, None] * e_w
    expert = g_idx[:, None] * EXPERTS_PER_GROUP + e_idx

    A = T * TOP_K
    flat_e = expert.reshape(A).astype(jnp.int32)
    flat_tok = jnp.repeat(jnp.arange(T, dtype=jnp.int32), TOP_K)
    flat_w = weights.reshape(A)
    order = jnp.argsort(flat_e)
    sorted_e = flat_e[order]
    counts = jnp.zeros((N_EXPERTS,), jnp.int32).at[flat_e].add(1)
    padded = ((counts + ROUTE_BLOCK - 1) // ROUTE_BLOCK) * ROUTE_BLOCK
    start = jnp.cumsum(counts) - counts
    pend = jnp.cumsum(padded)
    pstart = pend - padded
    dest = pstart[sorted_e] + (jnp.arange(A, dtype=jnp.int32) - start[sorted_e])
    n_blocks = -(-(A + N_EXPERTS * (ROUTE_BLOCK - 1)) // ROUTE_BLOCK)
    P = n_blocks * ROUTE_BLOCK
    buf_tok = jnp.full((P,), T, jnp.int32).at[dest].set(flat_tok[order])
    buf_w = jnp.zeros((P,), x.dtype).at[dest].set(flat_w[order].astype(x.dtype))
    block_expert = jnp.clip(jnp.searchsorted(pend, jnp.arange(n_blocks, dtype=jnp.int32) * ROUTE_BLOCK, side='right'),
                            0, N_EXPERTS - 1)
    xpad = jnp.concatenate([xt, jnp.zeros((1, D), x.dtype)], axis=0)
    xb = xpad[buf_tok].reshape(n_blocks, ROUTE_BLOCK, D)

    def run_block(args):
        xblk, e = args
        h = jax.nn.silu(xblk @ w_gate[e]) * (xblk @ w_up[e])
        return h @ w_down[e]

    yb = lax.map(run_block, (xb, block_expert)).reshape(P, D)
    y = jax.ops.segment_sum(yb * buf_w[:, None], buf_tok, num_segments=T + 1)[:T]
    return y.reshape(B, S, D)


def setup_inputs(seed: int = 0) -> dict:
    key = jax.random.key(seed)
    ks = jax.random.split(key, 26)
    f32 = jnp.float32
    L, D = DEPTH, D_MODEL

    def nrm(k, shape, scale):
        return jax.random.normal(k, shape, f32) * scale

    x = jax.random.normal(ks[0], (BATCH, SEQ, D), f32)
    mem = jax.random.normal(ks[1], (BATCH, MEM_LEN, D), f32)
    offsets = jax.random.randint(ks[2], (BATCH, 1), 0, 64, dtype=jnp.int32) * CHUNK
    positions = (offsets + jnp.arange(SEQ, dtype=jnp.int32)[None, :]).astype(jnp.int32)
    return {
        "x": x,
        "mem": mem,
        "positions": positions,
        "ln_in_g": 1.0 + nrm(ks[3], (D,), 0.01),
        "ln_in_b": nrm(ks[4], (D,), 0.01),
        "w_in": nrm(ks[5], (L, D, IN_TOTAL), D ** -0.5),
        "rel_bias": nrm(ks[6], (L, ATT_HEADS, 2 * MAX_REL + 1), 0.1),
        "w_proj_ret": nrm(ks[7], (L, RET_V, D), RET_V ** -0.5),
        "w_proj_att": nrm(ks[8], (L, ATT_W, D), ATT_W ** -0.5),
        "w_out": nrm(ks[9], (L, D, D), DEEPNORM_BETA * D ** -0.5),
        "ln1_g": 1.0 + nrm(ks[10], (L, D), 0.01),
        "ln1_b": nrm(ks[11], (L, D), 0.01),
        "w_q_mem": nrm(ks[12], (L, D, D), D ** -0.5),
        "w_kv_mem": nrm(ks[13], (L, D, 2 * D), D ** -0.5),
        "w_o_mem": nrm(ks[14], (L, D, D), DEEPNORM_BETA * D ** -0.5),
        "ln2_g": 1.0 + nrm(ks[15], (L, D), 0.01),
        "ln2_b": nrm(ks[16], (L, D), 0.01),
        "w_group": nrm(ks[17], (L, D, N_GROUPS), D ** -0.5),
        "b_group": nrm(ks[18], (L, N_GROUPS), 0.01),
        "w_route": nrm(ks[19], (L, D, N_GROUPS * EXPERTS_PER_GROUP), D ** -0.5),
        "b_route": nrm(ks[20], (L, N_GROUPS, EXPERTS_PER_GROUP), 0.01),
        "w_gate": nrm(ks[21], (L, N_EXPERTS, D, D_EXPERT), D ** -0.5),
        "w_up": nrm(ks[22], (L, N_EXPERTS, D, D_EXPERT), D ** -0.5),
        "w_down": nrm(ks[23], (L, N_EXPERTS, D_EXPERT, D), DEEPNORM_BETA * D_EXPERT ** -0.5),
        "ln3_g": 1.0 + nrm(ks[24], (L, D), 0.01),
        "ln3_b": nrm(ks[25], (L, D), 0.01),
    }


def reference(x, mem, positions, ln_in_g, ln_in_b, w_in, rel_bias, w_proj_ret, w_proj_att, w_out,
              ln1_g, ln1_b, w_q_mem, w_kv_mem, w_o_mem, ln2_g, ln2_b,
              w_group, b_group, w_route, b_route, w_gate, w_up, w_down, ln3_g, ln3_b):
    h = layer_norm(x, ln_in_g, ln_in_b)
    for l in range(DEPTH):
        mix = hybrid_mixer(h, positions, w_in[l], rel_bias[l], w_proj_ret[l], w_proj_att[l], w_out[l])
        h = layer_norm(DEEPNORM_ALPHA * h + mix, ln1_g[l], ln1_b[l])
        cross = memory_attention(h, mem, w_q_mem[l], w_kv_mem[l], w_o_mem[l])
        h = layer_norm(DEEPNORM_ALPHA * h + cross, ln2_g[l], ln2_b[l])
        ffn = hierarchical_moe(h, w_group[l], b_group[l], w_route[l], b_route[l], w_gate[l], w_up[l], w_down[l])
        h = layer_norm(DEEPNORM_ALPHA * h + ffn, ln3_g[l], ln3_b[l])
    return h
```

```python
import numpy as np
import ml_dtypes
import concourse.bass as bass
import concourse.mybir as mybir
from concourse.bass_utils import run_bass_kernel_spmd
from contextlib import ExitStack

F32 = mybir.dt.float32; BF16 = mybir.dt.bfloat16; I32 = mybir.dt.int32; U32 = mybir.dt.uint32
AF = mybir.ActivationFunctionType; ALU = mybir.AluOpType; AX = mybir.AxisListType

NDS = 16
SUBSTOP = 99
SAME_ENG_SYNC = True


class Buf:
    __slots__ = ("name", "w", "r")

    def __init__(self, name):
        self.name = name; self.w = None; self.r = {}


class Sched:
    def __init__(self, nc, es):
        self.nc = nc
        self.e = dict(pe=nc.tensor, act=nc.scalar, dve=nc.vector, pool=nc.gpsimd, sp=nc.sync)
        self.sem = {k: es.enter_context(nc.semaphore("s_" + k)) for k in self.e}
        self.cnt = {k: 0 for k in self.e}
        self.known = {k: {} for k in self.e}
        self.dq = {}
        for q in ("sp", "pool", "act"):
            self.dq[q] = dict(sems=[es.enter_context(nc.semaphore("d_%s%d" % (q, i))) for i in range(NDS)], n=0)
        self.nwait = 0; self.ninst = 0

    def _sem(self, key):
        if isinstance(key, tuple):
            return self.dq[key[1]]["sems"][key[2]]
        return self.sem[key]

    def _wait(self, e, key, val):
        if self.known[e].get(key, 0) >= val:
            return
        self.e[e].wait_ge(self._sem(key), val)
        self.known[e][key] = val
        self.nwait += 1

    def _deps(self, e, reads, writes):
        need = {}
        for b in reads:
            if b.w is not None and need.get(b.w[0], 0) < b.w[1]:
                need[b.w[0]] = b.w[1]
        for b in writes:
            if b.w is not None and need.get(b.w[0], 0) < b.w[1]:
                need[b.w[0]] = b.w[1]
            for k, v in b.r.items():
                if need.get(k, 0) < v:
                    need[k] = v
        for k, v in need.items():
            if k == e and (e == "pe" or not SAME_ENG_SYNC):
                continue
            self._wait(e, k, v)

    def op(self, e, fn, reads=(), writes=(), sig=True):
        self._deps(e, reads, writes)
        inst = fn(self.e[e])
        seq = self.cnt[e] + 1
        if sig:
            inst.then_inc(self.sem[e], 1)
            self.cnt[e] = seq
        self.ninst += 1
        for b in reads:
            b.r[e] = seq
        for b in writes:
            b.w = (e, seq); b.r = {}
        return inst

    def dma(self, q, out_ap, in_ap, reads=(), writes=(), fn=None):
        d = self.dq[q]; i = d["n"] % NDS; rnd = d["n"] // NDS; d["n"] += 1
        key = ("d", q, i)
        if rnd > 0:
            self._wait(q, key, 16 * rnd)
        self._deps(q, reads, writes)
        if fn is None:
            inst = self.e[q].dma_start(out=out_ap, in_=in_ap)
        else:
            inst = fn(self.e[q])
        inst.then_inc(d["sems"][i], 16)
        val = 16 * (rnd + 1)
        self.ninst += 1
        for b in reads:
            b.r[key] = val
        for b in writes:
            b.w = (key, val); b.r = {}
        return inst

    def barrier(self):
        for q, d in self.dq.items():
            for i in range(NDS):
                uses = (d["n"] - i + NDS - 1) // NDS
                if uses > 0:
                    self._wait("sp", ("d", q, i), 16 * uses)
        for k in ("pe", "act", "dve", "pool"):
            if self.cnt[k] > 0:
                self._wait("sp", k, self.cnt[k])
        inst = self.e["sp"].nop()
        self.cnt["sp"] += 1
        inst.then_inc(self.sem["sp"], 1)
        for k in ("pe", "act", "dve", "pool"):
            self._wait(k, "sp", self.cnt["sp"])
            for k2 in ("pe", "act", "dve", "pool"):
                self.known[k][k2] = max(self.known[k].get(k2, 0), self.cnt[k2])
            for q, d in self.dq.items():
                for i in range(NDS):
                    uses = (d["n"] - i + NDS - 1) // NDS
                    if uses > 0:
                        self.known[k][("d", q, i)] = 16 * uses

    def finish(self):
        for q, d in self.dq.items():
            for i in range(min(NDS, d["n"])):
                last = (d["n"] - 1 - i) // NDS + 1 if d["n"] - 1 - i >= 0 else 0
                uses = (d["n"] - i + NDS - 1) // NDS
                if uses > 0:
                    self._wait("sp", ("d", q, i), 16 * uses)
        for k in ("pe", "act", "dve", "pool"):
            if self.cnt[k] > 0:
                self._wait("sp", k, self.cnt[k])


D = 1024; NE = 32; DE = 512; LN_EPS = 1e-5
ALPHA = float((2.0 * 2) ** 0.25)
NMEM = 256
RB_EXT = 1024


def host_consts(nblk):
    tabs = {}
    p = np.arange(128)
    g = 1.0 - np.exp2(-5.0 - np.arange(8, dtype=np.float64))
    lg = np.log(g)
    i = np.arange(128)[None, :]; j = np.arange(128)[:, None]
    dm = np.zeros((128, 8, 128))
    for h in range(8):
        m = np.exp(lg[h] * np.abs(i - j)) * ((j // 64) <= (i // 64))
        dm[:, h, :] = 0.125 * m
    tabs["DM"] = dm.reshape(128, 1024)
    qd = np.zeros((128, 4, 128))
    cd = np.zeros((128, 4))
    for c in range(4):
        for half in range(2):
            h = 2 * c + half
            qd[half * 64:(half + 1) * 64, c, :] = np.exp(lg[h] * (np.arange(128) + 1.0))[None, :]
            cd[half * 64:(half + 1) * 64, c] = np.exp(lg[h] * 128.0)
    tabs["QD"] = qd.reshape(128, 512)
    tabs["CD"] = cd
    kd = np.zeros((128, 8))
    for h in range(8):
        kd[:, h] = 0.125 * np.exp(lg[h] * (127.0 - np.arange(128)))
    tabs["KD"] = kd
    inv_freq = 1.0 / (10000.0 ** np.linspace(0.0, 1.0, 32, dtype=np.float32)).astype(np.float32)
    fq = (inv_freq.astype(np.float64) / (2 * np.pi))
    tabs["FQ"] = np.tile(np.concatenate([fq, fq])[None, :], (128, 1))
    tabs["IDENT"] = np.eye(128)
    tabs["ANTI"] = np.eye(128)[::-1].copy()
    tabs["USTRICT"] = (p[:, None] < p[None, :]).astype(np.float64)
    tabs["ONES"] = np.ones((128, 128))
    u32 = np.zeros((128, 32)); u32[:32, :] = (np.arange(32)[:, None] < np.arange(32)[None, :])
    tabs["U32"] = u32
    tabs["PIDX"] = p[:, None].astype(np.float64)
    ui32 = np.zeros((128, 32)); ui32[:32, :] = (np.arange(32)[:, None] <= np.arange(32)[None, :])
    tabs["UI32"] = ui32
    tabs["BLK"] = np.tile((256.0 * np.arange(nblk))[None, :], (128, 1))
    off = {}; cols = []; o = 0
    for k, v in tabs.items():
        v = np.asarray(v, dtype=np.float32)
        off[k] = (o, v.shape[1]); cols.append(v); o += v.shape[1]
    return np.ascontiguousarray(np.concatenate(cols, axis=1)), off


class Ctx:
    pass


def build(NSEQ, S, L=2, debug=None, stop=None):
    NTOK = NSEQ * S; NT = NTOK // 128; TS = S // 128
    NBLK = -(-(NTOK * 2 + NE * 255) // 256)
    PROWS = NBLK * 256
    consts_np, coff = host_consts(NBLK)
    NCONST = consts_np.shape[1]

    nc = bass.Bass("TRN2", target_bir_lowering=False)
    dbg = debug or ()

    def dram(name, shape, dt, kind="Internal"):
        if kind == "Internal" and name in dbg:
            kind = "ExternalOutput"
        return nc.dram_tensor(name, shape, dt, kind=kind).ap()

    I = {}
    I["x"] = dram("x", [NTOK, D], F32, "ExternalInput")
    I["mem"] = dram("mem", [NSEQ * NMEM, D], F32, "ExternalInput")
    I["pos"] = dram("pos", [128, NT], I32, "ExternalInput")
    I["consts"] = dram("consts", [128, NCONST], F32, "ExternalInput")
    I["lnall"] = dram("lnall", [2 + 6 * L, D], F32, "ExternalInput")
    I["w_in"] = dram("w_in", [L, D, 6656], F32, "ExternalInput")
    I["rb_ext"] = dram("rb_ext", [L, 8, RB_EXT], F32, "ExternalInput")
    I["w_proj_ret"] = dram("w_proj_ret", [L, D, D], F32, "ExternalInput")
    I["w_proj_att"] = dram("w_proj_att", [L, 512, D], F32, "ExternalInput")
    I["w_out"] = dram("w_out", [L, D, D], F32, "ExternalInput")
    I["w_q_mem"] = dram("w_q_mem", [L, D, D], F32, "ExternalInput")
    I["w_kv_mem"] = dram("w_kv_mem", [L, D, 2 * D], F32, "ExternalInput")
    I["w_o_mem"] = dram("w_o_mem", [L, D, D], F32, "ExternalInput")
    I["wr"] = dram("wr", [L, D, 36], F32, "ExternalInput")
    I["br"] = dram("br", [L, 36], F32, "ExternalInput")
    I["w_gate"] = dram("w_gate", [L * NE * 256, 2048], F32, "ExternalInput")
    I["w_up"] = dram("w_up", [L * NE * 256, 2048], F32, "ExternalInput")
    I["w_down"] = dram("w_down", [L * NE * 256, 2048], F32, "ExternalInput")
    OUT = dram("out", [NTOK, D], F32, "ExternalOutput")
    H = dram("H", [NTOK, D], F32); M1 = dram("M1", [NTOK, D], BF16)
    H1 = dram("H1", [NTOK, D], F32); H2 = dram("H2", [NTOK, D], F32)
    XS = dram("XS", [PROWS, D], BF16); YB = dram("YB", [PROWS, D], F32)
    B_H = [Buf("H%d" % t) for t in range(NT)]; B_M1 = [Buf("M1%d" % t) for t in range(NT)]
    B_H1 = [Buf("H1%d" % t) for t in range(NT)]; B_H2 = [Buf("H2%d" % t) for t in range(NT)]
    B_XS = Buf("XS"); B_YB = Buf("YB")
    DBGI = dram("DBGI", [128, 2 * NT + 2 * NBLK], I32) if "DBGI" in dbg else None
    DBGF = dram("DBGF", [128, 2 * NT + 64], F32) if "DBGI" in dbg else None

    with ExitStack() as es:
        S_ = Sched(nc, es)
        ctr = [0]

        def sbt(es_, shape, dt, name=None):
            ctr[0] += 1
            nm = "%s_%d" % (name or "t", ctr[0])
            return es_.enter_context(nc.sbuf_tensor(nm, shape, dt)), Buf(nm)

        def pst(es_, shape, dt, name=None):
            ctr[0] += 1
            nm = "%s_%d" % (name or "p", ctr[0])
            return es_.enter_context(nc.psum_tensor(nm, shape, dt)), Buf(nm)

        op = S_.op; dma = S_.dma

        cst, B_cst = sbt(es, [128, NCONST], F32, "cst")
        dma("sp", cst[:], I["consts"], writes=[B_cst])

        def ctab(name):
            o, w = coff[name]
            return cst[:, o:o + w]
        idb, B_idb = sbt(es, [128, 128], BF16, "idb")
        op("dve", lambda e: e.tensor_copy(idb[:], ctab("IDENT")), reads=[B_cst], writes=[B_idb])
        mhalf, B_mhalf = sbt(es, [128, 8], F32, "mhalf")
        op("pool", lambda e: e.memset(mhalf[:], -0.5), writes=[B_mhalf])
        posf, B_posf = sbt(es, [128, NT], F32, "posf")
        with ExitStack() as es0:
            posi, B_posi = sbt(es0, [128, NT], I32, "posi")
            dma("sp", posi[:], I["pos"], writes=[B_posi])
            op("dve", lambda e: e.tensor_copy(posf[:], posi[:]), reads=[B_posi], writes=[B_posf])
            S_.barrier()
        PT = [pst(es, [128, 8, 128], BF16, "PT") for _ in range(2)]
        PF = [pst(es, [128, 1024], F32, "PF") for _ in range(3)]
        rr = {"pt": 0, "pf": 0}

        def next_pt():
            rr["pt"] += 1
            return PT[rr["pt"] % 2]

        def next_pf():
            rr["pf"] += 1
            return PF[rr["pf"] % 3]

        MK1, B_MK1 = sbt(es, [128, NT, 32], BF16, "MK1")
        MK2, B_MK2 = sbt(es, [128, NT, 32], BF16, "MK2")
        W1, B_W1 = sbt(es, [128, NT], F32, "W1")
        W2, B_W2 = sbt(es, [128, NT], F32, "W2")
        DEST1, B_DEST1 = sbt(es, [128, NT], I32, "DEST1")
        DEST2, B_DEST2 = sbt(es, [128, NT], I32, "DEST2")
        WIDX, B_WIDX = sbt(es, [128, NBLK, 2], I32, "WIDX")
        REG_PROWS = nc.gpsimd.to_reg(PROWS - 1)
        REG_WROWS = nc.gpsimd.to_reg(L * NE * 256 - 1)
        onesb, B_onesb = sbt(es, [128, 128], BF16, "onesb")
        op("pool", lambda e: e.memset(onesb[:], 1.0), writes=[B_onesb])
        def load_w(es_, src_rows, K, ncols, name):
            w, B = sbt(es_, [128, K, ncols], BF16, name)
            for k in range(K):
                for c0 in range(0, ncols, 2048):
                    c1 = min(ncols, c0 + 2048)
                    dma("pool", w[:, k, c0:c1], src_rows[k * 128:(k + 1) * 128, c0:c1], writes=[B])
            return w, B

        def load_bcast(es_, row_ap, n, name):
            t, B = sbt(es_, [128, n], F32, name)
            dma("sp", t[:], row_ap.partition_broadcast(128), writes=[B])
            return t, B

        def transposes(src, B_src, n, dstT, B_dst, eng="dve", idt=None, B_id=None):
            pt, B_pt = next_pt()
            for k in range(n):
                op("pe", lambda e: e.transpose(pt[:, k, :], src[:, k * 128:(k + 1) * 128], idb[:]),
                   reads=[B_src, B_idb], writes=[B_pt], sig=(k == n - 1))
            if eng == "dve":
                op("dve", lambda e: e.tensor_copy(dstT[:, 0:n, :], pt[:, 0:n, :]), reads=[B_pt], writes=[B_dst])
            elif eng == "act":
                op("dve", lambda e: e.tensor_copy(dstT[:, 0:n, :], pt[:, 0:n, :]), reads=[B_pt], writes=[B_dst])
            return pt, B_pt

        def mm_tok(ps, B_ps, xT, B_xT, K, w, B_w, c0, ncols):
            ng = (ncols + 511) // 512
            for g_ in range(ng):
                a = g_ * 512; b = min(ncols, a + 512)
                for k in range(K):
                    op("pe", lambda e: e.matmul(ps[:, a:b], xT[:, k, :], w[:, k, c0 + a:c0 + b],
                                                start=(k == 0), stop=(k == K - 1)),
                       reads=[B_xT, B_w], writes=[B_ps], sig=(k == K - 1 and g_ == ng - 1))

        class LNState:
            pass

        def make_ln(es_):
            st = LNState()
            st.st, st.B_st = sbt(es_, [128, 2, 6], F32, "lnst")
            st.mv, st.B_mv = sbt(es_, [128, 2], F32, "lnmv")
            st.rs, st.B_rs = sbt(es_, [128, 1], F32, "lnrs")
            st.nm, st.B_nm = sbt(es_, [128, 1], F32, "lnnm")
            st.tmp, st.B_tmp = sbt(es_, [128, D], F32, "lntmp")
            return st

        def layernorm(st, src, B_src, dst, B_dst, g, B_g, b, B_b):
            for c in range(2):
                op("dve", lambda e: e.bn_stats(st.st[:, c, :], src[:, c * 512:(c + 1) * 512]), reads=[B_src], writes=[st.B_st])
            op("dve", lambda e: e.bn_aggr(st.mv[:], st.st[:].rearrange("p a b -> p (a b)")), reads=[st.B_st], writes=[st.B_mv])
            op("dve", lambda e: e.tensor_scalar(st.rs[:], st.mv[:, 1:2], LN_EPS, None, ALU.add), reads=[st.B_mv], writes=[st.B_rs])
            op("pool", lambda e: e.tensor_tensor(st.rs[:], st.rs[:], mhalf[:, 0:1], ALU.pow), reads=[st.B_rs, B_mhalf], writes=[st.B_rs])
            op("dve", lambda e: e.scalar_tensor_tensor(st.nm[:], st.mv[:, 0:1], -1.0, st.rs[:], ALU.mult, ALU.mult),
               reads=[st.B_mv, st.B_rs], writes=[st.B_nm])
            op("act", lambda e: e.activation(st.tmp[:], src[:], AF.Identity, bias=st.nm[:], scale=st.rs[:]),
               reads=[B_src, st.B_nm, st.B_rs], writes=[st.B_tmp])
            op("dve", lambda e: e.tensor_tensor(st.tmp[:], st.tmp[:], g, ALU.mult), reads=[st.B_tmp, B_g], writes=[st.B_tmp])
            op("pool", lambda e: e.tensor_tensor(dst[:], st.tmp[:], b, ALU.add), reads=[st.B_tmp, B_b], writes=[B_dst])


        def pass_P0():
            with ExitStack() as es1:
                lng, B_lng = load_bcast(es1, I["lnall"][0], D, "lng")
                lnb, B_lnb = load_bcast(es1, I["lnall"][1], D, "lnb")
                lnst = make_ln(es1)
                xt = [sbt(es1, [128, D], F32, "xt") for _ in range(2)]
                yt = [sbt(es1, [128, D], F32, "yt") for _ in range(2)]
                zt, B_zt = sbt(es1, [128, D], BF16, "zt")
                op("pool", lambda e: e.memset(zt[:], 0.0), writes=[B_zt])
                nz = PROWS // 128; zi = 0
                dma("sp", xt[0][0][:], I["x"][0:128, :], writes=[xt[0][1]])
                for t in range(NT):
                    while zi < nz * (t + 1) // NT:
                        dma("sp", XS[zi * 128:(zi + 1) * 128, :], zt[:], reads=[B_zt], writes=[B_XS])
                        zi += 1
                    if t + 1 < NT:
                        dma("sp", xt[(t + 1) % 2][0][:], I["x"][(t + 1) * 128:(t + 2) * 128, :], writes=[xt[(t + 1) % 2][1]])
                    x_, Bx = xt[t % 2]; y_, By = yt[t % 2]
                    layernorm(lnst, x_, Bx, y_, By, lng[:], B_lng, lnb[:], B_lnb)
                    dma("sp", H[t * 128:(t + 1) * 128, :], y_[:], reads=[By], writes=[B_H[t]])
                S_.barrier()

        def pass_R(l):
            with ExitStack() as es1:
                win = I["w_in"][l]
                wA, B_wA = load_w(es1, win[:, 0:3072], 8, 3072, "wA")
                wG, B_wG = load_w(es1, win[:, 4608:5632], 8, 1024, "wG")
                wP, B_wP = load_w(es1, I["w_proj_ret"][l], 8, 1024, "wP")
                cos2, B_cos2 = sbt(es1, [128, NT, 64], BF16, "cos2")
                sins, B_sins = sbt(es1, [128, NT, 64], BF16, "sins")
                GT = min(NT, 16)
                with ExitStack() as es2:
                    u, B_u = sbt(es2, [128, GT, 2, 64], F32, "u")
                    ki, B_ki = sbt(es2, [128, GT, 2, 64], I32, "ki")
                    kf, B_kf = sbt(es2, [128, GT, 2, 64], F32, "kf")
                    FQ = ctab("FQ")
                    uf = u[:].rearrange("p a b c -> p (a b c)"); kif = ki[:].rearrange("p a b c -> p (a b c)")
                    kff = kf[:].rearrange("p a b c -> p (a b c)")
                    for t0 in range(0, NT, GT):
                        for tt in range(GT):
                            op("dve", lambda e: e.tensor_scalar(u[:, tt, 0, :], FQ, posf[:, t0 + tt:t0 + tt + 1], None, ALU.mult),
                               reads=[B_cst, B_posf], writes=[B_u])
                        op("dve", lambda e: e.tensor_scalar(u[:, :, 1, :], u[:, :, 0, :], 0.25, None, ALU.add), reads=[B_u], writes=[B_u])
                        op("dve", lambda e: e.tensor_copy(kif, uf), reads=[B_u], writes=[B_ki])
                        op("dve", lambda e: e.tensor_copy(kff, kif), reads=[B_ki], writes=[B_kf])
                        op("dve", lambda e: e.tensor_tensor(uf, uf, kff, ALU.subtract), reads=[B_u, B_kf], writes=[B_u])
                        op("dve", lambda e: e.tensor_scalar(kff, uf, 0.5, None, ALU.is_gt), reads=[B_u], writes=[B_kf])
                        op("dve", lambda e: e.tensor_tensor(uf, uf, kff, ALU.subtract), reads=[B_u, B_kf], writes=[B_u])
                        op("dve", lambda e: e.tensor_scalar(kff, uf, -0.5, None, ALU.is_lt), reads=[B_u], writes=[B_kf])
                        op("dve", lambda e: e.tensor_tensor(uf, uf, kff, ALU.add), reads=[B_u, B_kf], writes=[B_u])
                        op("act", lambda e: e.activation(kff, uf, AF.Sin, scale=float(2 * np.pi)), reads=[B_u], writes=[B_kf])
                        op("dve", lambda e: e.tensor_copy(cos2[:, t0:t0 + GT, :], kf[:, :, 1, :]), reads=[B_kf], writes=[B_cos2])
                        op("dve", lambda e: e.tensor_scalar(sins[:, t0:t0 + GT, 0:32], kf[:, :, 0, 0:32], -1.0, None, ALU.mult), reads=[B_kf], writes=[B_sins])
                        op("dve", lambda e: e.tensor_copy(sins[:, t0:t0 + GT, 32:64], kf[:, :, 0, 32:64]), reads=[B_kf], writes=[B_sins])
                    S_.barrier()
                DM = ctab("DM").rearrange("p (h i) -> p h i", h=8)
                QD = ctab("QD").rearrange("p (c i) -> p c i", c=4)
                CDt = ctab("CD"); KD = ctab("KD")
                ht = [sbt(es1, [128, D], F32, "ht") for _ in range(2)]
                hb, B_hb = sbt(es1, [128, D], BF16, "hb")
                hT, B_hT = sbt(es1, [128, 8, 128], BF16, "hT")
                rA, B_rA = sbt(es1, [128, 16, 64], F32, "rA")
                rB, B_rB = sbt(es1, [128, 16, 64], F32, "rB")
                rots = [sbt(es1, [128, 16, 64], BF16, "rot") for _ in range(2)]
                kps = [sbt(es1, [128, 8, 64], BF16, "kp") for _ in range(2)]
                vrs = [sbt(es1, [128, D], BF16, "vr") for _ in range(2)]
                sgs = [sbt(es1, [128, D], BF16, "sg") for _ in range(2)]
                sgrs = [sbt(es1, [128, D], BF16, "sgr") for _ in range(2)]
                qkA, B_qkA = sbt(es1, [128, 8, 128], BF16, "qkA")
                kT = qkA[:, 4:8, :]; B_kT = B_qkA
                qTe, B_qTe = sbt(es1, [128, 4, 128], BF16, "qTe")
                qTo, B_qTo = sbt(es1, [128, 4, 128], BF16, "qTo")
                qpe, B_qpe = sbt(es1, [128, 4, 128], BF16, "qpe")
                qpo, B_qpo = sbt(es1, [128, 4, 128], BF16, "qpo")
                for (t_, B_) in ((qTe, B_qTe), (qTo, B_qTo), (qpe, B_qpe), (qpo, B_qpo)):
                    op("pool", lambda e: e.memset(t_[:], 0.0), writes=[B_])
                qsel = [(qTe, B_qTe), (qTo, B_qTo)]; qpsel = [(qpe, B_qpe), (qpo, B_qpo)]
                sTd, B_sTd = sbt(es1, [128, 8, 128], BF16, "sTd")
                state, B_state = sbt(es1, [128, 4, 128], F32, "state")
                stbf, B_stbf = sbt(es1, [128, 4, 128], BF16, "stbf")
                sq = rB[:].rearrange("p (a x) b -> p a (x b)", a=8); B_sq = B_rB
                dsb = rA[:].rearrange("p a b -> p (a b)"); B_dsb = B_rA
                st8, B_st8 = sbt(es1, [128, 8, 6], F32, "st8")
                mv8, B_mv8 = sbt(es1, [128, 8, 2], F32, "mv8")
                rs8, B_rs8 = sbt(es1, [128, 8], F32, "rs8")
                yr, B_yr = sbt(es1, [128, D], BF16, "yr")
                yrT, B_yrT = sbt(es1, [128, 8, 128], BF16, "yrT")
                m1 = [sbt(es1, [128, D], BF16, "m1")] * 2

                def loadh(t):
                    dma("sp", ht[t % 2][0][:], H[t * 128:(t + 1) * 128, :], reads=[B_H[t]], writes=[ht[t % 2][1]])

                def F1(t):
                    h_, B_h = ht[t % 2]
                    rot, B_rot = rots[t % 2]; kp, B_kp = kps[t % 2]; vr, B_vr = vrs[t % 2]
                    op("act", lambda e: e.copy(hb[:], h_[:]), reads=[B_h], writes=[B_hb])
                    transposes(hb, B_hb, 8, hT, B_hT, eng="dve")
                    pqk, B_pqk = next_pf()
                    mm_tok(pqk, B_pqk, hT, B_hT, 8, wA, B_wA, 0, 1024)
                    pv, B_pv = next_pf()
                    mm_tok(pv, B_pv, hT, B_hT, 8, wA, B_wA, 1024, 1024)
                    z3 = pqk[:].rearrange("p (a b) -> p a b", b=64)
                    cb = cos2[:, t, :].unsqueeze(1).broadcast_to([128, 16, 64])
                    op("dve", lambda e: e.tensor_tensor(rA[:], z3, cb, ALU.mult), reads=[B_pqk, B_cos2], writes=[B_rA])
                    nsb = sins[:, t, 0:32].unsqueeze(1).broadcast_to([128, 16, 32])
                    psb = sins[:, t, 32:64].unsqueeze(1).broadcast_to([128, 16, 32])
                    op("dve", lambda e: e.tensor_tensor(rB[:, :, 0:32], z3[:, :, 32:64], nsb, ALU.mult), reads=[B_pqk, B_sins], writes=[B_rB])
                    op("dve", lambda e: e.tensor_tensor(rB[:, :, 32:64], z3[:, :, 0:32], psb, ALU.mult), reads=[B_pqk, B_sins], writes=[B_rB])
                    op("pool", lambda e: e.tensor_tensor(rot[:], rA[:], rB[:], ALU.add), reads=[B_rA, B_rB], writes=[B_rot])
                    kdb = KD.unsqueeze(2).broadcast_to([128, 8, 64])
                    op("pool", lambda e: e.tensor_tensor(kp[:], rot[:, 8:16, :], kdb, ALU.mult), reads=[B_rot, B_cst], writes=[B_kp])
                    op("act", lambda e: e.copy(vr[:], pv[:]), reads=[B_pv], writes=[B_vr])

                def F2(t):
                    sg, B_sg = sgs[t % 2]; sgr, B_sgr = sgrs[t % 2]
                    pg, B_pg = next_pf()
                    mm_tok(pg, B_pg, hT, B_hT, 8, wA, B_wA, 2048, 1024)
                    op("act", lambda e: e.activation(sg[:], pg[:], AF.Sigmoid), reads=[B_pg], writes=[B_sg])
                    op("dve", lambda e: e.tensor_tensor(sg[:], pg[:], sg[:], ALU.mult), reads=[B_pg, B_sg], writes=[B_sg])
                    pgr, B_pgr = next_pf()
                    mm_tok(pgr, B_pgr, hT, B_hT, 8, wG, B_wG, 0, 1024)
                    op("act", lambda e: e.activation(sgr[:], pgr[:], AF.Sigmoid), reads=[B_pgr], writes=[B_sgr])

                def Bk1(t):
                    ts = t % TS
                    rot, B_rot = rots[t % 2]; kp, B_kp = kps[t % 2]; vr, B_vr = vrs[t % 2]
                    rotf = rot[:].rearrange("p a b -> p (a b)")
                    pt, B_pt = next_pt()
                    for k in range(8):
                        op("pe", lambda e: e.transpose(pt[:, k, :], rotf[:, k * 128:(k + 1) * 128], idb[:]),
                           reads=[B_rot, B_idb], writes=[B_pt], sig=(k == 7))
                    op("dve", lambda e: e.tensor_copy(qkA[:], pt[:]), reads=[B_pt], writes=[B_qkA])
                    op("act", lambda e: e.copy(qTe[0:64, :, :], qkA[0:64, 0:4, :]), reads=[B_qkA], writes=[B_qTe])
                    op("act", lambda e: e.copy(qTo[64:128, :, :], qkA[64:128, 0:4, :]), reads=[B_qkA], writes=[B_qTo])
                    op("pool", lambda e: e.tensor_tensor(qpe[0:64, :, :], qkA[0:64, 0:4, :], QD[0:64], ALU.mult), reads=[B_qkA, B_cst], writes=[B_qpe])
                    op("pool", lambda e: e.tensor_tensor(qpo[64:128, :, :], qkA[64:128, 0:4, :], QD[64:128], ALU.mult), reads=[B_qkA, B_cst], writes=[B_qpo])
                    psc, B_psc = next_pf()
                    psc3 = psc[:].rearrange("p (h i) -> p h i", h=8)
                    for h in range(8):
                        c = h // 2
                        qs_, B_qs = qsel[h % 2]
                        op("pe", lambda e: e.matmul(psc3[:, h, :], kT[:, c, :], qs_[:, c, :], start=True, stop=True),
                           reads=[B_kT, B_qs], writes=[B_psc], sig=(h == 7))
                    op("dve", lambda e: e.tensor_tensor(sTd[:], psc3, DM, ALU.mult), reads=[B_psc, B_cst], writes=[B_sTd])
                    po, B_po = next_pf()
                    po3 = po[:].rearrange("p (h i) -> p h i", h=8)
                    for h in range(8):
                        c = h // 2
                        op("pe", lambda e: e.matmul(po3[:, h, :], sTd[:, h, :], vr[:, h * 128:(h + 1) * 128], start=True, stop=(ts == 0)),
                           reads=[B_sTd, B_vr], writes=[B_po], sig=(ts == 0 and h == 7))
                        if ts > 0:
                            qp_, B_qp = qpsel[h % 2]
                            op("pe", lambda e: e.matmul(po3[:, h, :], qp_[:, c, :], stbf[:, c, :], start=False, stop=True),
                               reads=[B_qp, B_stbf], writes=[B_po], sig=(h == 7))
                    pds, B_pds = next_pf()
                    pds4 = pds[:].rearrange("p (a c e) -> p a c e", a=2, c=4)
                    kpf = kp[:].rearrange("p a b -> p (a b)")
                    for h in range(8):
                        c = h // 2; par = h % 2
                        op("pe", lambda e: e.matmul(pds4[:, par, c, :], kpf[:, c * 128:(c + 1) * 128], vr[:, h * 128:(h + 1) * 128], start=True, stop=True),
                           reads=[B_kp, B_vr], writes=[B_pds], sig=(h == 7))
                    op("act", lambda e: e.copy(dsb, pds[:]), reads=[B_pds], writes=[B_dsb])
                    dsb4 = dsb.rearrange("p (a c e) -> p a c e", a=2, c=4)
                    if ts == 0:
                        for par in range(2):
                            b0 = par * 64
                            op("dve", lambda e: e.tensor_copy(state[b0:b0 + 64, :, :], dsb4[b0:b0 + 64, par, :, :]), reads=[B_dsb], writes=[B_state])
                    else:
                        op("pool", lambda e: e.tensor_tensor(state[:], state[:], CDt.unsqueeze(2).broadcast_to([128, 4, 128]), ALU.mult),
                           reads=[B_state, B_cst], writes=[B_state])
                        for par in range(2):
                            b0 = par * 64
                            op("dve", lambda e: e.tensor_tensor(state[b0:b0 + 64, :, :], state[b0:b0 + 64, :, :], dsb4[b0:b0 + 64, par, :, :], ALU.add),
                               reads=[B_dsb, B_state], writes=[B_state])
                    op("act", lambda e: e.copy(stbf[:], state[:]), reads=[B_state], writes=[B_stbf])
                    op("act", lambda e: e.copy(sq.rearrange("p a b -> p (a b)"), po[:]), reads=[B_po], writes=[B_sq])
                    for h in range(8):
                        op("dve", lambda e: e.bn_stats(st8[:, h, :], sq[:, h, :]), reads=[B_sq], writes=[B_st8])
                    for h in range(8):
                        op("dve", lambda e: e.bn_aggr(mv8[:, h, :], st8[:, h, :]), reads=[B_st8], writes=[B_mv8])
                    op("dve", lambda e: e.tensor_scalar(rs8[:], mv8[:, :, 1], LN_EPS, None, ALU.add), reads=[B_mv8], writes=[B_rs8])
                    op("pool", lambda e: e.tensor_tensor(rs8[:], rs8[:], mhalf[:], ALU.pow), reads=[B_rs8, B_mhalf], writes=[B_rs8])
                    for h in range(8):
                        op("dve", lambda e: e.tensor_scalar(sq[:, h, :], sq[:, h, :], mv8[:, h, 0:1], rs8[:, h:h + 1], ALU.subtract, ALU.mult),
                           reads=[B_sq, B_mv8, B_rs8], writes=[B_sq])

                def Bk2(t):
                    sg, B_sg = sgs[t % 2]; sgr, B_sgr = sgrs[t % 2]
                    op("pool", lambda e: e.tensor_tensor(yr[:], sq.rearrange("p a b -> p (a b)"), sg[:], ALU.mult), reads=[B_sq, B_sg], writes=[B_yr])
                    transposes(yr, B_yr, 8, yrT, B_yrT, eng="dve")
                    pp, B_pp = next_pf()
                    mm_tok(pp, B_pp, yrT, B_yrT, 8, wP, B_wP, 0, 1024)
                    m1_, B_m1 = m1[t % 2]
                    op("dve", lambda e: e.tensor_tensor(m1_[:], pp[:], sgr[:], ALU.mult), reads=[B_pp, B_sgr], writes=[B_m1])
                    dma("sp", M1[t * 128:(t + 1) * 128, :], m1_[:], reads=[B_m1], writes=[B_M1[t]])

                loadh(0)
                if NT > 1:
                    loadh(1)
                F1(0); F2(0)
                for t in range(NT):
                    if t + 1 < NT:
                        F1(t + 1)
                    if t + 2 < NT:
                        loadh(t + 2)
                    Bk1(t)
                    if t + 1 < NT:
                        F2(t + 1)
                    Bk2(t)
                S_.barrier()

        def next_pf_ex(excl):
            while True:
                p = next_pf()
                if all(p[0] is not x[0] for x in excl):
                    return p

        def pass_B1(l):
            with ExitStack() as es1:
                win = I["w_in"][l]
                wB, B_wB = load_w(es1, win[:, 3072:4608], 8, 1536, "wB")
                wGa, B_wGa = load_w(es1, win[:, 5632:6656], 8, 1024, "wGa")
                wPa, B_wPa = load_w(es1, I["w_proj_att"][l], 4, 1024, "wPa")
                wO, B_wO = load_w(es1, I["w_out"][l], 8, 1024, "wO")
                g1, B_g1 = load_bcast(es1, I["lnall"][2 + 6 * l + 0], D, "g1")
                b1, B_b1 = load_bcast(es1, I["lnall"][2 + 6 * l + 1], D, "b1")
                lnst = make_ln(es1)
                btn, B_btn = sbt(es1, [128, 5, 8, 128], BF16, "btn")
                with ExitStack() as es2:
                    bt2, B_bt2 = sbt(es2, [128, 5, 8, 128], BF16, "bt2")
                    rbt = I["rb_ext"].tensor
                    for r in range(5):
                        src = bass.AP(tensor=rbt, offset=l * 8 * RB_EXT + 256 + (4 - r) * 128 - 127, ap=[[1, 128], [RB_EXT, 8], [1, 128]])
                        dma("pool", bt2[:, r, :, :], src, writes=[B_bt2])
                    op("pool", lambda e: e.memset(bt2[64:128, 0, :, 64:128], -30000.0), writes=[B_bt2])
                    op("pool", lambda e: e.memset(bt2[0:64, 4, :, 0:64], -30000.0), writes=[B_bt2])
                    antib, B_antib = sbt(es2, [128, 128], BF16, "antib")
                    op("dve", lambda e: e.tensor_copy(antib[:], ctab("ANTI")), reads=[B_cst], writes=[B_antib])
                    for r in range(5):
                        pf, B_pf = next_pf()
                        for hq in range(2):
                            op("pe", lambda e: e.matmul(pf[:, hq * 512:(hq + 1) * 512], antib[:], bt2[:, r, hq * 4:(hq + 1) * 4, :], start=True, stop=True),
                               reads=[B_antib, B_bt2], writes=[B_pf], sig=(hq == 1))
                        op("act", lambda e: e.copy(btn[:, r, :, :], pf[:].rearrange("p (h i) -> p h i", h=8)), reads=[B_pf], writes=[B_btn])
                    S_.barrier()
                ht = [sbt(es1, [128, D], F32, "ht") for _ in range(2)]
                m1 = [sbt(es1, [128, D], BF16, "m1") for _ in range(2)]
                hb, B_hb = sbt(es1, [128, D], BF16, "hb")
                hT, B_hT = sbt(es1, [128, 8, 128], BF16, "hT")
                qaA, B_qaA = sbt(es1, [128, 4, 128], BF16, "qaA")
                qkt, B_qkt = sbt(es1, [128, D], BF16, "qkt")
                sb5s = [sbt(es1, [128, 5, 128], F32, "sb5") for _ in range(2)]
                qasels = []
                for _ in range(2):
                    qaTe, B_qaTe = sbt(es1, [128, 4, 128], BF16, "qaTe")
                    qaTo, B_qaTo = sbt(es1, [128, 4, 128], BF16, "qaTo")
                    for (t_, B_) in ((qaTe, B_qaTe), (qaTo, B_qaTo)):
                        op("pool", lambda e: e.memset(t_[:], 0.0), writes=[B_])
                    qasels.append([(qaTe, B_qaTe), (qaTo, B_qaTo)])
                kring = [sbt(es1, [128, 4, 128], BF16, "kring") for _ in range(6)]
                vring = [sbt(es1, [128, 8, 65], BF16, "vring") for _ in range(6)]
                for v_, Bv in vring:
                    op("pool", lambda e: e.memset(v_[:, :, 64:65], 1.0), writes=[Bv])
                sgas = [sbt(es1, [128, D], BF16, "sga") for _ in range(2)]
                pTs = [sbt(es1, [128, 5, 128], BF16, "pT") for _ in range(2)]
                rden, B_rden = sbt(es1, [128, 8], F32, "rden")
                ya, B_ya = sbt(es1, [128, 8, 64], BF16, "ya")
                yaT, B_yaT = sbt(es1, [128, 4, 128], BF16, "yaT")
                t2, B_t2 = sbt(es1, [128, D], F32, "t2")
                posb = t2[:].rearrange("p (a b) -> p a b", a=8); B_posb = B_t2
                hs = t2; B_hs = B_t2
                mg, B_mg = sbt(es1, [128, D], BF16, "mg")
                mT, B_mT = sbt(es1, [128, 8, 128], BF16, "mT")
                rs_, B_rs = sbt(es1, [128, D], F32, "resid")
                h1 = [sbt(es1, [128, D], F32, "h1")] * 2

                def loads(t):
                    dma("sp", ht[t % 2][0][:], H[t * 128:(t + 1) * 128, :], reads=[B_H[t]], writes=[ht[t % 2][1]])
                    dma("sp", m1[t % 2][0][:], M1[t * 128:(t + 1) * 128, :], reads=[B_M1[t]], writes=[m1[t % 2][1]])
                def Fa(t):
                    ts = t % TS
                    h_, B_h = ht[t % 2]
                    vr_, B_vr = vring[ts % 6]
                    op("act", lambda e: e.copy(hb[:], h_[:]), reads=[B_h], writes=[B_hb])
                    transposes(hb, B_hb, 8, hT, B_hT, eng="dve")
                    pq, B_pq = next_pf()
                    mm_tok(pq, B_pq, hT, B_hT, 8, wB, B_wB, 0, 1024)
                    op("dve", lambda e: e.tensor_scalar(qkt[:, 0:512], pq[:, 0:512], 0.125, None, ALU.mult), reads=[B_pq], writes=[B_qkt])
                    op("act", lambda e: e.copy(qkt[:, 512:1024], pq[:, 512:1024]), reads=[B_pq], writes=[B_qkt])
                    pv, B_pv = next_pf()
                    mm_tok(pv, B_pv, hT, B_hT, 8, wB, B_wB, 1024, 512)
                    op("act", lambda e: e.copy(vr_[:, :, 0:64], pv[:, 0:512].rearrange("p (h d) -> p h d", h=8)), reads=[B_pv], writes=[B_vr])

                def Fb(t):
                    ts = t % TS
                    kr, B_kr = kring[ts % 6]
                    (qaTe, B_qaTe), (qaTo, B_qaTo) = qasels[t % 2]
                    sga, B_sga = sgas[t % 2]
                    ptq, B_ptq = next_pt()
                    for k in range(8):
                        op("pe", lambda e: e.transpose(ptq[:, k, :], qkt[:, k * 128:(k + 1) * 128], idb[:]),
                           reads=[B_qkt, B_idb], writes=[B_ptq], sig=(k == 7))
                    op("dve", lambda e: e.tensor_copy(qaA[:], ptq[:, 0:4, :]), reads=[B_ptq], writes=[B_qaA])
                    op("dve", lambda e: e.tensor_copy(kr[:], ptq[:, 4:8, :]), reads=[B_ptq], writes=[B_kr])
                    op("act", lambda e: e.copy(qaTe[0:64, :, :], qaA[0:64, :, :]), reads=[B_qaA], writes=[B_qaTe])
                    op("act", lambda e: e.copy(qaTo[64:128, :, :], qaA[64:128, :, :]), reads=[B_qaA], writes=[B_qaTo])
                    pg, B_pg = next_pf()
                    mm_tok(pg, B_pg, hT, B_hT, 8, wGa, B_wGa, 0, 1024)
                    op("act", lambda e: e.activation(sga[:], pg[:], AF.Sigmoid), reads=[B_pg], writes=[B_sga])

                loads(0)
                if NT > 1:
                    loads(1)
                Fa(0); Fb(0)
                for t in range(NT):
                    ts = t % TS
                    h_, B_h = ht[t % 2]; m1_, B_m1 = m1[t % 2]
                    qasel = qasels[t % 2]
                    sga, B_sga = sgas[t % 2]
                    po = next_pf()
                    po3 = po[0][:].rearrange("p (h i) -> p h i", h=8)
                    r_lo = max(0, 4 - ts)
                    def scores(h):
                        c = h // 2
                        ps, B_ps = next_pf_ex([po])
                        ps5 = ps[:, 0:640].rearrange("p (r i) -> p r i", r=5)
                        for r in range(r_lo, 5):
                            kt = ts - 4 + r
                            kk, B_kk = kring[kt % 6]
                            qa_, B_qa = qasel[h % 2]
                            op("pe", lambda e: e.matmul(ps5[:, r, :], kk[:, c, :], qa_[:, c, :], start=True, stop=True),
                               reads=[B_kk, B_qa], writes=[B_ps], sig=(r == 4))
                        return ps5, B_ps
                    pss = {0: scores(0)}
                    for h in range(8):
                        if h + 1 < 8:
                            pss[h + 1] = scores(h + 1)
                        ps5, B_ps = pss[h]
                        pT, B_pT = pTs[h % 2]
                        sb5, B_sb5 = sb5s[h % 2]
                        op("dve", lambda e: e.tensor_tensor(sb5[:, r_lo:5, :], ps5[:, r_lo:5, :], btn[:, r_lo:5, h, :], ALU.add),
                           reads=[B_ps, B_btn], writes=[B_sb5])
                        op("act", lambda e: e.activation(pT[:, r_lo:5, :], sb5[:, r_lo:5, :], AF.Exp), reads=[B_sb5], writes=[B_pT])
                        for r in range(r_lo, 5):
                            kt = ts - 4 + r
                            vv, B_vv = vring[kt % 6]
                            op("pe", lambda e: e.matmul(po3[:, h, 0:65], pT[:, r, :], vv[:, h, :], start=(r == r_lo), stop=(r == 4)),
                               reads=[B_pT, B_vv], writes=[po[1]], sig=(r == 4))
                    op("act", lambda e: e.copy(t2[:], po[0][:]), reads=[po[1]], writes=[B_posb])
                    op("dve", lambda e: e.reciprocal(rden[:], posb[:, :, 64]), reads=[B_posb], writes=[B_rden])
                    for h in range(8):
                        op("dve", lambda e: e.tensor_scalar(ya[:, h, :], posb[:, h, 0:64], rden[:, h:h + 1], None, ALU.mult),
                           reads=[B_posb, B_rden], writes=[B_ya])
                    if t + 1 < NT:
                        Fa(t + 1)
                    transposes(ya[:].rearrange("p a b -> p (a b)"), B_ya, 4, yaT, B_yaT, eng="act")
                    pp, B_pp = next_pf()
                    mm_tok(pp, B_pp, yaT, B_yaT, 4, wPa, B_wPa, 0, 1024)
                    op("dve", lambda e: e.tensor_tensor(t2[:], pp[:], sga[:], ALU.mult), reads=[B_pp, B_sga], writes=[B_t2])
                    op("pool", lambda e: e.tensor_tensor(mg[:], t2[:], m1_[:], ALU.add), reads=[B_t2, B_m1], writes=[B_mg])
                    if t + 1 < NT:
                        Fb(t + 1)
                    transposes(mg, B_mg, 8, mT, B_mT, eng="dve")
                    px, B_px = next_pf()
                    mm_tok(px, B_px, mT, B_mT, 8, wO, B_wO, 0, 1024)
                    op("act", lambda e: e.mul(hs[:], h_[:], ALPHA), reads=[B_h], writes=[B_hs])
                    op("dve", lambda e: e.tensor_tensor(rs_[:], px[:], hs[:], ALU.add), reads=[B_hs, B_px], writes=[B_rs])
                    if t + 2 < NT:
                        loads(t + 2)
                    h1_, B_h1 = h1[t % 2]
                    layernorm(lnst, rs_, B_rs, h1_, B_h1, g1[:], B_g1, b1[:], B_b1)
                    dma("sp", H1[t * 128:(t + 1) * 128, :], h1_[:], reads=[B_h1], writes=[B_H1[t]])
                S_.barrier()

        def pass_B2(l):
            with ExitStack() as es1:
                wQ, B_wQ = load_w(es1, I["w_q_mem"][l], 8, 1024, "wQ")
                wOm, B_wOm = load_w(es1, I["w_o_mem"][l], 8, 1024, "wOm")
                g2, B_g2 = load_bcast(es1, I["lnall"][2 + 6 * l + 2], D, "g2")
                b2, B_b2 = load_bcast(es1, I["lnall"][2 + 6 * l + 3], D, "b2")
                brt, B_brt = load_bcast(es1, I["br"][l], 36, "brt")
                wrt, B_wrt = sbt(es1, [128, 8, 36], F32, "wrt")
                dma("sp", wrt[:], I["wr"][l].rearrange("(k p) n -> p k n", p=128), writes=[B_wrt])
                lnst = make_ln(es1)
                kTs = [sbt(es1, [128, 8, NMEM], BF16, "kTs") for _ in range(NSEQ)]
                Vs = [sbt(es1, [128, 2, D], BF16, "Vs") for _ in range(NSEQ)]
                with ExitStack() as es2:
                    wkv, B_wkv = load_w(es2, I["w_kv_mem"][l], 8, 2048, "wkv")
                    memb, B_memb = sbt(es2, [128, 2, D], BF16, "memb")
                    memT, B_memT = sbt(es2, [128, 8, NMEM], BF16, "memT")
                    for s_ in range(NSEQ):
                        for mc in range(2):
                            dma("pool", memb[:, mc, :], I["mem"][s_ * NMEM + mc * 128:s_ * NMEM + (mc + 1) * 128, :], writes=[B_memb])
                        for mc in range(2):
                            pt, B_pt = next_pt()
                            for k in range(8):
                                op("pe", lambda e: e.transpose(pt[:, k, :], memb[:, mc, k * 128:(k + 1) * 128], idb[:]),
                                   reads=[B_memb, B_idb], writes=[B_pt], sig=(k == 7))
                            op("dve", lambda e: e.tensor_copy(memT[:, :, mc * 128:(mc + 1) * 128], pt[:]), reads=[B_pt], writes=[B_memT])
                        kT_, B_kT = kTs[s_]; V_, B_V = Vs[s_]
                        for half in range(2):
                            pf, B_pf = next_pf()
                            for c4 in range(4):
                                c = half * 4 + c4
                                for k in range(8):
                                    op("pe", lambda e: e.matmul(pf[:, c4 * 256:(c4 + 1) * 256], wkv[:, k, c * 128:(c + 1) * 128], memT[:, k, :],
                                                                start=(k == 0), stop=(k == 7)),
                                       reads=[B_wkv, B_memT], writes=[B_pf], sig=(k == 7 and c4 == 3))
                            op("dve", lambda e: e.tensor_scalar(kT_[:, half * 4:(half + 1) * 4, :], pf[:].rearrange("p (c m) -> p c m", c=4), 1.0 / 16, None, ALU.mult),
                               reads=[B_pf], writes=[B_kT])
                        for mc in range(2):
                            pf, B_pf = next_pf()
                            for g_ in range(2):
                                for k in range(8):
                                    op("pe", lambda e: e.matmul(pf[:, g_ * 512:(g_ + 1) * 512], memT[:, k, mc * 128:(mc + 1) * 128],
                                                                wkv[:, k, 1024 + g_ * 512:1024 + (g_ + 1) * 512], start=(k == 0), stop=(k == 7)),
                                       reads=[B_wkv, B_memT], writes=[B_pf], sig=(k == 7 and g_ == 1))
                            op("act", lambda e: e.copy(V_[:, mc, :], pf[:]), reads=[B_pf], writes=[B_V])
                    S_.barrier()
                ht = [sbt(es1, [128, D], F32, "h1t") for _ in range(2)]
                hb, B_hb = sbt(es1, [128, D], BF16, "hb")
                hT, B_hT = sbt(es1, [128, 8, 128], BF16, "hT")
                qmT, B_qmT = sbt(es1, [128, 8, 128], BF16, "qmT")
                qtok, B_qtok = sbt(es1, [128, D], BF16, "qtok")
                pm, B_pm = sbt(es1, [128, 8, 128], BF16, "pm")
                rden, B_rden = sbt(es1, [128, 4], F32, "rden")
                ob, B_ob = sbt(es1, [128, 4, 256], BF16, "ob")
                osb, B_osb = sbt(es1, [128, D], F32, "osb")
                hs, B_hs = sbt(es1, [128, D], F32, "hs")
                oT, B_oT = sbt(es1, [128, 8, 128], BF16, "oT")
                rs_, B_rs = sbt(es1, [128, D], F32, "resid")
                h2 = [sbt(es1, [128, D], F32, "h2") for _ in range(2)]
                h2T, B_h2T = sbt(es1, [128, 8, 128], F32, "h2T")
                sm, B_sm = sbt(es1, [128, 256], F32, "rsm")
                idf = ctab("IDENT")
                qmTs = [(qmT, B_qmT), sbt(es1, [128, 8, 128], BF16, "qmT2")]

                def loadh1(t):
                    dma("sp", ht[t % 2][0][:], H1[t * 128:(t + 1) * 128, :], reads=[B_H1[t]], writes=[ht[t % 2][1]])

                def Fa(t):
                    h_, B_h = ht[t % 2]
                    op("act", lambda e: e.copy(hb[:], h_[:]), reads=[B_h], writes=[B_hb])
                    transposes(hb, B_hb, 8, hT, B_hT, eng="dve")
                    pq, B_pq = next_pf()
                    mm_tok(pq, B_pq, hT, B_hT, 8, wQ, B_wQ, 0, 1024)
                    op("act", lambda e: e.copy(qtok[:], pq[:]), reads=[B_pq], writes=[B_qtok])

                def Fb(t):
                    q_, B_q = qmTs[t % 2]
                    transposes(qtok, B_qtok, 8, q_, B_q, eng="dve")

                loadh1(0)
                if NT > 1:
                    loadh1(1)
                Fa(0); Fb(0)
                for t in range(NT):
                    s_ = t // TS
                    kT_, B_kT = kTs[s_]; V_, B_V = Vs[s_]
                    h_, B_h = ht[t % 2]
                    qmT, B_qmT = qmTs[t % 2]
                    ps, B_ps = next_pf()
                    ps3 = ps[:].rearrange("p (a i) -> p a i", a=8)
                    for hh in range(4):
                        for mc in range(2):
                            for c2 in range(2):
                                c = hh * 2 + c2
                                op("pe", lambda e: e.matmul(ps3[:, hh * 2 + mc, :], kT_[:, c, mc * 128:(mc + 1) * 128], qmT[:, c, :], start=(c2 == 0), stop=(c2 == 1)),
                                   reads=[B_kT, B_qmT], writes=[B_ps], sig=(hh == 3 and mc == 1 and c2 == 1))
                    op("act", lambda e: e.activation(pm[:], ps3, AF.Exp), reads=[B_ps], writes=[B_pm])
                    po, B_po = next_pf()
                    pd, B_pd = next_pf()
                    for hh in range(4):
                        for mc in range(2):
                            op("pe", lambda e: e.matmul(po[:, hh * 256:(hh + 1) * 256], pm[:, hh * 2 + mc, :], V_[:, mc, hh * 256:(hh + 1) * 256], start=(mc == 0), stop=(mc == 1)),
                               reads=[B_pm, B_V], writes=[B_po], sig=(hh == 3 and mc == 1))
                    for hh in range(4):
                        for mc in range(2):
                            op("pe", lambda e: e.matmul(pd[:, hh:hh + 1], pm[:, hh * 2 + mc, :], onesb[:, 0:1], start=(mc == 0), stop=(mc == 1)),
                               reads=[B_pm, B_onesb], writes=[B_pd], sig=(hh == 3 and mc == 1))
                    op("act", lambda e: e.copy(osb[:], po[:]), reads=[B_po], writes=[B_osb])
                    op("act", lambda e: e.copy(rden[:], pd[:, 0:4]), reads=[B_pd], writes=[B_rden])
                    op("dve", lambda e: e.reciprocal(rden[:], rden[:]), reads=[B_rden], writes=[B_rden])
                    for hh in range(4):
                        op("dve", lambda e: e.tensor_scalar(ob[:, hh, :], osb[:, hh * 256:(hh + 1) * 256], rden[:, hh:hh + 1], None, ALU.mult),
                           reads=[B_osb, B_rden], writes=[B_ob])
                    if t + 1 < NT:
                        Fa(t + 1)
                    transposes(ob[:].rearrange("p a b -> p (a b)"), B_ob, 8, oT, B_oT, eng="act")
                    px, B_px = next_pf()
                    mm_tok(px, B_px, oT, B_oT, 8, wOm, B_wOm, 0, 1024)
                    op("act", lambda e: e.mul(hs[:], h_[:], ALPHA), reads=[B_h], writes=[B_hs])
                    op("dve", lambda e: e.tensor_tensor(rs_[:], px[:], hs[:], ALU.add), reads=[B_hs, B_px], writes=[B_rs])
                    h2_, B_h2 = h2[t % 2]
                    layernorm(lnst, rs_, B_rs, h2_, B_h2, g2[:], B_g2, b2[:], B_b2)
                    dma("sp", H2[t * 128:(t + 1) * 128, :], h2_[:], reads=[B_h2], writes=[B_H2[t]])
                    if t + 2 < NT:
                        loadh1(t + 2)
                    if t + 1 < NT:
                        Fb(t + 1)
                    pt_, B_ptf = next_pf()
                    pt3 = pt_[:].rearrange("p (c i) -> p c i", c=8)
                    for k in range(8):
                        op("pe", lambda e: e.transpose(pt3[:, k, :], h2_[:, k * 128:(k + 1) * 128], idf), reads=[B_h2, B_cst], writes=[B_ptf], sig=(k == 7))
                    op("act", lambda e: e.copy(h2T[:], pt3), reads=[B_ptf], writes=[B_h2T])
                    pl, B_pl = next_pf()
                    for k in range(8):
                        op("pe", lambda e: e.matmul(pl[:, 0:36], h2T[:, k, :], wrt[:, k, :], start=(k == 0), stop=(k == 7)),
                           reads=[B_h2T, B_wrt], writes=[B_pl], sig=(k == 7))
                    lg = sm[:, 0:36]; gmax = sm[:, 36:37]; gm = sm[:, 40:44]; ngmax = sm[:, 37:38]; gexp = sm[:, 44:48]
                    gsum = sm[:, 38:39]; gw = sm[:, 39:40]; pen = sm[:, 48:52]; em = sm[:, 64:96]; mk1 = sm[:, 96:128]
                    em2 = sm[:, 128:160]; mk2 = sm[:, 160:192]; m1v = sm[:, 52:53]; m2v = sm[:, 53:54]; dd = sm[:, 54:55]
                    e2 = sm[:, 55:56]; den = sm[:, 56:57]; w1 = sm[:, 57:58]; w2 = sm[:, 58:59]
                    R_ = [B_sm]
                    op("dve", lambda e: e.tensor_tensor(lg, pl[:, 0:36], brt[:], ALU.add), reads=[B_pl, B_brt], writes=R_)
                    op("dve", lambda e: e.tensor_reduce(gmax, lg[:, 0:4], AX.X, ALU.max), reads=R_, writes=R_)
                    op("dve", lambda e: e.tensor_scalar(gm, lg[:, 0:4], gmax, None, ALU.is_equal), reads=R_, writes=R_)
                    op("dve", lambda e: e.tensor_scalar(ngmax, gmax, -1.0, None, ALU.mult), reads=R_, writes=R_)
                    op("act", lambda e: e.activation(gexp, lg[:, 0:4], AF.Exp, bias=ngmax), reads=R_, writes=R_)
                    op("dve", lambda e: e.tensor_reduce(gsum, gexp, AX.X, ALU.add), reads=R_, writes=R_)
                    op("dve", lambda e: e.reciprocal(gw, gsum), reads=R_, writes=R_)
                    op("dve", lambda e: e.tensor_scalar(pen, gm, 1.0, 1e30, ALU.subtract, ALU.mult), reads=R_, writes=R_)
                    op("dve", lambda e: e.tensor_tensor(em.rearrange("p (g x) -> p g x", g=4), lg[:, 4:36].rearrange("p (g x) -> p g x", g=4),
                                                        pen.unsqueeze(2).broadcast_to([128, 4, 8]), ALU.add), reads=R_, writes=R_)
                    op("dve", lambda e: e.tensor_reduce(m1v, em, AX.X, ALU.max), reads=R_, writes=R_)
                    op("dve", lambda e: e.tensor_scalar(mk1, em, m1v, None, ALU.is_equal), reads=R_, writes=R_)
                    op("dve", lambda e: e.scalar_tensor_tensor(em2, mk1, -1e30, em, ALU.mult, ALU.add), reads=R_, writes=R_)
                    op("dve", lambda e: e.tensor_reduce(m2v, em2, AX.X, ALU.max), reads=R_, writes=R_)
                    op("dve", lambda e: e.tensor_scalar(mk2, em2, m2v, None, ALU.is_equal), reads=R_, writes=R_)
                    op("dve", lambda e: e.tensor_tensor(dd, m2v, m1v, ALU.subtract), reads=R_, writes=R_)
                    op("act", lambda e: e.activation(e2, dd, AF.Exp), reads=R_, writes=R_)
                    op("dve", lambda e: e.tensor_scalar(den, e2, 1.0, None, ALU.add), reads=R_, writes=R_)
                    op("dve", lambda e: e.reciprocal(w1, den), reads=R_, writes=R_)
                    op("dve", lambda e: e.tensor_tensor(w2, e2, w1, ALU.mult), reads=R_, writes=R_)
                    op("dve", lambda e: e.tensor_tensor(W1[:, t:t + 1], w1, gw, ALU.mult), reads=R_, writes=[B_W1])
                    op("dve", lambda e: e.tensor_tensor(W2[:, t:t + 1], w2, gw, ALU.mult), reads=R_, writes=[B_W2])
                    op("dve", lambda e: e.tensor_copy(MK1[:, t, :], mk1), reads=R_, writes=[B_MK1])
                    op("dve", lambda e: e.tensor_copy(MK2[:, t, :], mk2), reads=R_, writes=[B_MK2])
                S_.barrier()

        def pass_C0(l):
            with ExitStack() as es1:
                mk12, B_mk12 = sbt(es1, [128, NT, 32], BF16, "mk12")
                op("dve", lambda e: e.tensor_tensor(mk12[:], MK1[:], MK2[:], ALU.add), reads=[B_MK1, B_MK2], writes=[B_mk12])
                ustb, B_ustb = sbt(es1, [128, 128], BF16, "ustb")
                op("dve", lambda e: e.tensor_copy(ustb[:], ctab("USTRICT")), reads=[B_cst], writes=[B_ustb])
                pc, B_pc = next_pf()
                for t in range(NT):
                    op("pe", lambda e: e.matmul(pc[:, 0:32], onesb[:], mk12[:, t, :], start=(t == 0), stop=(t == NT - 1)),
                       reads=[B_mk12, B_onesb], writes=[B_pc], sig=(t == NT - 1))
                cs, B_cs = sbt(es1, [128, 32], F32, "cs")
                ci, B_ci = sbt(es1, [128, 3, 32], I32, "ci")
                op("act", lambda e: e.copy(cs[:], pc[:, 0:32]), reads=[B_pc], writes=[B_cs])
                op("dve", lambda e: e.tensor_scalar(cs[:], cs[:], 255.0, None, ALU.add), reads=[B_cs], writes=[B_cs])
                op("dve", lambda e: e.tensor_copy(ci[:, 0, :], cs[:]), reads=[B_cs], writes=[B_ci])
                op("dve", lambda e: e.tensor_scalar(ci[:, 1, :], ci[:, 0, :], 8, None, ALU.arith_shift_right), reads=[B_ci], writes=[B_ci])
                op("dve", lambda e: e.tensor_scalar(ci[:, 2, :], ci[:, 1, :], 8, None, ALU.logical_shift_left), reads=[B_ci], writes=[B_ci])
                pse, B_pse = sbt(es1, [128, 64], F32, "pse")
                cum = [sbt(es1, [128, 32], F32, "cum") for _ in range(2)]
                op("dve", lambda e: e.tensor_copy(cs[:], ci[:, 2, :]), reads=[B_ci], writes=[B_cs])
                op("dve", lambda e: e.tensor_copy(cum[0][0][:], cs[:]), reads=[B_cs], writes=[cum[0][1]])
                cur = 0
                for sh in (1, 2, 4, 8, 16):
                    a_, B_a = cum[cur]; b_, B_b = cum[1 - cur]
                    op("dve", lambda e: e.tensor_copy(b_[:, 0:sh], a_[:, 0:sh]), reads=[B_a], writes=[B_b])
                    op("dve", lambda e: e.tensor_tensor(b_[:, sh:32], a_[:, sh:32], a_[:, 0:32 - sh], ALU.add), reads=[B_a], writes=[B_b])
                    cur = 1 - cur
                inc_, B_inc = cum[cur]
                op("dve", lambda e: e.tensor_copy(pse[:, 32:64], inc_[:]), reads=[B_inc], writes=[B_pse])
                op("dve", lambda e: e.tensor_tensor(pse[:, 0:32], inc_[:], cs[:], ALU.subtract), reads=[B_inc, B_cs], writes=[B_pse])
                cmp_, B_cmp = sbt(es1, [128, NBLK, 32], F32, "cmp")
                bet, B_bet = sbt(es1, [128, NBLK], F32, "bet")
                BLK = ctab("BLK")
                op("dve", lambda e: e.tensor_tensor(cmp_[:], pse[:, 32:64].unsqueeze(1).broadcast_to([128, NBLK, 32]),
                                                    BLK.unsqueeze(2).broadcast_to([128, NBLK, 32]), ALU.is_le), reads=[B_pse, B_cst], writes=[B_cmp])
                op("dve", lambda e: e.tensor_reduce(bet[:], cmp_[:], AX.X, ALU.add), reads=[B_cmp], writes=[B_bet])
                op("dve", lambda e: e.tensor_scalar(bet[:], bet[:], 31.0, 256.0, ALU.min, ALU.mult), reads=[B_bet], writes=[B_bet])
                pb2, B_pb2 = sbt(es1, [128, 2], F32, "pb2")
                op("dve", lambda e: e.tensor_scalar(pb2[:, 0:1], ctab("PIDX"), 2.0, float(l * NE * 256), ALU.mult, ALU.add), reads=[B_cst], writes=[B_pb2])
                op("dve", lambda e: e.tensor_scalar(pb2[:, 1:2], pb2[:, 0:1], 1.0, None, ALU.add), reads=[B_pb2], writes=[B_pb2])
                wf, B_wf = sbt(es1, [128, NBLK, 2], F32, "wf")
                for c in range(2):
                    op("dve", lambda e: e.tensor_scalar(wf[:, :, c], bet[:], pb2[:, c:c + 1], None, ALU.add), reads=[B_bet, B_pb2], writes=[B_wf])
                op("dve", lambda e: e.tensor_copy(WIDX[:], wf[:]), reads=[B_wf], writes=[B_WIDX])
                mkc, B_mkc = sbt(es1, [128, 32], BF16, "mkc")
                op("pool", lambda e: e.memset(mkc[:], 0.0), writes=[B_mkc])
                dst, B_dst = sbt(es1, [128, 32], F32, "dst")
                tmp, B_tmp = sbt(es1, [128, 32], F32, "dtmp")
                dd, B_dd = sbt(es1, [128, 2], F32, "dd")
                ht = [sbt(es1, [128, D], F32, "h2t") for _ in range(2)]
                xb = [sbt(es1, [128, D], BF16, "xb") for _ in range(2)]
                dma("sp", ht[0][0][:], H2[0:128, :], reads=[B_H2[0]], writes=[ht[0][1]])
                for t in range(NT):
                    if t + 1 < NT:
                        dma("sp", ht[(t + 1) % 2][0][:], H2[(t + 1) * 128:(t + 2) * 128, :], reads=[B_H2[t + 1]], writes=[ht[(t + 1) % 2][1]])
                    pr, B_pr = next_pf()
                    op("pe", lambda e: e.matmul(pr[:, 0:32], ustb[:], mk12[:, t, :], start=True, stop=(t == 0)), reads=[B_ustb, B_mk12], writes=[B_pr], sig=(t == 0))
                    if t > 0:
                        op("pe", lambda e: e.matmul(pr[:, 0:32], onesb[:], mkc[:], start=False, stop=True), reads=[B_onesb, B_mkc], writes=[B_pr])
                    op("dve", lambda e: e.tensor_tensor(dst[:], pr[:, 0:32], pse[:, 0:32], ALU.add), reads=[B_pr, B_pse], writes=[B_dst])
                    op("dve", lambda e: e.tensor_tensor(tmp[:], dst[:], MK1[:, t, :], ALU.mult), reads=[B_dst, B_MK1], writes=[B_tmp])
                    op("dve", lambda e: e.tensor_reduce(dd[:, 0:1], tmp[:], AX.X, ALU.add), reads=[B_tmp], writes=[B_dd])
                    op("dve", lambda e: e.tensor_tensor(tmp[:], dst[:], MK2[:, t, :], ALU.mult), reads=[B_dst, B_MK2], writes=[B_tmp])
                    op("dve", lambda e: e.tensor_reduce(dd[:, 1:2], tmp[:], AX.X, ALU.add), reads=[B_tmp], writes=[B_dd])
                    op("dve", lambda e: e.tensor_copy(DEST1[:, t:t + 1], dd[:, 0:1]), reads=[B_dd], writes=[B_DEST1])
                    op("dve", lambda e: e.tensor_copy(DEST2[:, t:t + 1], dd[:, 1:2]), reads=[B_dd], writes=[B_DEST2])
                    op("dve", lambda e: e.tensor_tensor(mkc[:], mkc[:], mk12[:, t, :], ALU.add), reads=[B_mkc, B_mk12], writes=[B_mkc])
                    h_, B_h = ht[t % 2]; x_, B_x = xb[t % 2]
                    op("act", lambda e: e.copy(x_[:], h_[:]), reads=[B_h], writes=[B_x])
                    for DST, B_D in ((DEST1, B_DEST1), (DEST2, B_DEST2)):
                        dma("pool", None, None, reads=[B_x, B_D], writes=[B_XS],
                            fn=lambda e: e.indirect_dma_start(out=XS, out_offset=bass.IndirectOffsetOnAxis(ap=DST[:, t:t + 1], axis=0),
                                                              in_=x_[:], in_offset=None, bounds_check=REG_PROWS, oob_is_err=False))
                if "DBGI" in dbg and l == 0:
                    dma("sp", DBGI[:, 0:NT], DEST1[:], reads=[B_DEST1])
                    dma("sp", DBGI[:, NT:2 * NT], DEST2[:], reads=[B_DEST2])
                    dma("sp", DBGI[:, 2 * NT:2 * NT + 2 * NBLK], WIDX[:].rearrange("p a b -> p (a b)"), reads=[B_WIDX])
                    dma("sp", DBGF[:, 0:NT], W1[:], reads=[B_W1])
                    dma("sp", DBGF[:, NT:2 * NT], W2[:], reads=[B_W2])
                    dma("sp", DBGF[:, 2 * NT:2 * NT + 64], pse[:], reads=[B_pse])
                S_.barrier()

        def pass_D(l):
            with ExitStack() as es1:
                wg = [sbt(es1, [128, 4096], BF16, "wg") for _ in range(3)]
                wu = [sbt(es1, [128, 4096], BF16, "wu") for _ in range(3)]
                wd = [sbt(es1, [128, 4096], BF16, "wd") for _ in range(3)]
                xs = [sbt(es1, [128, 2, D], BF16, "xs") for _ in range(3)]
                xT = [sbt(es1, [128, 8, 256], BF16, "xT") for _ in range(2)]
                sgt, B_sgt = sbt(es1, [128, 1024], F32, "sgt")
                hTt, B_hTt = sbt(es1, [128, 4, 256], BF16, "hTt")
                yt = [sbt(es1, [128, D], F32, "yt") for _ in range(2)]

                def loadx(b):
                    dma("sp", xs[b % 3][0][:], XS[b * 256:(b + 1) * 256, :].rearrange("(s p) d -> p s d", p=128), reads=[B_XS], writes=[xs[b % 3][1]])

                def loadw(b):
                    for (wt, src) in ((wg, I["w_gate"]), (wu, I["w_up"]), (wd, I["w_down"])):
                        w_, B_w = wt[b % 3]
                        for c in range(2):
                            dma("pool", None, None, reads=[B_WIDX], writes=[B_w],
                                fn=lambda e: e.indirect_dma_start(out=w_[:, c * 2048:(c + 1) * 2048], out_offset=None, in_=src,
                                                                  in_offset=bass.IndirectOffsetOnAxis(ap=WIDX[:, b, c:c + 1], axis=0),
                                                                  bounds_check=REG_WROWS, oob_is_err=False))
                def xpose(b):
                    xs_, B_xs = xs[b % 3]; xT_, B_xT = xT[b % 2]
                    for sub in range(2):
                        pt, B_pt = next_pt()
                        xv = xs_[:, sub, :].rearrange("p (m j) -> p j m", j=8)
                        for j in range(8):
                            op("pe", lambda e: e.transpose(pt[:, j, :], xv[:, j, :], idb[:]), reads=[B_xs, B_idb], writes=[B_pt], sig=(j == 7))
                        op("dve", lambda e: e.tensor_copy(xT_[:, :, sub * 128:(sub + 1) * 128], pt[:]), reads=[B_pt], writes=[B_xT])
                loadx(0); loadw(0)
                if NBLK > 1:
                    loadx(1); loadw(1)
                xpose(0)
                for b in range(NBLK):
                    if b + 2 < NBLK:
                        loadx(b + 2)
                        loadw(b + 2)
                    xT_, B_xT = xT[b % 2]
                    wg_, B_wg = wg[b % 3]; wu_, B_wu = wu[b % 3]; wd_, B_wd = wd[b % 3]
                    pG, B_pG = next_pf(); pU, B_pU = next_pf()
                    for (pX, B_pX, w_, B_w) in ((pG, B_pG, wg_, B_wg), (pU, B_pU, wu_, B_wu)):
                        w4 = w_[:].rearrange("p (j m c) -> p j m c", j=8, c=4)
                        for cc in range(4):
                            for j in range(8):
                                op("pe", lambda e: e.matmul(pX[:, cc * 256:(cc + 1) * 256], w4[:, j, :, cc], xT_[:, j, :], start=(j == 0), stop=(j == 7)),
                                   reads=[B_w, B_xT], writes=[B_pX], sig=(j == 7 and cc == 3))
                    op("act", lambda e: e.activation(sgt[:], pG[:], AF.Sigmoid), reads=[B_pG], writes=[B_sgt])
                    op("dve", lambda e: e.tensor_tensor(sgt[:], pG[:], sgt[:], ALU.mult), reads=[B_pG, B_sgt], writes=[B_sgt])
                    op("dve", lambda e: e.tensor_tensor(hTt[:].rearrange("p a b -> p (a b)"), pU[:], sgt[:], ALU.mult), reads=[B_sgt, B_pU], writes=[B_hTt])
                    if b + 1 < NBLK:
                        xpose(b + 1)
                    for sub in range(2):
                        pY, B_pY = next_pf()
                        for g_ in range(2):
                            for cc in range(4):
                                op("pe", lambda e: e.matmul(pY[:, g_ * 512:(g_ + 1) * 512], hTt[:, cc, sub * 128:(sub + 1) * 128],
                                                            wd_[:, cc * 1024 + g_ * 512:cc * 1024 + (g_ + 1) * 512], start=(cc == 0), stop=(cc == 3)),
                                   reads=[B_hTt, B_wd], writes=[B_pY], sig=(cc == 3 and g_ == 1))
                        y_, B_y = yt[sub]
                        op("act", lambda e: e.copy(y_[:], pY[:]), reads=[B_pY], writes=[B_y])
                        r0 = b * 256 + sub * 128
                        dma("sp", YB[r0:r0 + 128, :], y_[:], reads=[B_y], writes=[B_YB])
                S_.barrier()

        def pass_E(l, last):
            with ExitStack() as es1:
                g3, B_g3 = load_bcast(es1, I["lnall"][2 + 6 * l + 4], D, "g3")
                b3, B_b3 = load_bcast(es1, I["lnall"][2 + 6 * l + 5], D, "b3")
                lnst = make_ln(es1)
                ht = [sbt(es1, [128, D], F32, "h2t") for _ in range(2)]
                y1 = [sbt(es1, [128, D], F32, "y1") for _ in range(2)]
                y2 = [sbt(es1, [128, D], F32, "y2") for _ in range(2)]
                acc, B_acc = sbt(es1, [128, D], F32, "acc")
                ot = [sbt(es1, [128, D], F32, "ot") for _ in range(2)]

                def loads(t):
                    dma("sp", ht[t % 2][0][:], H2[t * 128:(t + 1) * 128, :], reads=[B_H2[t]], writes=[ht[t % 2][1]])
                    for (yy, DST, B_D) in ((y1, DEST1, B_DEST1), (y2, DEST2, B_DEST2)):
                        y_, B_y = yy[t % 2]
                        dma("pool", None, None, reads=[B_YB, B_D], writes=[B_y],
                            fn=lambda e: e.indirect_dma_start(out=y_[:], out_offset=None, in_=YB,
                                                              in_offset=bass.IndirectOffsetOnAxis(ap=DST[:, t:t + 1], axis=0),
                                                              bounds_check=REG_PROWS, oob_is_err=False))
                loads(0)
                for t in range(NT):
                    if t + 1 < NT:
                        loads(t + 1)
                    h_, B_h = ht[t % 2]; y1_, B_y1 = y1[t % 2]; y2_, B_y2 = y2[t % 2]
                    op("act", lambda e: e.mul(acc[:], h_[:], ALPHA), reads=[B_h], writes=[B_acc])
                    op("dve", lambda e: e.scalar_tensor_tensor(acc[:], y1_[:], W1[:, t:t + 1], acc[:], ALU.mult, ALU.add), reads=[B_y1, B_W1, B_acc], writes=[B_acc])
                    op("dve", lambda e: e.scalar_tensor_tensor(acc[:], y2_[:], W2[:, t:t + 1], acc[:], ALU.mult, ALU.add), reads=[B_y2, B_W2, B_acc], writes=[B_acc])
                    o_, B_o = ot[t % 2]
                    layernorm(lnst, acc, B_acc, o_, B_o, g3[:], B_g3, b3[:], B_b3)
                    if last:
                        dma("sp", OUT[t * 128:(t + 1) * 128, :], o_[:], reads=[B_o])
                    else:
                        dma("sp", H[t * 128:(t + 1) * 128, :], o_[:], reads=[B_o], writes=[B_H[t]])
                S_.barrier()

        pass_P0()
        if stop != "P0":
            for l in range(L):
                pass_R(l)
                if stop == "R":
                    break
                pass_B1(l)
                if stop == "B1":
                    break
                pass_B2(l)
                if stop == "B2":
                    break
                pass_C0(l)
                if stop == "C0":
                    break
                pass_D(l)
                if stop == "D":
                    break
                pass_E(l, l == L - 1 or stop == "L0")
                if stop == "L0":
                    break
        S_.finish()
    return nc, consts_np


def prep_core_inputs(inp, seqs, consts_np):
    L = inp["w_in"].shape[0]
    S = inp["x"].shape[1]
    f = lambda a: np.ascontiguousarray(a)
    m = {}
    m["x"] = f(inp["x"][seqs].reshape(-1, D))
    m["mem"] = f(inp["mem"][seqs].reshape(-1, D))
    pos = inp["positions"][seqs].reshape(-1).astype(np.int32)
    m["pos"] = f(pos.reshape(-1, 128).T)
    m["consts"] = consts_np
    rows = [inp["ln_in_g"], inp["ln_in_b"]]
    for l in range(L):
        rows += [inp["ln1_g"][l], inp["ln1_b"][l], inp["ln2_g"][l], inp["ln2_b"][l], inp["ln3_g"][l], inp["ln3_b"][l]]
    m["lnall"] = f(np.stack(rows).astype(np.float32))
    m["w_in"] = inp["w_in"]
    idx = np.clip(np.arange(RB_EXT), 0, 512)
    m["rb_ext"] = f(inp["rel_bias"][:, :, idx])
    for k in ("w_proj_ret", "w_proj_att", "w_out", "w_q_mem", "w_kv_mem", "w_o_mem"):
        m[k] = inp[k]
    m["wr"] = f(np.concatenate([inp["w_group"], inp["w_route"]], axis=2))
    m["br"] = f(np.concatenate([inp["b_group"], inp["b_route"].reshape(L, 32)], axis=1))
    m["w_gate"] = inp["w_gate"].reshape(L * NE * 256, 2048)
    m["w_up"] = inp["w_up"].reshape(L * NE * 256, 2048)
    m["w_down"] = inp["w_down"].reshape(L * NE * 256, 2048)
    return m


def kernel(**inputs):
    inp = {k: np.asarray(v) for k, v in inputs.items()}
    nc, consts_np = build(2, 4096, L=2)
    in_maps = [prep_core_inputs(inp, [2 * c, 2 * c + 1], consts_np) for c in range(8)]
    res = run_bass_kernel_spmd(nc, in_maps, core_ids=list(range(8)))
    out = np.stack([np.asarray(res.results[c]["out"]) for c in range(8)])
    return np.ascontiguousarray(out.reshape(16, 4096, D).astype(np.float32))
```

```python
import numpy as np
import ml_dtypes
import concourse.bass as bass
import concourse.mybir as mybir
from concourse.bass_utils import run_bass_kernel_spmd
from contextlib import ExitStack

F32 = mybir.dt.float32; BF16 = mybir.dt.bfloat16; I32 = mybir.dt.int32; U32 = mybir.dt.uint32
AF = mybir.ActivationFunctionType; ALU = mybir.AluOpType; AX = mybir.AxisListType

NDS = 16
SUBSTOP = 99
SAME_ENG_SYNC = True


class Buf:
    __slots__ = ("name", "w", "r")

    def __init__(self, name):
        self.name = name; self.w = None; self.r = {}


class Sched:
    def __init__(self, nc, es):
        self.nc = nc
        self.e = dict(pe=nc.tensor, act=nc.scalar, dve=nc.vector, pool=nc.gpsimd, sp=nc.sync)
        self.sem = {k: es.enter_context(nc.semaphore("s_" + k)) for k in self.e}
        self.cnt = {k: 0 for k in self.e}
        self.known = {k: {} for k in self.e}
        self.dq = {}
        for q in ("sp", "pool", "act"):
            self.dq[q] = dict(sems=[es.enter_context(nc.semaphore("d_%s%d" % (q, i))) for i in range(NDS)], n=0)
        self.nwait = 0; self.ninst = 0

    def _sem(self, key):
        if isinstance(key, tuple):
            return self.dq[key[1]]["sems"][key[2]]
        return self.sem[key]

    def _wait(self, e, key, val):
        if self.known[e].get(key, 0) >= val:
            return
        self.e[e].wait_ge(self._sem(key), val)
        self.known[e][key] = val
        self.nwait += 1

    def _deps(self, e, reads, writes):
        need = {}
        for b in reads:
            if b.w is not None and need.get(b.w[0], 0) < b.w[1]:
                need[b.w[0]] = b.w[1]
        for b in writes:
            if b.w is not None and need.get(b.w[0], 0) < b.w[1]:
                need[b.w[0]] = b.w[1]
            for k, v in b.r.items():
                if need.get(k, 0) < v:
                    need[k] = v
        for k, v in need.items():
            if k == e and (e == "pe" or not SAME_ENG_SYNC):
                continue
            self._wait(e, k, v)

    def op(self, e, fn, reads=(), writes=(), sig=True):
        self._deps(e, reads, writes)
        inst = fn(self.e[e])
        seq = self.cnt[e] + 1
        if sig:
            inst.then_inc(self.sem[e], 1)
            self.cnt[e] = seq
        self.ninst += 1
        for b in reads:
            b.r[e] = seq
        for b in writes:
            b.w = (e, seq); b.r = {}
        return inst

    def dma(self, q, out_ap, in_ap, reads=(), writes=(), fn=None):
        d = self.dq[q]; i = d["n"] % NDS; rnd = d["n"] // NDS; d["n"] += 1
        key = ("d", q, i)
        if rnd > 0:
            self._wait(q, key, 16 * rnd)
        self._deps(q, reads, writes)
        if fn is None:
            inst = self.e[q].dma_start(out=out_ap, in_=in_ap)
        else:
            inst = fn(self.e[q])
        inst.then_inc(d["sems"][i], 16)
        val = 16 * (rnd + 1)
        self.ninst += 1
        for b in reads:
            b.r[key] = val
        for b in writes:
            b.w = (key, val); b.r = {}
        return inst

    def barrier(self):
        for q, d in self.dq.items():
            for i in range(NDS):
                uses = (d["n"] - i + NDS - 1) // NDS
                if uses > 0:
                    self._wait("sp", ("d", q, i), 16 * uses)
        for k in ("pe", "act", "dve", "pool"):
            if self.cnt[k] > 0:
                self._wait("sp", k, self.cnt[k])
        inst = self.e["sp"].nop()
        self.cnt["sp"] += 1
        inst.then_inc(self.sem["sp"], 1)
        for k in ("pe", "act", "dve", "pool"):
            self._wait(k, "sp", self.cnt["sp"])
            for k2 in ("pe", "act", "dve", "pool"):
                self.known[k][k2] = max(self.known[k].get(k2, 0), self.cnt[k2])
            for q, d in self.dq.items():
                for i in range(NDS):
                    uses = (d["n"] - i + NDS - 1) // NDS
                    if uses > 0:
                        self.known[k][("d", q, i)] = 16 * uses

    def finish(self):
        for q, d in self.dq.items():
            for i in range(min(NDS, d["n"])):
                last = (d["n"] - 1 - i) // NDS + 1 if d["n"] - 1 - i >= 0 else 0
                uses = (d["n"] - i + NDS - 1) // NDS
                if uses > 0:
                    self._wait("sp", ("d", q, i), 16 * uses)
        for k in ("pe", "act", "dve", "pool"):
            if self.cnt[k] > 0:
                self._wait("sp", k, self.cnt[k])


D = 1024; NE = 32; DE = 512; LN_EPS = 1e-5
ALPHA = float((2.0 * 2) ** 0.25)
NMEM = 256
RB_EXT = 1024


def host_consts(nblk):
    tabs = {}
    p = np.arange(128)
    g = 1.0 - np.exp2(-5.0 - np.arange(8, dtype=np.float64))
    lg = np.log(g)
    i = np.arange(128)[None, :]; j = np.arange(128)[:, None]
    dm = np.zeros((128, 8, 128))
    for h in range(8):
        m = np.exp(lg[h] * np.abs(i - j)) * ((j // 64) <= (i // 64))
        dm[:, h, :] = 0.125 * m
    tabs["DM"] = dm.reshape(128, 1024)
    qd = np.zeros((128, 4, 128))
    cd = np.zeros((128, 4))
    for c in range(4):
        for half in range(2):
            h = 2 * c + half
            qd[half * 64:(half + 1) * 64, c, :] = np.exp(lg[h] * (np.arange(128) + 1.0))[None, :]
            cd[half * 64:(half + 1) * 64, c] = np.exp(lg[h] * 128.0)
    tabs["QD"] = qd.reshape(128, 512)
    tabs["CD"] = cd
    kd = np.zeros((128, 8))
    for h in range(8):
        kd[:, h] = 0.125 * np.exp(lg[h] * (127.0 - np.arange(128)))
    tabs["KD"] = kd
    inv_freq = 1.0 / (10000.0 ** np.linspace(0.0, 1.0, 32, dtype=np.float32)).astype(np.float32)
    fq = (inv_freq.astype(np.float64) / (2 * np.pi))
    tabs["FQ"] = np.tile(np.concatenate([fq, fq])[None, :], (128, 1))
    tabs["IDENT"] = np.eye(128)
    tabs["ANTI"] = np.eye(128)[::-1].copy()
    tabs["USTRICT"] = (p[:, None] < p[None, :]).astype(np.float64)
    tabs["ONES"] = np.ones((128, 128))
    u32 = np.zeros((128, 32)); u32[:32, :] = (np.arange(32)[:, None] < np.arange(32)[None, :])
    tabs["U32"] = u32
    tabs["PIDX"] = p[:, None].astype(np.float64)
    ui32 = np.zeros((128, 32)); ui32[:32, :] = (np.arange(32)[:, None] <= np.arange(32)[None, :])
    tabs["UI32"] = ui32
    tabs["BLK"] = np.tile((256.0 * np.arange(nblk))[None, :], (128, 1))
    off = {}; cols = []; o = 0
    for k, v in tabs.items():
        v = np.asarray(v, dtype=np.float32)
        off[k] = (o, v.shape[1]); cols.append(v); o += v.shape[1]
    return np.ascontiguousarray(np.concatenate(cols, axis=1)), off


class Ctx:
    pass


def build(NSEQ, S, L=2, debug=None, stop=None):
    NTOK = NSEQ * S; NT = NTOK // 128; TS = S // 128
    NBLK = -(-(NTOK * 2 + NE * 255) // 256)
    PROWS = NBLK * 256
    consts_np, coff = host_consts(NBLK)
    NCONST = consts_np.shape[1]

    nc = bass.Bass("TRN2", target_bir_lowering=False)
    dbg = debug or ()

    def dram(name, shape, dt, kind="Internal"):
        if kind == "Internal" and name in dbg:
            kind = "ExternalOutput"
        return nc.dram_tensor(name, shape, dt, kind=kind).ap()

    I = {}
    I["x"] = dram("x", [NTOK, D], F32, "ExternalInput")
    I["mem"] = dram("mem", [NSEQ * NMEM, D], F32, "ExternalInput")
    I["pos"] = dram("pos", [128, NT], I32, "ExternalInput")
    I["consts"] = dram("consts", [128, NCONST], F32, "ExternalInput")
    I["lnall"] = dram("lnall", [2 + 6 * L, D], F32, "ExternalInput")
    I["w_in"] = dram("w_in", [L, D, 6656], F32, "ExternalInput")
    I["rb_ext"] = dram("rb_ext", [L, 8, RB_EXT], F32, "ExternalInput")
    I["w_proj_ret"] = dram("w_proj_ret", [L, D, D], F32, "ExternalInput")
    I["w_proj_att"] = dram("w_proj_att", [L, 512, D], F32, "ExternalInput")
    I["w_out"] = dram("w_out", [L, D, D], F32, "ExternalInput")
    I["w_q_mem"] = dram("w_q_mem", [L, D, D], F32, "ExternalInput")
    I["w_kv_mem"] = dram("w_kv_mem", [L, D, 2 * D], F32, "ExternalInput")
    I["w_o_mem"] = dram("w_o_mem", [L, D, D], F32, "ExternalInput")
    I["wr"] = dram("wr", [L, D, 36], F32, "ExternalInput")
    I["br"] = dram("br", [L, 36], F32, "ExternalInput")
    I["w_gate"] = dram("w_gate", [L * NE * 256, 2048], F32, "ExternalInput")
    I["w_up"] = dram("w_up", [L * NE * 256, 2048], F32, "ExternalInput")
    I["w_down"] = dram("w_down", [L * NE * 256, 2048], F32, "ExternalInput")
    OUT = dram("out", [NTOK, D], F32, "ExternalOutput")
    H = dram("H", [NTOK, D], F32); M1 = dram("M1", [NTOK, D], BF16)
    H1 = dram("H1", [NTOK, D], F32); H2 = dram("H2", [NTOK, D], F32)
    XS = dram("XS", [PROWS, D], BF16); YB = dram("YB", [PROWS, D], F32)
    B_H = [Buf("H%d" % t) for t in range(NT)]; B_M1 = [Buf("M1%d" % t) for t in range(NT)]
    B_H1 = [Buf("H1%d" % t) for t in range(NT)]; B_H2 = [Buf("H2%d" % t) for t in range(NT)]
    B_XS = Buf("XS"); B_YB = Buf("YB")
    DBGI = dram("DBGI", [128, 2 * NT + 2 * NBLK], I32) if "DBGI" in dbg else None
    DBGF = dram("DBGF", [128, 2 * NT + 64], F32) if "DBGI" in dbg else None

    with ExitStack() as es:
        S_ = Sched(nc, es)
        ctr = [0]

        def sbt(es_, shape, dt, name=None):
            ctr[0] += 1
            nm = "%s_%d" % (name or "t", ctr[0])
            return es_.enter_context(nc.sbuf_tensor(nm, shape, dt)), Buf(nm)

        def pst(es_, shape, dt, name=None):
            ctr[0] += 1
            nm = "%s_%d" % (name or "p", ctr[0])
            return es_.enter_context(nc.psum_tensor(nm, shape, dt)), Buf(nm)

        op = S_.op; dma = S_.dma

        cst, B_cst = sbt(es, [128, NCONST], F32, "cst")
        dma("sp", cst[:], I["consts"], writes=[B_cst])

        def ctab(name):
            o, w = coff[name]
            return cst[:, o:o + w]
        idb, B_idb = sbt(es, [128, 128], BF16, "idb")
        op("dve", lambda e: e.tensor_copy(idb[:], ctab("IDENT")), reads=[B_cst], writes=[B_idb])
        mhalf, B_mhalf = sbt(es, [128, 8], F32, "mhalf")
        op("pool", lambda e: e.memset(mhalf[:], -0.5), writes=[B_mhalf])
        posf, B_posf = sbt(es, [128, NT], F32, "posf")
        with ExitStack() as es0:
            posi, B_posi = sbt(es0, [128, NT], I32, "posi")
            dma("sp", posi[:], I["pos"], writes=[B_posi])
            op("dve", lambda e: e.tensor_copy(posf[:], posi[:]), reads=[B_posi], writes=[B_posf])
            S_.barrier()
        PT = [pst(es, [128, 8, 128], BF16, "PT") for _ in range(2)]
        PF = [pst(es, [128, 1024], F32, "PF") for _ in range(3)]
        rr = {"pt": 0, "pf": 0}

        def next_pt():
            rr["pt"] += 1
            return PT[rr["pt"] % 2]

        def next_pf():
            rr["pf"] += 1
            return PF[rr["pf"] % 3]

        MK1, B_MK1 = sbt(es, [128, NT, 32], BF16, "MK1")
        MK2, B_MK2 = sbt(es, [128, NT, 32], BF16, "MK2")
        W1, B_W1 = sbt(es, [128, NT], F32, "W1")
        W2, B_W2 = sbt(es, [128, NT], F32, "W2")
        DEST1, B_DEST1 = sbt(es, [128, NT], I32, "DEST1")
        DEST2, B_DEST2 = sbt(es, [128, NT], I32, "DEST2")
        WIDX, B_WIDX = sbt(es, [128, NBLK, 2], I32, "WIDX")
        REG_PROWS = nc.gpsimd.to_reg(PROWS - 1)
        REG_WROWS = nc.gpsimd.to_reg(L * NE * 256 - 1)
        onesb, B_onesb = sbt(es, [128, 128], BF16, "onesb")
        op("pool", lambda e: e.memset(onesb[:], 1.0), writes=[B_onesb])
        def load_w(es_, src_rows, K, ncols, name):
            w, B = sbt(es_, [128, K, ncols], BF16, name)
            for k in range(K):
                for c0 in range(0, ncols, 2048):
                    c1 = min(ncols, c0 + 2048)
                    dma("pool", w[:, k, c0:c1], src_rows[k * 128:(k + 1) * 128, c0:c1], writes=[B])
            return w, B

        def load_bcast(es_, row_ap, n, name):
            t, B = sbt(es_, [128, n], F32, name)
            dma("sp", t[:], row_ap.partition_broadcast(128), writes=[B])
            return t, B

        def transposes(src, B_src, n, dstT, B_dst, eng="dve", idt=None, B_id=None):
            pt, B_pt = next_pt()
            for k in range(n):
                op("pe", lambda e: e.transpose(pt[:, k, :], src[:, k * 128:(k + 1) * 128], idb[:]),
                   reads=[B_src, B_idb], writes=[B_pt], sig=(k == n - 1))
            if eng == "dve":
                op("dve", lambda e: e.tensor_copy(dstT[:, 0:n, :], pt[:, 0:n, :]), reads=[B_pt], writes=[B_dst])
            elif eng == "act":
                op("dve", lambda e: e.tensor_copy(dstT[:, 0:n, :], pt[:, 0:n, :]), reads=[B_pt], writes=[B_dst])
            return pt, B_pt

        def mm_tok(ps, B_ps, xT, B_xT, K, w, B_w, c0, ncols):
            ng = (ncols + 511) // 512
            for g_ in range(ng):
                a = g_ * 512; b = min(ncols, a + 512)
                for k in range(K):
                    op("pe", lambda e: e.matmul(ps[:, a:b], xT[:, k, :], w[:, k, c0 + a:c0 + b],
                                                start=(k == 0), stop=(k == K - 1)),
                       reads=[B_xT, B_w], writes=[B_ps], sig=(k == K - 1 and g_ == ng - 1))

        class LNState:
            pass

        def make_ln(es_):
            st = LNState()
            st.st, st.B_st = sbt(es_, [128, 2, 6], F32, "lnst")
            st.mv, st.B_mv = sbt(es_, [128, 2], F32, "lnmv")
            st.rs, st.B_rs = sbt(es_, [128, 1], F32, "lnrs")
            st.nm, st.B_nm = sbt(es_, [128, 1], F32, "lnnm")
            st.tmp, st.B_tmp = sbt(es_, [128, D], F32, "lntmp")
            return st

        def layernorm(st, src, B_src, dst, B_dst, g, B_g, b, B_b):
            for c in range(2):
                op("dve", lambda e: e.bn_stats(st.st[:, c, :], src[:, c * 512:(c + 1) * 512]), reads=[B_src], writes=[st.B_st])
            op("dve", lambda e: e.bn_aggr(st.mv[:], st.st[:].rearrange("p a b -> p (a b)")), reads=[st.B_st], writes=[st.B_mv])
            op("dve", lambda e: e.tensor_scalar(st.rs[:], st.mv[:, 1:2], LN_EPS, None, ALU.add), reads=[st.B_mv], writes=[st.B_rs])
            op("pool", lambda e: e.tensor_tensor(st.rs[:], st.rs[:], mhalf[:, 0:1], ALU.pow), reads=[st.B_rs, B_mhalf], writes=[st.B_rs])
            op("dve", lambda e: e.scalar_tensor_tensor(st.nm[:], st.mv[:, 0:1], -1.0, st.rs[:], ALU.mult, ALU.mult),
               reads=[st.B_mv, st.B_rs], writes=[st.B_nm])
            op("act", lambda e: e.activation(st.tmp[:], src[:], AF.Identity, bias=st.nm[:], scale=st.rs[:]),
               reads=[B_src, st.B_nm, st.B_rs], writes=[st.B_tmp])
            op("dve", lambda e: e.tensor_tensor(st.tmp[:], st.tmp[:], g, ALU.mult), reads=[st.B_tmp, B_g], writes=[st.B_tmp])
            op("pool", lambda e: e.tensor_tensor(dst[:], st.tmp[:], b, ALU.add), reads=[st.B_tmp, B_b], writes=[B_dst])


        def pass_P0():
            with ExitStack() as es1:
                lng, B_lng = load_bcast(es1, I["lnall"][0], D, "lng")
                lnb, B_lnb = load_bcast(es1, I["lnall"][1], D, "lnb")
                lnst = make_ln(es1)
                xt = [sbt(es1, [128, D], F32, "xt") for _ in range(2)]
                yt = [sbt(es1, [128, D], F32, "yt") for _ in range(2)]
                zt, B_zt = sbt(es1, [128, D], BF16, "zt")
                op("pool", lambda e: e.memset(zt[:], 0.0), writes=[B_zt])
                nz = PROWS // 128; zi = 0
                dma("sp", xt[0][0][:], I["x"][0:128, :], writes=[xt[0][1]])
                for t in range(NT):
                    while zi < nz * (t + 1) // NT:
                        dma("sp", XS[zi * 128:(zi + 1) * 128, :], zt[:], reads=[B_zt], writes=[B_XS])
                        zi += 1
                    if t + 1 < NT:
                        dma("sp", xt[(t + 1) % 2][0][:], I["x"][(t + 1) * 128:(t + 2) * 128, :], writes=[xt[(t + 1) % 2][1]])
                    x_, Bx = xt[t % 2]; y_, By = yt[t % 2]
                    layernorm(lnst, x_, Bx, y_, By, lng[:], B_lng, lnb[:], B_lnb)
                    dma("sp", H[t * 128:(t + 1) * 128, :], y_[:], reads=[By], writes=[B_H[t]])
                S_.barrier()

        def pass_R(l):
            with ExitStack() as es1:
                win = I["w_in"][l]
                wA, B_wA = load_w(es1, win[:, 0:3072], 8, 3072, "wA")
                wG, B_wG = load_w(es1, win[:, 4608:5632], 8, 1024, "wG")
                wP, B_wP = load_w(es1, I["w_proj_ret"][l], 8, 1024, "wP")
                cos2, B_cos2 = sbt(es1, [128, NT, 64], BF16, "cos2")
                sins, B_sins = sbt(es1, [128, NT, 64], BF16, "sins")
                GT = min(NT, 16)
                with ExitStack() as es2:
                    u, B_u = sbt(es2, [128, GT, 2, 64], F32, "u")
                    ki, B_ki = sbt(es2, [128, GT, 2, 64], I32, "ki")
                    kf, B_kf = sbt(es2, [128, GT, 2, 64], F32, "kf")
                    FQ = ctab("FQ")
                    uf = u[:].rearrange("p a b c -> p (a b c)"); kif = ki[:].rearrange("p a b c -> p (a b c)")
                    kff = kf[:].rearrange("p a b c -> p (a b c)")
                    for t0 in range(0, NT, GT):
                        for tt in range(GT):
                            op("dve", lambda e: e.tensor_scalar(u[:, tt, 0, :], FQ, posf[:, t0 + tt:t0 + tt + 1], None, ALU.mult),
                               reads=[B_cst, B_posf], writes=[B_u])
                        op("dve", lambda e: e.tensor_scalar(u[:, :, 1, :], u[:, :, 0, :], 0.25, None, ALU.add), reads=[B_u], writes=[B_u])
                        op("dve", lambda e: e.tensor_copy(kif, uf), reads=[B_u], writes=[B_ki])
                        op("dve", lambda e: e.tensor_copy(kff, kif), reads=[B_ki], writes=[B_kf])
                        op("dve", lambda e: e.tensor_tensor(uf, uf, kff, ALU.subtract), reads=[B_u, B_kf], writes=[B_u])
                        op("dve", lambda e: e.tensor_scalar(kff, uf, 0.5, None, ALU.is_gt), reads=[B_u], writes=[B_kf])
                        op("dve", lambda e: e.tensor_tensor(uf, uf, kff, ALU.subtract), reads=[B_u, B_kf], writes=[B_u])
                        op("dve", lambda e: e.tensor_scalar(kff, uf, -0.5, None, ALU.is_lt), reads=[B_u], writes=[B_kf])
                        op("dve", lambda e: e.tensor_tensor(uf, uf, kff, ALU.add), reads=[B_u, B_kf], writes=[B_u])
                        op("act", lambda e: e.activation(kff, uf, AF.Sin, scale=float(2 * np.pi)), reads=[B_u], writes=[B_kf])
                        op("dve", lambda e: e.tensor_copy(cos2[:, t0:t0 + GT, :], kf[:, :, 1, :]), reads=[B_kf], writes=[B_cos2])
                        op("dve", lambda e: e.tensor_scalar(sins[:, t0:t0 + GT, 0:32], kf[:, :, 0, 0:32], -1.0, None, ALU.mult), reads=[B_kf], writes=[B_sins])
                        op("dve", lambda e: e.tensor_copy(sins[:, t0:t0 + GT, 32:64], kf[:, :, 0, 32:64]), reads=[B_kf], writes=[B_sins])
                    S_.barrier()
                DM = ctab("DM").rearrange("p (h i) -> p h i", h=8)
                QD = ctab("QD").rearrange("p (c i) -> p c i", c=4)
                CDt = ctab("CD"); KD = ctab("KD")
                ht = [sbt(es1, [128, D], F32, "ht") for _ in range(2)]
                hb, B_hb = sbt(es1, [128, D], BF16, "hb")
                hT, B_hT = sbt(es1, [128, 8, 128], BF16, "hT")
                rA, B_rA = sbt(es1, [128, 16, 64], F32, "rA")
                rB, B_rB = sbt(es1, [128, 16, 64], F32, "rB")
                rots = [sbt(es1, [128, 16, 64], BF16, "rot") for _ in range(2)]
                kps = [sbt(es1, [128, 8, 64], BF16, "kp") for _ in range(2)]
                vrs = [sbt(es1, [128, D], BF16, "vr") for _ in range(2)]
                sgs = [sbt(es1, [128, D], BF16, "sg") for _ in range(2)]
                sgrs = [sbt(es1, [128, D], BF16, "sgr") for _ in range(2)]
                qkA, B_qkA = sbt(es1, [128, 8, 128], BF16, "qkA")
                kT = qkA[:, 4:8, :]; B_kT = B_qkA
                qTe, B_qTe = sbt(es1, [128, 4, 128], BF16, "qTe")
                qTo, B_qTo = sbt(es1, [128, 4, 128], BF16, "qTo")
                qpe, B_qpe = sbt(es1, [128, 4, 128], BF16, "qpe")
                qpo, B_qpo = sbt(es1, [128, 4, 128], BF16, "qpo")
                for (t_, B_) in ((qTe, B_qTe), (qTo, B_qTo), (qpe, B_qpe), (qpo, B_qpo)):
                    op("pool", lambda e: e.memset(t_[:], 0.0), writes=[B_])
                qsel = [(qTe, B_qTe), (qTo, B_qTo)]; qpsel = [(qpe, B_qpe), (qpo, B_qpo)]
                sTd, B_sTd = sbt(es1, [128, 8, 128], BF16, "sTd")
                state, B_state = sbt(es1, [128, 4, 128], F32, "state")
                stbf, B_stbf = sbt(es1, [128, 4, 128], BF16, "stbf")
                sq = rB[:].rearrange("p (a x) b -> p a (x b)", a=8); B_sq = B_rB
                dsb = rA[:].rearrange("p a b -> p (a b)"); B_dsb = B_rA
                st8, B_st8 = sbt(es1, [128, 8, 6], F32, "st8")
                mv8, B_mv8 = sbt(es1, [128, 8, 2], F32, "mv8")
                rs8, B_rs8 = sbt(es1, [128, 8], F32, "rs8")
                yr, B_yr = sbt(es1, [128, D], BF16, "yr")
                yrT, B_yrT = sbt(es1, [128, 8, 128], BF16, "yrT")
                m1 = [sbt(es1, [128, D], BF16, "m1")] * 2

                def loadh(t):
                    dma("sp", ht[t % 2][0][:], H[t * 128:(t + 1) * 128, :], reads=[B_H[t]], writes=[ht[t % 2][1]])

                def F1(t):
                    h_, B_h = ht[t % 2]
                    rot, B_rot = rots[t % 2]; kp, B_kp = kps[t % 2]; vr, B_vr = vrs[t % 2]
                    op("act", lambda e: e.copy(hb[:], h_[:]), reads=[B_h], writes=[B_hb])
                    transposes(hb, B_hb, 8, hT, B_hT, eng="dve")
                    pqk, B_pqk = next_pf()
                    mm_tok(pqk, B_pqk, hT, B_hT, 8, wA, B_wA, 0, 1024)
                    pv, B_pv = next_pf()
                    mm_tok(pv, B_pv, hT, B_hT, 8, wA, B_wA, 1024, 1024)
                    z3 = pqk[:].rearrange("p (a b) -> p a b", b=64)
                    cb = cos2[:, t, :].unsqueeze(1).broadcast_to([128, 16, 64])
                    op("dve", lambda e: e.tensor_tensor(rA[:], z3, cb, ALU.mult), reads=[B_pqk, B_cos2], writes=[B_rA])
                    nsb = sins[:, t, 0:32].unsqueeze(1).broadcast_to([128, 16, 32])
                    psb = sins[:, t, 32:64].unsqueeze(1).broadcast_to([128, 16, 32])
                    op("dve", lambda e: e.tensor_tensor(rB[:, :, 0:32], z3[:, :, 32:64], nsb, ALU.mult), reads=[B_pqk, B_sins], writes=[B_rB])
                    op("dve", lambda e: e.tensor_tensor(rB[:, :, 32:64], z3[:, :, 0:32], psb, ALU.mult), reads=[B_pqk, B_sins], writes=[B_rB])
                    op("pool", lambda e: e.tensor_tensor(rot[:], rA[:], rB[:], ALU.add), reads=[B_rA, B_rB], writes=[B_rot])
                    kdb = KD.unsqueeze(2).broadcast_to([128, 8, 64])
                    op("pool", lambda e: e.tensor_tensor(kp[:], rot[:, 8:16, :], kdb, ALU.mult), reads=[B_rot, B_cst], writes=[B_kp])
                    op("act", lambda e: e.copy(vr[:], pv[:]), reads=[B_pv], writes=[B_vr])

                def F2(t):
                    sg, B_sg = sgs[t % 2]; sgr, B_sgr = sgrs[t % 2]
                    pg, B_pg = next_pf()
                    mm_tok(pg, B_pg, hT, B_hT, 8, wA, B_wA, 2048, 1024)
                    op("act", lambda e: e.activation(sg[:], pg[:], AF.Sigmoid), reads=[B_pg], writes=[B_sg])
                    op("dve", lambda e: e.tensor_tensor(sg[:], pg[:], sg[:], ALU.mult), reads=[B_pg, B_sg], writes=[B_sg])
                    pgr, B_pgr = next_pf()
                    mm_tok(pgr, B_pgr, hT, B_hT, 8, wG, B_wG, 0, 1024)
                    op("act", lambda e: e.activation(sgr[:], pgr[:], AF.Sigmoid), reads=[B_pgr], writes=[B_sgr])

                def Bk1(t):
                    ts = t % TS
                    rot, B_rot = rots[t % 2]; kp, B_kp = kps[t % 2]; vr, B_vr = vrs[t % 2]
                    rotf = rot[:].rearrange("p a b -> p (a b)")
                    pt, B_pt = next_pt()
                    for k in range(8):
                        op("pe", lambda e: e.transpose(pt[:, k, :], rotf[:, k * 128:(k + 1) * 128], idb[:]),
                           reads=[B_rot, B_idb], writes=[B_pt], sig=(k == 7))
                    op("dve", lambda e: e.tensor_copy(qkA[:], pt[:]), reads=[B_pt], writes=[B_qkA])
                    op("act", lambda e: e.copy(qTe[0:64, :, :], qkA[0:64, 0:4, :]), reads=[B_qkA], writes=[B_qTe])
                    op("act", lambda e: e.copy(qTo[64:128, :, :], qkA[64:128, 0:4, :]), reads=[B_qkA], writes=[B_qTo])
                    op("pool", lambda e: e.tensor_tensor(qpe[0:64, :, :], qkA[0:64, 0:4, :], QD[0:64], ALU.mult), reads=[B_qkA, B_cst], writes=[B_qpe])
                    op("pool", lambda e: e.tensor_tensor(qpo[64:128, :, :], qkA[64:128, 0:4, :], QD[64:128], ALU.mult), reads=[B_qkA, B_cst], writes=[B_qpo])
                    psc, B_psc = next_pf()
                    psc3 = psc[:].rearrange("p (h i) -> p h i", h=8)
                    for h in range(8):
                        c = h // 2
                        qs_, B_qs = qsel[h % 2]
                        op("pe", lambda e: e.matmul(psc3[:, h, :], kT[:, c, :], qs_[:, c, :], start=True, stop=True),
                           reads=[B_kT, B_qs], writes=[B_psc], sig=(h == 7))
                    op("dve", lambda e: e.tensor_tensor(sTd[:], psc3, DM, ALU.mult), reads=[B_psc, B_cst], writes=[B_sTd])
                    po, B_po = next_pf()
                    po3 = po[:].rearrange("p (h i) -> p h i", h=8)
                    for h in range(8):
                        c = h // 2
                        op("pe", lambda e: e.matmul(po3[:, h, :], sTd[:, h, :], vr[:, h * 128:(h + 1) * 128], start=True, stop=(ts == 0)),
                           reads=[B_sTd, B_vr], writes=[B_po], sig=(ts == 0 and h == 7))
                        if ts > 0:
                            qp_, B_qp = qpsel[h % 2]
                            op("pe", lambda e: e.matmul(po3[:, h, :], qp_[:, c, :], stbf[:, c, :], start=False, stop=True),
                               reads=[B_qp, B_stbf], writes=[B_po], sig=(h == 7))
                    pds, B_pds = next_pf()
                    pds4 = pds[:].rearrange("p (a c e) -> p a c e", a=2, c=4)
                    kpf = kp[:].rearrange("p a b -> p (a b)")
                    for h in range(8):
                        c = h // 2; par = h % 2
                        op("pe", lambda e: e.matmul(pds4[:, par, c, :], kpf[:, c * 128:(c + 1) * 128], vr[:, h * 128:(h + 1) * 128], start=True, stop=True),
                           reads=[B_kp, B_vr], writes=[B_pds], sig=(h == 7))
                    op("act", lambda e: e.copy(dsb, pds[:]), reads=[B_pds], writes=[B_dsb])
                    dsb4 = dsb.rearrange("p (a c e) -> p a c e", a=2, c=4)
                    if ts == 0:
                        for par in range(2):
                            b0 = par * 64
                            op("dve", lambda e: e.tensor_copy(state[b0:b0 + 64, :, :], dsb4[b0:b0 + 64, par, :, :]), reads=[B_dsb], writes=[B_state])
                    else:
                        op("pool", lambda e: e.tensor_tensor(state[:], state[:], CDt.unsqueeze(2).broadcast_to([128, 4, 128]), ALU.mult),
                           reads=[B_state, B_cst], writes=[B_state])
                        for par in range(2):
                            b0 = par * 64
                            op("dve", lambda e: e.tensor_tensor(state[b0:b0 + 64, :, :], state[b0:b0 + 64, :, :], dsb4[b0:b0 + 64, par, :, :], ALU.add),
                               reads=[B_dsb, B_state], writes=[B_state])
                    op("act", lambda e: e.copy(stbf[:], state[:]), reads=[B_state], writes=[B_stbf])
                    op("act", lambda e: e.copy(sq.rearrange("p a b -> p (a b)"), po[:]), reads=[B_po], writes=[B_sq])
                    for h in range(8):
                        op("dve", lambda e: e.bn_stats(st8[:, h, :], sq[:, h, :]), reads=[B_sq], writes=[B_st8])
                    for h in range(8):
                        op("dve", lambda e: e.bn_aggr(mv8[:, h, :], st8[:, h, :]), reads=[B_st8], writes=[B_mv8])
                    op("dve", lambda e: e.tensor_scalar(rs8[:], mv8[:, :, 1], LN_EPS, None, ALU.add), reads=[B_mv8], writes=[B_rs8])
                    op("pool", lambda e: e.tensor_tensor(rs8[:], rs8[:], mhalf[:], ALU.pow), reads=[B_rs8, B_mhalf], writes=[B_rs8])
                    for h in range(8):
                        op("dve", lambda e: e.tensor_scalar(sq[:, h, :], sq[:, h, :], mv8[:, h, 0:1], rs8[:, h:h + 1], ALU.subtract, ALU.mult),
                           reads=[B_sq, B_mv8, B_rs8], writes=[B_sq])

                def Bk2(t):
                    sg, B_sg = sgs[t % 2]; sgr, B_sgr = sgrs[t % 2]
                    op("pool", lambda e: e.tensor_tensor(yr[:], sq.rearrange("p a b -> p (a b)"), sg[:], ALU.mult), reads=[B_sq, B_sg], writes=[B_yr])
                    transposes(yr, B_yr, 8, yrT, B_yrT, eng="dve")
                    pp, B_pp = next_pf()
                    mm_tok(pp, B_pp, yrT, B_yrT, 8, wP, B_wP, 0, 1024)
                    m1_, B_m1 = m1[t % 2]
                    op("dve", lambda e: e.tensor_tensor(m1_[:], pp[:], sgr[:], ALU.mult), reads=[B_pp, B_sgr], writes=[B_m1])
                    dma("sp", M1[t * 128:(t + 1) * 128, :], m1_[:], reads=[B_m1], writes=[B_M1[t]])

                loadh(0)
                if NT > 1:
                    loadh(1)
                F1(0); F2(0)
                for t in range(NT):
                    if t + 1 < NT:
                        F1(t + 1)
                    if t + 2 < NT:
                        loadh(t + 2)
                    Bk1(t)
                    if t + 1 < NT:
                        F2(t + 1)
                    Bk2(t)
                S_.barrier()

        def next_pf_ex(excl):
            while True:
                p = next_pf()
                if all(p[0] is not x[0] for x in excl):
                    return p

        def pass_B1(l):
            with ExitStack() as es1:
                win = I["w_in"][l]
                wB, B_wB = load_w(es1, win[:, 3072:4608], 8, 1536, "wB")
                wGa, B_wGa = load_w(es1, win[:, 5632:6656], 8, 1024, "wGa")
                wPa, B_wPa = load_w(es1, I["w_proj_att"][l], 4, 1024, "wPa")
                wO, B_wO = load_w(es1, I["w_out"][l], 8, 1024, "wO")
                g1, B_g1 = load_bcast(es1, I["lnall"][2 + 6 * l + 0], D, "g1")
                b1, B_b1 = load_bcast(es1, I["lnall"][2 + 6 * l + 1], D, "b1")
                lnst = make_ln(es1)
                btn, B_btn = sbt(es1, [128, 5, 8, 128], BF16, "btn")
                with ExitStack() as es2:
                    bt2, B_bt2 = sbt(es2, [128, 5, 8, 128], BF16, "bt2")
                    rbt = I["rb_ext"].tensor
                    for r in range(5):
                        src = bass.AP(tensor=rbt, offset=l * 8 * RB_EXT + 256 + (4 - r) * 128 - 127, ap=[[1, 128], [RB_EXT, 8], [1, 128]])
                        dma("pool", bt2[:, r, :, :], src, writes=[B_bt2])
                    op("pool", lambda e: e.memset(bt2[64:128, 0, :, 64:128], -30000.0), writes=[B_bt2])
                    op("pool", lambda e: e.memset(bt2[0:64, 4, :, 0:64], -30000.0), writes=[B_bt2])
                    antib, B_antib = sbt(es2, [128, 128], BF16, "antib")
                    op("dve", lambda e: e.tensor_copy(antib[:], ctab("ANTI")), reads=[B_cst], writes=[B_antib])
                    for r in range(5):
                        pf, B_pf = next_pf()
                        for hq in range(2):
                            op("pe", lambda e: e.matmul(pf[:, hq * 512:(hq + 1) * 512], antib[:], bt2[:, r, hq * 4:(hq + 1) * 4, :], start=True, stop=True),
                               reads=[B_antib, B_bt2], writes=[B_pf], sig=(hq == 1))
                        op("act", lambda e: e.copy(btn[:, r, :, :], pf[:].rearrange("p (h i) -> p h i", h=8)), reads=[B_pf], writes=[B_btn])
                    S_.barrier()
                ht = [sbt(es1, [128, D], F32, "ht") for _ in range(2)]
                m1 = [sbt(es1, [128, D], BF16, "m1") for _ in range(2)]
                hb, B_hb = sbt(es1, [128, D], BF16, "hb")
                hT, B_hT = sbt(es1, [128, 8, 128], BF16, "hT")
                qaA, B_qaA = sbt(es1, [128, 4, 128], BF16, "qaA")
                qkt, B_qkt = sbt(es1, [128, D], BF16, "qkt")
                sb5s = [sbt(es1, [128, 5, 128], F32, "sb5") for _ in range(2)]
                qasels = []
                for _ in range(2):
                    qaTe, B_qaTe = sbt(es1, [128, 4, 128], BF16, "qaTe")
                    qaTo, B_qaTo = sbt(es1, [128, 4, 128], BF16, "qaTo")
                    for (t_, B_) in ((qaTe, B_qaTe), (qaTo, B_qaTo)):
                        op("pool", lambda e: e.memset(t_[:], 0.0), writes=[B_])
                    qasels.append([(qaTe, B_qaTe), (qaTo, B_qaTo)])
                kring = [sbt(es1, [128, 4, 128], BF16, "kring") for _ in range(6)]
                vring = [sbt(es1, [128, 8, 65], BF16, "vring") for _ in range(6)]
                for v_, Bv in vring:
                    op("pool", lambda e: e.memset(v_[:, :, 64:65], 1.0), writes=[Bv])
                sgas = [sbt(es1, [128, D], BF16, "sga") for _ in range(2)]
                pTs = [sbt(es1, [128, 5, 128], BF16, "pT") for _ in range(2)]
                rden, B_rden = sbt(es1, [128, 8], F32, "rden")
                ya, B_ya = sbt(es1, [128, 8, 64], BF16, "ya")
                yaT, B_yaT = sbt(es1, [128, 4, 128], BF16, "yaT")
                t2, B_t2 = sbt(es1, [128, D], F32, "t2")
                posb = t2[:].rearrange("p (a b) -> p a b", a=8); B_posb = B_t2
                hs = t2; B_hs = B_t2
                mg, B_mg = sbt(es1, [128, D], BF16, "mg")
                mT, B_mT = sbt(es1, [128, 8, 128], BF16, "mT")
                rs_, B_rs = sbt(es1, [128, D], F32, "resid")
                h1 = [sbt(es1, [128, D], F32, "h1")] * 2

                def loads(t):
                    dma("sp", ht[t % 2][0][:], H[t * 128:(t + 1) * 128, :], reads=[B_H[t]], writes=[ht[t % 2][1]])
                    dma("sp", m1[t % 2][0][:], M1[t * 128:(t + 1) * 128, :], reads=[B_M1[t]], writes=[m1[t % 2][1]])
                def Fa(t):
                    ts = t % TS
                    h_, B_h = ht[t % 2]
                    vr_, B_vr = vring[ts % 6]
                    op("act", lambda e: e.copy(hb[:], h_[:]), reads=[B_h], writes=[B_hb])
                    transposes(hb, B_hb, 8, hT, B_hT, eng="dve")
                    pq, B_pq = next_pf()
                    mm_tok(pq, B_pq, hT, B_hT, 8, wB, B_wB, 0, 1024)
                    op("dve", lambda e: e.tensor_scalar(qkt[:, 0:512], pq[:, 0:512], 0.125, None, ALU.mult), reads=[B_pq], writes=[B_qkt])
                    op("act", lambda e: e.copy(qkt[:, 512:1024], pq[:, 512:1024]), reads=[B_pq], writes=[B_qkt])
                    pv, B_pv = next_pf()
                    mm_tok(pv, B_pv, hT, B_hT, 8, wB, B_wB, 1024, 512)
                    op("act", lambda e: e.copy(vr_[:, :, 0:64], pv[:, 0:512].rearrange("p (h d) -> p h d", h=8)), reads=[B_pv], writes=[B_vr])

                def Fb(t):
                    ts = t % TS
                    kr, B_kr = kring[ts % 6]
                    (qaTe, B_qaTe), (qaTo, B_qaTo) = qasels[t % 2]
                    sga, B_sga = sgas[t % 2]
                    ptq, B_ptq = next_pt()
                    for k in range(8):
                        op("pe", lambda e: e.transpose(ptq[:, k, :], qkt[:, k * 128:(k + 1) * 128], idb[:]),
                           reads=[B_qkt, B_idb], writes=[B_ptq], sig=(k == 7))
                    op("dve", lambda e: e.tensor_copy(qaA[:], ptq[:, 0:4, :]), reads=[B_ptq], writes=[B_qaA])
                    op("dve", lambda e: e.tensor_copy(kr[:], ptq[:, 4:8, :]), reads=[B_ptq], writes=[B_kr])
                    op("act", lambda e: e.copy(qaTe[0:64, :, :], qaA[0:64, :, :]), reads=[B_qaA], writes=[B_qaTe])
                    op("act", lambda e: e.copy(qaTo[64:128, :, :], qaA[64:128, :, :]), reads=[B_qaA], writes=[B_qaTo])
                    pg, B_pg = next_pf()
                    mm_tok(pg, B_pg, hT, B_hT, 8, wGa, B_wGa, 0, 1024)
                    op("act", lambda e: e.activation(sga[:], pg[:], AF.Sigmoid), reads=[B_pg], writes=[B_sga])

                loads(0)
                if NT > 1:
                    loads(1)
                Fa(0); Fb(0)
                for t in range(NT):
                    ts = t % TS
                    h_, B_h = ht[t % 2]; m1_, B_m1 = m1[t % 2]
                    qasel = qasels[t % 2]
                    sga, B_sga = sgas[t % 2]
                    po = next_pf()
                    po3 = po[0][:].rearrange("p (h i) -> p h i", h=8)
                    r_lo = max(0, 4 - ts)
                    def scores(h):
                        c = h // 2
                        ps, B_ps = next_pf_ex([po])
                        ps5 = ps[:, 0:640].rearrange("p (r i) -> p r i", r=5)
                        for r in range(r_lo, 5):
                            kt = ts - 4 + r
                            kk, B_kk = kring[kt % 6]
                            qa_, B_qa = qasel[h % 2]
                            op("pe", lambda e: e.matmul(ps5[:, r, :], kk[:, c, :], qa_[:, c, :], start=True, stop=True),
                               reads=[B_kk, B_qa], writes=[B_ps], sig=(r == 4))
                        return ps5, B_ps
                    pss = {0: scores(0)}
                    for h in range(8):
                        if h + 1 < 8:
                            pss[h + 1] = scores(h + 1)
                        ps5, B_ps = pss[h]
                        pT, B_pT = pTs[h % 2]
                        sb5, B_sb5 = sb5s[h % 2]
                        op("dve", lambda e: e.tensor_tensor(sb5[:, r_lo:5, :], ps5[:, r_lo:5, :], btn[:, r_lo:5, h, :], ALU.add),
                           reads=[B_ps, B_btn], writes=[B_sb5])
                        op("act", lambda e: e.activation(pT[:, r_lo:5, :], sb5[:, r_lo:5, :], AF.Exp), reads=[B_sb5], writes=[B_pT])
                        for r in range(r_lo, 5):
                            kt = ts - 4 + r
                            vv, B_vv = vring[kt % 6]
                            op("pe", lambda e: e.matmul(po3[:, h, 0:65], pT[:, r, :], vv[:, h, :], start=(r == r_lo), stop=(r == 4)),
                               reads=[B_pT, B_vv], writes=[po[1]], sig=(r == 4))
                    op("act", lambda e: e.copy(t2[:], po[0][:]), reads=[po[1]], writes=[B_posb])
                    op("dve", lambda e: e.reciprocal(rden[:], posb[:, :, 64]), reads=[B_posb], writes=[B_rden])
                    for h in range(8):
                        op("dve", lambda e: e.tensor_scalar(ya[:, h, :], posb[:, h, 0:64], rden[:, h:h + 1], None, ALU.mult),
                           reads=[B_posb, B_rden], writes=[B_ya])
                    if t + 1 < NT:
                        Fa(t + 1)
                    transposes(ya[:].rearrange("p a b -> p (a b)"), B_ya, 4, yaT, B_yaT, eng="act")
                    pp, B_pp = next_pf()
                    mm_tok(pp, B_pp, yaT, B_yaT, 4, wPa, B_wPa, 0, 1024)
                    op("dve", lambda e: e.tensor_tensor(t2[:], pp[:], sga[:], ALU.mult), reads=[B_pp, B_sga], writes=[B_t2])
                    op("pool", lambda e: e.tensor_tensor(mg[:], t2[:], m1_[:], ALU.add), reads=[B_t2, B_m1], writes=[B_mg])
                    if t + 1 < NT:
                        Fb(t + 1)
                    transposes(mg, B_mg, 8, mT, B_mT, eng="dve")
                    px, B_px = next_pf()
                    mm_tok(px, B_px, mT, B_mT, 8, wO, B_wO, 0, 1024)
                    op("act", lambda e: e.mul(hs[:], h_[:], ALPHA), reads=[B_h], writes=[B_hs])
                    op("dve", lambda e: e.tensor_tensor(rs_[:], px[:], hs[:], ALU.add), reads=[B_hs, B_px], writes=[B_rs])
                    if t + 2 < NT:
                        loads(t + 2)
                    h1_, B_h1 = h1[t % 2]
                    layernorm(lnst, rs_, B_rs, h1_, B_h1, g1[:], B_g1, b1[:], B_b1)
                    dma("sp", H1[t * 128:(t + 1) * 128, :], h1_[:], reads=[B_h1], writes=[B_H1[t]])
                S_.barrier()

        def pass_B2(l):
            with ExitStack() as es1:
                wQ, B_wQ = load_w(es1, I["w_q_mem"][l], 8, 1024, "wQ")
                wOm, B_wOm = load_w(es1, I["w_o_mem"][l], 8, 1024, "wOm")
                g2, B_g2 = load_bcast(es1, I["lnall"][2 + 6 * l + 2], D, "g2")
                b2, B_b2 = load_bcast(es1, I["lnall"][2 + 6 * l + 3], D, "b2")
                brt, B_brt = load_bcast(es1, I["br"][l], 36, "brt")
                wrt, B_wrt = sbt(es1, [128, 8, 36], F32, "wrt")
                dma("sp", wrt[:], I["wr"][l].rearrange("(k p) n -> p k n", p=128), writes=[B_wrt])
                lnst = make_ln(es1)
                kTs = [sbt(es1, [128, 8, NMEM], BF16, "kTs") for _ in range(NSEQ)]
                Vs = [sbt(es1, [128, 2, D], BF16, "Vs") for _ in range(NSEQ)]
                with ExitStack() as es2:
                    wkv, B_wkv = load_w(es2, I["w_kv_mem"][l], 8, 2048, "wkv")
                    memb, B_memb = sbt(es2, [128, 2, D], BF16, "memb")
                    memT, B_memT = sbt(es2, [128, 8, NMEM], BF16, "memT")
                    for s_ in range(NSEQ):
                        for mc in range(2):
                            dma("pool", memb[:, mc, :], I["mem"][s_ * NMEM + mc * 128:s_ * NMEM + (mc + 1) * 128, :], writes=[B_memb])
                        for mc in range(2):
                            pt, B_pt = next_pt()
                            for k in range(8):
                                op("pe", lambda e: e.transpose(pt[:, k, :], memb[:, mc, k * 128:(k + 1) * 128], idb[:]),
                                   reads=[B_memb, B_idb], writes=[B_pt], sig=(k == 7))
                            op("dve", lambda e: e.tensor_copy(memT[:, :, mc * 128:(mc + 1) * 128], pt[:]), reads=[B_pt], writes=[B_memT])
                        kT_, B_kT = kTs[s_]; V_, B_V = Vs[s_]
                        for half in range(2):
                            pf, B_pf = next_pf()
                            for c4 in range(4):
                                c = half * 4 + c4
                                for k in range(8):
                                    op("pe", lambda e: e.matmul(pf[:, c4 * 256:(c4 + 1) * 256], wkv[:, k, c * 128:(c + 1) * 128], memT[:, k, :],
                                                                start=(k == 0), stop=(k == 7)),
                                       reads=[B_wkv, B_memT], writes=[B_pf], sig=(k == 7 and c4 == 3))
                            op("dve", lambda e: e.tensor_scalar(kT_[:, half * 4:(half + 1) * 4, :], pf[:].rearrange("p (c m) -> p c m", c=4), 1.0 / 16, None, ALU.mult),
                               reads=[B_pf], writes=[B_kT])
                        for mc in range(2):
                            pf, B_pf = next_pf()
                            for g_ in range(2):
                                for k in range(8):
                                    op("pe", lambda e: e.matmul(pf[:, g_ * 512:(g_ + 1) * 512], memT[:, k, mc * 128:(mc + 1) * 128],
                                                                wkv[:, k, 1024 + g_ * 512:1024 + (g_ + 1) * 512], start=(k == 0), stop=(k == 7)),
                                       reads=[B_wkv, B_memT], writes=[B_pf], sig=(k == 7 and g_ == 1))
                            op("act", lambda e: e.copy(V_[:, mc, :], pf[:]), reads=[B_pf], writes=[B_V])
                    S_.barrier()
                ht = [sbt(es1, [128, D], F32, "h1t") for _ in range(2)]
                hb, B_hb = sbt(es1, [128, D], BF16, "hb")
                hT, B_hT = sbt(es1, [128, 8, 128], BF16, "hT")
                qmT, B_qmT = sbt(es1, [128, 8, 128], BF16, "qmT")
                qtok, B_qtok = sbt(es1, [128, D], BF16, "qtok")
                pm, B_pm = sbt(es1, [128, 8, 128], BF16, "pm")
                rden, B_rden = sbt(es1, [128, 4], F32, "rden")
                ob, B_ob = sbt(es1, [128, 4, 256], BF16, "ob")
                osb, B_osb = sbt(es1, [128, D], F32, "osb")
                hs, B_hs = sbt(es1, [128, D], F32, "hs")
                oT, B_oT = sbt(es1, [128, 8, 128], BF16, "oT")
                rs_, B_rs = sbt(es1, [128, D], F32, "resid")
                h2 = [sbt(es1, [128, D], F32, "h2") for _ in range(2)]
                h2T, B_h2T = sbt(es1, [128, 8, 128], F32, "h2T")
                sm, B_sm = sbt(es1, [128, 256], F32, "rsm")
                idf = ctab("IDENT")
                qmTs = [(qmT, B_qmT), sbt(es1, [128, 8, 128], BF16, "qmT2")]

                def loadh1(t):
                    dma("sp", ht[t % 2][0][:], H1[t * 128:(t + 1) * 128, :], reads=[B_H1[t]], writes=[ht[t % 2][1]])

                def Fa(t):
                    h_, B_h = ht[t % 2]
                    op("act", lambda e: e.copy(hb[:], h_[:]), reads=[B_h], writes=[B_hb])
                    transposes(hb, B_hb, 8, hT, B_hT, eng="dve")
                    pq, B_pq = next_pf()
                    mm_tok(pq, B_pq, hT, B_hT, 8, wQ, B_wQ, 0, 1024)
                    op("act", lambda e: e.copy(qtok[:], pq[:]), reads=[B_pq], writes=[B_qtok])

                def Fb(t):
                    q_, B_q = qmTs[t % 2]
                    transposes(qtok, B_qtok, 8, q_, B_q, eng="dve")

                loadh1(0)
                if NT > 1:
                    loadh1(1)
                Fa(0); Fb(0)
                for t in range(NT):
                    s_ = t // TS
                    kT_, B_kT = kTs[s_]; V_, B_V = Vs[s_]
                    h_, B_h = ht[t % 2]
                    qmT, B_qmT = qmTs[t % 2]
                    ps, B_ps = next_pf()
                    ps3 = ps[:].rearrange("p (a i) -> p a i", a=8)
                    for hh in range(4):
                        for mc in range(2):
                            for c2 in range(2):
                                c = hh * 2 + c2
                                op("pe", lambda e: e.matmul(ps3[:, hh * 2 + mc, :], kT_[:, c, mc * 128:(mc + 1) * 128], qmT[:, c, :], start=(c2 == 0), stop=(c2 == 1)),
                                   reads=[B_kT, B_qmT], writes=[B_ps], sig=(hh == 3 and mc == 1 and c2 == 1))
                    op("act", lambda e: e.activation(pm[:], ps3, AF.Exp), reads=[B_ps], writes=[B_pm])
                    po, B_po = next_pf()
                    pd, B_pd = next_pf()
                    for hh in range(4):
                        for mc in range(2):
                            op("pe", lambda e: e.matmul(po[:, hh * 256:(hh + 1) * 256], pm[:, hh * 2 + mc, :], V_[:, mc, hh * 256:(hh + 1) * 256], start=(mc == 0), stop=(mc == 1)),
                               reads=[B_pm, B_V], writes=[B_po], sig=(hh == 3 and mc == 1))
                    for hh in range(4):
                        for mc in range(2):
                            op("pe", lambda e: e.matmul(pd[:, hh:hh + 1], pm[:, hh * 2 + mc, :], onesb[:, 0:1], start=(mc == 0), stop=(mc == 1)),
                               reads=[B_pm, B_onesb], writes=[B_pd], sig=(hh == 3 and mc == 1))
                    op("act", lambda e: e.copy(osb[:], po[:]), reads=[B_po], writes=[B_osb])
                    op("act", lambda e: e.copy(rden[:], pd[:, 0:4]), reads=[B_pd], writes=[B_rden])
                    op("dve", lambda e: e.reciprocal(rden[:], rden[:]), reads=[B_rden], writes=[B_rden])
                    for hh in range(4):
                        op("dve", lambda e: e.tensor_scalar(ob[:, hh, :], osb[:, hh * 256:(hh + 1) * 256], rden[:, hh:hh + 1], None, ALU.mult),
                           reads=[B_osb, B_rden], writes=[B_ob])
                    if t + 1 < NT:
                        Fa(t + 1)
                    transposes(ob[:].rearrange("p a b -> p (a b)"), B_ob, 8, oT, B_oT, eng="act")
                    px, B_px = next_pf()
                    mm_tok(px, B_px, oT, B_oT, 8, wOm, B_wOm, 0, 1024)
                    op("act", lambda e: e.mul(hs[:], h_[:], ALPHA), reads=[B_h], writes=[B_hs])
                    op("dve", lambda e: e.tensor_tensor(rs_[:], px[:], hs[:], ALU.add), reads=[B_hs, B_px], writes=[B_rs])
                    h2_, B_h2 = h2[t % 2]
                    layernorm(lnst, rs_, B_rs, h2_, B_h2, g2[:], B_g2, b2[:], B_b2)
                    dma("sp", H2[t * 128:(t + 1) * 128, :], h2_[:], reads=[B_h2], writes=[B_H2[t]])
                    if t + 2 < NT:
                        loadh1(t + 2)
                    if t + 1 < NT:
                        Fb(t + 1)
                    pt_, B_ptf = next_pf()
                    pt3 = pt_[:].rearrange("p (c i) -> p c i", c=8)
                    for k in range(8):
                        op("pe", lambda e: e.transpose(pt3[:, k, :], h2_[:, k * 128:(k + 1) * 128], idf), reads=[B_h2, B_cst], writes=[B_ptf], sig=(k == 7))
                    op("act", lambda e: e.copy(h2T[:], pt3), reads=[B_ptf], writes=[B_h2T])
                    pl, B_pl = next_pf()
                    for k in range(8):
                        op("pe", lambda e: e.matmul(pl[:, 0:36], h2T[:, k, :], wrt[:, k, :], start=(k == 0), stop=(k == 7)),
                           reads=[B_h2T, B_wrt], writes=[B_pl], sig=(k == 7))
                    lg = sm[:, 0:36]; gmax = sm[:, 36:37]; gm = sm[:, 40:44]; ngmax = sm[:, 37:38]; gexp = sm[:, 44:48]
                    gsum = sm[:, 38:39]; gw = sm[:, 39:40]; pen = sm[:, 48:52]; em = sm[:, 64:96]; mk1 = sm[:, 96:128]
                    em2 = sm[:, 128:160]; mk2 = sm[:, 160:192]; m1v = sm[:, 52:53]; m2v = sm[:, 53:54]; dd = sm[:, 54:55]
                    e2 = sm[:, 55:56]; den = sm[:, 56:57]; w1 = sm[:, 57:58]; w2 = sm[:, 58:59]
                    R_ = [B_sm]
                    op("dve", lambda e: e.tensor_tensor(lg, pl[:, 0:36], brt[:], ALU.add), reads=[B_pl, B_brt], writes=R_)
                    op("dve", lambda e: e.tensor_reduce(gmax, lg[:, 0:4], AX.X, ALU.max), reads=R_, writes=R_)
                    op("dve", lambda e: e.tensor_scalar(gm, lg[:, 0:4], gmax, None, ALU.is_equal), reads=R_, writes=R_)
                    op("dve", lambda e: e.tensor_scalar(ngmax, gmax, -1.0, None, ALU.mult), reads=R_, writes=R_)
                    op("act", lambda e: e.activation(gexp, lg[:, 0:4], AF.Exp, bias=ngmax), reads=R_, writes=R_)
                    op("dve", lambda e: e.tensor_reduce(gsum, gexp, AX.X, ALU.add), reads=R_, writes=R_)
                    op("dve", lambda e: e.reciprocal(gw, gsum), reads=R_, writes=R_)
                    op("dve", lambda e: e.tensor_scalar(pen, gm, 1.0, 1e30, ALU.subtract, ALU.mult), reads=R_, writes=R_)
                    op("dve", lambda e: e.tensor_tensor(em.rearrange("p (g x) -> p g x", g=4), lg[:, 4:36].rearrange("p (g x) -> p g x", g=4),
                                                        pen.unsqueeze(2).broadcast_to([128, 4, 8]), ALU.add), reads=R_, writes=R_)
                    op("dve", lambda e: e.tensor_reduce(m1v, em, AX.X, ALU.max), reads=R_, writes=R_)
                    op("dve", lambda e: e.tensor_scalar(mk1, em, m1v, None, ALU.is_equal), reads=R_, writes=R_)
                    op("dve", lambda e: e.scalar_tensor_tensor(em2, mk1, -1e30, em, ALU.mult, ALU.add), reads=R_, writes=R_)
                    op("dve", lambda e: e.tensor_reduce(m2v, em2, AX.X, ALU.max), reads=R_, writes=R_)
                    op("dve", lambda e: e.tensor_scalar(mk2, em2, m2v, None, ALU.is_equal), reads=R_, writes=R_)
                    op("dve", lambda e: e.tensor_tensor(dd, m2v, m1v, ALU.subtract), reads=R_, writes=R_)
                    op("act", lambda e: e.activation(e2, dd, AF.Exp), reads=R_, writes=R_)
                    op("dve", lambda e: e.tensor_scalar(den, e2, 1.0, None, ALU.add), reads=R_, writes=R_)
                    op("dve", lambda e: e.reciprocal(w1, den), reads=R_, writes=R_)
                    op("dve", lambda e: e.tensor_tensor(w2, e2, w1, ALU.mult), reads=R_, writes=R_)
                    op("dve", lambda e: e.tensor_tensor(W1[:, t:t + 1], w1, gw, ALU.mult), reads=R_, writes=[B_W1])
                    op("dve", lambda e: e.tensor_tensor(W2[:, t:t + 1], w2, gw, ALU.mult), reads=R_, writes=[B_W2])
                    op("dve", lambda e: e.tensor_copy(MK1[:, t, :], mk1), reads=R_, writes=[B_MK1])
                    op("dve", lambda e: e.tensor_copy(MK2[:, t, :], mk2), reads=R_, writes=[B_MK2])
                S_.barrier()

        def pass_C0(l):
            with ExitStack() as es1:
                mk12, B_mk12 = sbt(es1, [128, NT, 32], BF16, "mk12")
                op("dve", lambda e: e.tensor_tensor(mk12[:], MK1[:], MK2[:], ALU.add), reads=[B_MK1, B_MK2], writes=[B_mk12])
                ustb, B_ustb = sbt(es1, [128, 128], BF16, "ustb")
                op("dve", lambda e: e.tensor_copy(ustb[:], ctab("USTRICT")), reads=[B_cst], writes=[B_ustb])
                pc, B_pc = next_pf()
                for t in range(NT):
                    op("pe", lambda e: e.matmul(pc[:, 0:32], onesb[:], mk12[:, t, :], start=(t == 0), stop=(t == NT - 1)),
                       reads=[B_mk12, B_onesb], writes=[B_pc], sig=(t == NT - 1))
                cs, B_cs = sbt(es1, [128, 32], F32, "cs")
                ci, B_ci = sbt(es1, [128, 3, 32], I32, "ci")
                op("act", lambda e: e.copy(cs[:], pc[:, 0:32]), reads=[B_pc], writes=[B_cs])
                op("dve", lambda e: e.tensor_scalar(cs[:], cs[:], 255.0, None, ALU.add), reads=[B_cs], writes=[B_cs])
                op("dve", lambda e: e.tensor_copy(ci[:, 0, :], cs[:]), reads=[B_cs], writes=[B_ci])
                op("dve", lambda e: e.tensor_scalar(ci[:, 1, :], ci[:, 0, :], 8, None, ALU.arith_shift_right), reads=[B_ci], writes=[B_ci])
                op("dve", lambda e: e.tensor_scalar(ci[:, 2, :], ci[:, 1, :], 8, None, ALU.logical_shift_left), reads=[B_ci], writes=[B_ci])
                pse, B_pse = sbt(es1, [128, 64], F32, "pse")
                cum = [sbt(es1, [128, 32], F32, "cum") for _ in range(2)]
                op("dve", lambda e: e.tensor_copy(cs[:], ci[:, 2, :]), reads=[B_ci], writes=[B_cs])
                op("dve", lambda e: e.tensor_copy(cum[0][0][:], cs[:]), reads=[B_cs], writes=[cum[0][1]])
                cur = 0
                for sh in (1, 2, 4, 8, 16):
                    a_, B_a = cum[cur]; b_, B_b = cum[1 - cur]
                    op("dve", lambda e: e.tensor_copy(b_[:, 0:sh], a_[:, 0:sh]), reads=[B_a], writes=[B_b])
                    op("dve", lambda e: e.tensor_tensor(b_[:, sh:32], a_[:, sh:32], a_[:, 0:32 - sh], ALU.add), reads=[B_a], writes=[B_b])
                    cur = 1 - cur
                inc_, B_inc = cum[cur]
                op("dve", lambda e: e.tensor_copy(pse[:, 32:64], inc_[:]), reads=[B_inc], writes=[B_pse])
                op("dve", lambda e: e.tensor_tensor(pse[:, 0:32], inc_[:], cs[:], ALU.subtract), reads=[B_inc, B_cs], writes=[B_pse])
                cmp_, B_cmp = sbt(es1, [128, NBLK, 32], F32, "cmp")
                bet, B_bet = sbt(es1, [128, NBLK], F32, "bet")
                BLK = ctab("BLK")
                op("dve", lambda e: e.tensor_tensor(cmp_[:], pse[:, 32:64].unsqueeze(1).broadcast_to([128, NBLK, 32]),
                                                    BLK.unsqueeze(2).broadcast_to([128, NBLK, 32]), ALU.is_le), reads=[B_pse, B_cst], writes=[B_cmp])
                op("dve", lambda e: e.tensor_reduce(bet[:], cmp_[:], AX.X, ALU.add), reads=[B_cmp], writes=[B_bet])
                flg, B_flg = sbt(es1, [128, NBLK], F32, "flg")
                op("dve", lambda e: e.tensor_scalar(flg[:], bet[:], 31.5, 1.0e6, ALU.is_gt, ALU.mult), reads=[B_bet], writes=[B_flg])
                op("dve", lambda e: e.tensor_scalar(bet[:], bet[:], 31.0, 256.0, ALU.min, ALU.mult), reads=[B_bet], writes=[B_bet])
                op("dve", lambda e: e.tensor_tensor(bet[:], bet[:], flg[:], ALU.add), reads=[B_bet, B_flg], writes=[B_bet])
                pb2, B_pb2 = sbt(es1, [128, 2], F32, "pb2")
                op("dve", lambda e: e.tensor_scalar(pb2[:, 0:1], ctab("PIDX"), 2.0, float(l * NE * 256), ALU.mult, ALU.add), reads=[B_cst], writes=[B_pb2])
                op("dve", lambda e: e.tensor_scalar(pb2[:, 1:2], pb2[:, 0:1], 1.0, None, ALU.add), reads=[B_pb2], writes=[B_pb2])
                wf, B_wf = sbt(es1, [128, NBLK, 2], F32, "wf")
                for c in range(2):
                    op("dve", lambda e: e.tensor_scalar(wf[:, :, c], bet[:], pb2[:, c:c + 1], None, ALU.add), reads=[B_bet, B_pb2], writes=[B_wf])
                op("dve", lambda e: e.tensor_copy(WIDX[:], wf[:]), reads=[B_wf], writes=[B_WIDX])
                mkc, B_mkc = sbt(es1, [128, 32], BF16, "mkc")
                op("pool", lambda e: e.memset(mkc[:], 0.0), writes=[B_mkc])
                dst, B_dst = sbt(es1, [128, 32], F32, "dst")
                tmp, B_tmp = sbt(es1, [128, 32], F32, "dtmp")
                dd, B_dd = sbt(es1, [128, 2], F32, "dd")
                ht = [sbt(es1, [128, D], F32, "h2t") for _ in range(2)]
                xb = [sbt(es1, [128, D], BF16, "xb") for _ in range(2)]
                dma("sp", ht[0][0][:], H2[0:128, :], reads=[B_H2[0]], writes=[ht[0][1]])
                for t in range(NT):
                    if t + 1 < NT:
                        dma("sp", ht[(t + 1) % 2][0][:], H2[(t + 1) * 128:(t + 2) * 128, :], reads=[B_H2[t + 1]], writes=[ht[(t + 1) % 2][1]])
                    pr, B_pr = next_pf()
                    op("pe", lambda e: e.matmul(pr[:, 0:32], ustb[:], mk12[:, t, :], start=True, stop=(t == 0)), reads=[B_ustb, B_mk12], writes=[B_pr], sig=(t == 0))
                    if t > 0:
                        op("pe", lambda e: e.matmul(pr[:, 0:32], onesb[:], mkc[:], start=False, stop=True), reads=[B_onesb, B_mkc], writes=[B_pr])
                    op("dve", lambda e: e.tensor_tensor(dst[:], pr[:, 0:32], pse[:, 0:32], ALU.add), reads=[B_pr, B_pse], writes=[B_dst])
                    op("dve", lambda e: e.tensor_tensor(tmp[:], dst[:], MK1[:, t, :], ALU.mult), reads=[B_dst, B_MK1], writes=[B_tmp])
                    op("dve", lambda e: e.tensor_reduce(dd[:, 0:1], tmp[:], AX.X, ALU.add), reads=[B_tmp], writes=[B_dd])
                    op("dve", lambda e: e.tensor_tensor(tmp[:], dst[:], MK2[:, t, :], ALU.mult), reads=[B_dst, B_MK2], writes=[B_tmp])
                    op("dve", lambda e: e.tensor_reduce(dd[:, 1:2], tmp[:], AX.X, ALU.add), reads=[B_tmp], writes=[B_dd])
                    op("dve", lambda e: e.tensor_copy(DEST1[:, t:t + 1], dd[:, 0:1]), reads=[B_dd], writes=[B_DEST1])
                    op("dve", lambda e: e.tensor_copy(DEST2[:, t:t + 1], dd[:, 1:2]), reads=[B_dd], writes=[B_DEST2])
                    op("dve", lambda e: e.tensor_tensor(mkc[:], mkc[:], mk12[:, t, :], ALU.add), reads=[B_mkc, B_mk12], writes=[B_mkc])
                    h_, B_h = ht[t % 2]; x_, B_x = xb[t % 2]
                    op("act", lambda e: e.copy(x_[:], h_[:]), reads=[B_h], writes=[B_x])
                    for DST, B_D in ((DEST1, B_DEST1), (DEST2, B_DEST2)):
                        dma("pool", None, None, reads=[B_x, B_D], writes=[B_XS],
                            fn=lambda e: e.indirect_dma_start(out=XS, out_offset=bass.IndirectOffsetOnAxis(ap=DST[:, t:t + 1], axis=0),
                                                              in_=x_[:], in_offset=None, bounds_check=REG_PROWS, oob_is_err=False))
                if "DBGI" in dbg and l == 0:
                    dma("sp", DBGI[:, 0:NT], DEST1[:], reads=[B_DEST1])
                    dma("sp", DBGI[:, NT:2 * NT], DEST2[:], reads=[B_DEST2])
                    dma("sp", DBGI[:, 2 * NT:2 * NT + 2 * NBLK], WIDX[:].rearrange("p a b -> p (a b)"), reads=[B_WIDX])
                    dma("sp", DBGF[:, 0:NT], W1[:], reads=[B_W1])
                    dma("sp", DBGF[:, NT:2 * NT], W2[:], reads=[B_W2])
                    dma("sp", DBGF[:, 2 * NT:2 * NT + 64], pse[:], reads=[B_pse])
                S_.barrier()

        def pass_D(l):
            with ExitStack() as es1:
                wg = [sbt(es1, [128, 4096], BF16, "wg") for _ in range(3)]
                wu = [sbt(es1, [128, 4096], BF16, "wu") for _ in range(3)]
                wd = [sbt(es1, [128, 4096], BF16, "wd") for _ in range(3)]
                xs = [sbt(es1, [128, 2, D], BF16, "xs") for _ in range(3)]
                xT = [sbt(es1, [128, 8, 256], BF16, "xT") for _ in range(2)]
                sgt, B_sgt = sbt(es1, [128, 1024], F32, "sgt")
                hTt, B_hTt = sbt(es1, [128, 4, 256], BF16, "hTt")
                yt = [sbt(es1, [128, D], F32, "yt") for _ in range(2)]

                def loadx(b):
                    dma("sp", xs[b % 3][0][:], XS[b * 256:(b + 1) * 256, :].rearrange("(s p) d -> p s d", p=128), reads=[B_XS], writes=[xs[b % 3][1]])

                def loadw(b):
                    for (wt, src) in ((wg, I["w_gate"]), (wu, I["w_up"]), (wd, I["w_down"])):
                        w_, B_w = wt[b % 3]
                        for c in range(2):
                            dma("pool", None, None, reads=[B_WIDX], writes=[B_w],
                                fn=lambda e: e.indirect_dma_start(out=w_[:, c * 2048:(c + 1) * 2048], out_offset=None, in_=src,
                                                                  in_offset=bass.IndirectOffsetOnAxis(ap=WIDX[:, b, c:c + 1], axis=0),
                                                                  bounds_check=REG_WROWS, oob_is_err=False))
                def xpose(b):
                    xs_, B_xs = xs[b % 3]; xT_, B_xT = xT[b % 2]
                    for sub in range(2):
                        pt, B_pt = next_pt()
                        xv = xs_[:, sub, :].rearrange("p (m j) -> p j m", j=8)
                        for j in range(8):
                            op("pe", lambda e: e.transpose(pt[:, j, :], xv[:, j, :], idb[:]), reads=[B_xs, B_idb], writes=[B_pt], sig=(j == 7))
                        op("dve", lambda e: e.tensor_copy(xT_[:, :, sub * 128:(sub + 1) * 128], pt[:]), reads=[B_pt], writes=[B_xT])
                loadx(0); loadw(0)
                if NBLK > 1:
                    loadx(1); loadw(1)
                xpose(0)
                for b in range(NBLK):
                    if b + 2 < NBLK:
                        loadx(b + 2)
                        loadw(b + 2)
                    xT_, B_xT = xT[b % 2]
                    wg_, B_wg = wg[b % 3]; wu_, B_wu = wu[b % 3]; wd_, B_wd = wd[b % 3]
                    pG, B_pG = next_pf(); pU, B_pU = next_pf()
                    for (pX, B_pX, w_, B_w) in ((pG, B_pG, wg_, B_wg), (pU, B_pU, wu_, B_wu)):
                        w4 = w_[:].rearrange("p (j m c) -> p j m c", j=8, c=4)
                        for cc in range(4):
                            for j in range(8):
                                op("pe", lambda e: e.matmul(pX[:, cc * 256:(cc + 1) * 256], w4[:, j, :, cc], xT_[:, j, :], start=(j == 0), stop=(j == 7)),
                                   reads=[B_w, B_xT], writes=[B_pX], sig=(j == 7 and cc == 3))
                    op("act", lambda e: e.activation(sgt[:], pG[:], AF.Sigmoid), reads=[B_pG], writes=[B_sgt])
                    op("dve", lambda e: e.tensor_tensor(sgt[:], pG[:], sgt[:], ALU.mult), reads=[B_pG, B_sgt], writes=[B_sgt])
                    op("dve", lambda e: e.tensor_tensor(hTt[:].rearrange("p a b -> p (a b)"), pU[:], sgt[:], ALU.mult), reads=[B_sgt, B_pU], writes=[B_hTt])
                    if b + 1 < NBLK:
                        xpose(b + 1)
                    for sub in range(2):
                        pY, B_pY = next_pf()
                        for g_ in range(2):
                            for cc in range(4):
                                op("pe", lambda e: e.matmul(pY[:, g_ * 512:(g_ + 1) * 512], hTt[:, cc, sub * 128:(sub + 1) * 128],
                                                            wd_[:, cc * 1024 + g_ * 512:cc * 1024 + (g_ + 1) * 512], start=(cc == 0), stop=(cc == 3)),
                                   reads=[B_hTt, B_wd], writes=[B_pY], sig=(cc == 3 and g_ == 1))
                        y_, B_y = yt[sub]
                        op("act", lambda e: e.copy(y_[:], pY[:]), reads=[B_pY], writes=[B_y])
                        r0 = b * 256 + sub * 128
                        dma("sp", YB[r0:r0 + 128, :], y_[:], reads=[B_y], writes=[B_YB])
                S_.barrier()

        def pass_E(l, last):
            with ExitStack() as es1:
                g3, B_g3 = load_bcast(es1, I["lnall"][2 + 6 * l + 4], D, "g3")
                b3, B_b3 = load_bcast(es1, I["lnall"][2 + 6 * l + 5], D, "b3")
                lnst = make_ln(es1)
                ht = [sbt(es1, [128, D], F32, "h2t") for _ in range(2)]
                y1 = [sbt(es1, [128, D], F32, "y1") for _ in range(2)]
                y2 = [sbt(es1, [128, D], F32, "y2") for _ in range(2)]
                acc, B_acc = sbt(es1, [128, D], F32, "acc")
                ot = [sbt(es1, [128, D], F32, "ot") for _ in range(2)]

                def loads(t):
                    dma("sp", ht[t % 2][0][:], H2[t * 128:(t + 1) * 128, :], reads=[B_H2[t]], writes=[ht[t % 2][1]])
                    for (yy, DST, B_D) in ((y1, DEST1, B_DEST1), (y2, DEST2, B_DEST2)):
                        y_, B_y = yy[t % 2]
                        dma("pool", None, None, reads=[B_YB, B_D], writes=[B_y],
                            fn=lambda e: e.indirect_dma_start(out=y_[:], out_offset=None, in_=YB,
                                                              in_offset=bass.IndirectOffsetOnAxis(ap=DST[:, t:t + 1], axis=0),
                                                              bounds_check=REG_PROWS, oob_is_err=False))
                loads(0)
                for t in range(NT):
                    if t + 1 < NT:
                        loads(t + 1)
                    h_, B_h = ht[t % 2]; y1_, B_y1 = y1[t % 2]; y2_, B_y2 = y2[t % 2]
                    op("act", lambda e: e.mul(acc[:], h_[:], ALPHA), reads=[B_h], writes=[B_acc])
                    op("dve", lambda e: e.scalar_tensor_tensor(acc[:], y1_[:], W1[:, t:t + 1], acc[:], ALU.mult, ALU.add), reads=[B_y1, B_W1, B_acc], writes=[B_acc])
                    op("dve", lambda e: e.scalar_tensor_tensor(acc[:], y2_[:], W2[:, t:t + 1], acc[:], ALU.mult, ALU.add), reads=[B_y2, B_W2, B_acc], writes=[B_acc])
                    o_, B_o = ot[t % 2]
                    layernorm(lnst, acc, B_acc, o_, B_o, g3[:], B_g3, b3[:], B_b3)
                    if last:
                        dma("sp", OUT[t * 128:(t + 1) * 128, :], o_[:], reads=[B_o])
                    else:
                        dma("sp", H[t * 128:(t + 1) * 128, :], o_[:], reads=[B_o], writes=[B_H[t]])
                S_.barrier()

        pass_P0()
        if stop != "P0":
            for l in range(L):
                pass_R(l)
                if stop == "R":
                    break
                pass_B1(l)
                if stop == "B1":
                    break
                pass_B2(l)
                if stop == "B2":
                    break
                pass_C0(l)
                if stop == "C0":
                    break
                pass_D(l)
                if stop == "D":
                    break
                pass_E(l, l == L - 1 or stop == "L0")
                if stop == "L0":
                    break
        S_.finish()
    return nc, consts_np


def prep_core_inputs(inp, seqs, consts_np):
    L = inp["w_in"].shape[0]
    S = inp["x"].shape[1]
    f = lambda a: np.ascontiguousarray(a)
    m = {}
    m["x"] = f(inp["x"][seqs].reshape(-1, D))
    m["mem"] = f(inp["mem"][seqs].reshape(-1, D))
    pos = inp["positions"][seqs].reshape(-1).astype(np.int32)
    m["pos"] = f(pos.reshape(-1, 128).T)
    m["consts"] = consts_np
    rows = [inp["ln_in_g"], inp["ln_in_b"]]
    for l in range(L):
        rows += [inp["ln1_g"][l], inp["ln1_b"][l], inp["ln2_g"][l], inp["ln2_b"][l], inp["ln3_g"][l], inp["ln3_b"][l]]
    m["lnall"] = f(np.stack(rows).astype(np.float32))
    m["w_in"] = inp["w_in"]
    idx = np.clip(np.arange(RB_EXT), 0, 512)
    m["rb_ext"] = f(inp["rel_bias"][:, :, idx])
    for k in ("w_proj_ret", "w_proj_att", "w_out", "w_q_mem", "w_kv_mem", "w_o_mem"):
        m[k] = inp[k]
    m["wr"] = f(np.concatenate([inp["w_group"], inp["w_route"]], axis=2))
    m["br"] = f(np.concatenate([inp["b_group"], inp["b_route"].reshape(L, 32)], axis=1))
    m["w_gate"] = inp["w_gate"].reshape(L * NE * 256, 2048)
    m["w_up"] = inp["w_up"].reshape(L * NE * 256, 2048)
    m["w_down"] = inp["w_down"].reshape(L * NE * 256, 2048)
    return m


def kernel(**inputs):
    inp = {k: np.asarray(v) for k, v in inputs.items()}
    nc, consts_np = build(2, 4096, L=2)
    in_maps = [prep_core_inputs(inp, [2 * c, 2 * c + 1], consts_np) for c in range(8)]
    res = run_bass_kernel_spmd(nc, in_maps, core_ids=list(range(8)))
    out = np.stack([np.asarray(res.results[c]["out"]) for c in range(8)])
    return np.ascontiguousarray(out.reshape(16, 4096, D).astype(np.float32))
```

```python
import numpy as np
import ml_dtypes
import concourse.bass as bass
import concourse.mybir as mybir
from concourse.bass_utils import run_bass_kernel_spmd
from contextlib import ExitStack

F32 = mybir.dt.float32; BF16 = mybir.dt.bfloat16; I32 = mybir.dt.int32; U32 = mybir.dt.uint32
AF = mybir.ActivationFunctionType; ALU = mybir.AluOpType; AX = mybir.AxisListType

NDS = 16
SUBSTOP = 99
SAME_ENG_SYNC = True


class Buf:
    __slots__ = ("name", "w", "r")

    def __init__(self, name):
        self.name = name; self.w = None; self.r = {}


class Sched:
    def __init__(self, nc, es):
        self.nc = nc
        self.e = dict(pe=nc.tensor, act=nc.scalar, dve=nc.vector, pool=nc.gpsimd, sp=nc.sync)
        self.sem = {k: es.enter_context(nc.semaphore("s_" + k)) for k in self.e}
        self.cnt = {k: 0 for k in self.e}
        self.known = {k: {} for k in self.e}
        self.dq = {}
        for q in ("sp", "pool", "act"):
            self.dq[q] = dict(sems=[es.enter_context(nc.semaphore("d_%s%d" % (q, i))) for i in range(NDS)], n=0)
        self.nwait = 0; self.ninst = 0

    def _sem(self, key):
        if isinstance(key, tuple):
            return self.dq[key[1]]["sems"][key[2]]
        return self.sem[key]

    def _wait(self, e, key, val):
        if self.known[e].get(key, 0) >= val:
            return
        self.e[e].wait_ge(self._sem(key), val)
        self.known[e][key] = val
        self.nwait += 1

    def _deps(self, e, reads, writes):
        need = {}
        for b in reads:
            if b.w is not None and need.get(b.w[0], 0) < b.w[1]:
                need[b.w[0]] = b.w[1]
        for b in writes:
            if b.w is not None and need.get(b.w[0], 0) < b.w[1]:
                need[b.w[0]] = b.w[1]
            for k, v in b.r.items():
                if need.get(k, 0) < v:
                    need[k] = v
        for k, v in need.items():
            if k == e and (e == "pe" or not SAME_ENG_SYNC):
                continue
            self._wait(e, k, v)

    def op(self, e, fn, reads=(), writes=(), sig=True):
        self._deps(e, reads, writes)
        inst = fn(self.e[e])
        seq = self.cnt[e] + 1
        if sig:
            inst.then_inc(self.sem[e], 1)
            self.cnt[e] = seq
        self.ninst += 1
        for b in reads:
            b.r[e] = seq
        for b in writes:
            b.w = (e, seq); b.r = {}
        return inst

    def dma(self, q, out_ap, in_ap, reads=(), writes=(), fn=None):
        d = self.dq[q]; i = d["n"] % NDS; rnd = d["n"] // NDS; d["n"] += 1
        key = ("d", q, i)
        if rnd > 0:
            self._wait(q, key, 16 * rnd)
        self._deps(q, reads, writes)
        if fn is None:
            inst = self.e[q].dma_start(out=out_ap, in_=in_ap)
        else:
            inst = fn(self.e[q])
        inst.then_inc(d["sems"][i], 16)
        val = 16 * (rnd + 1)
        self.ninst += 1
        for b in reads:
            b.r[key] = val
        for b in writes:
            b.w = (key, val); b.r = {}
        return inst

    def barrier(self):
        for q, d in self.dq.items():
            for i in range(NDS):
                uses = (d["n"] - i + NDS - 1) // NDS
                if uses > 0:
                    self._wait("sp", ("d", q, i), 16 * uses)
        for k in ("pe", "act", "dve", "pool"):
            if self.cnt[k] > 0:
                self._wait("sp", k, self.cnt[k])
        inst = self.e["sp"].nop()
        self.cnt["sp"] += 1
        inst.then_inc(self.sem["sp"], 1)
        for k in ("pe", "act", "dve", "pool"):
            self._wait(k, "sp", self.cnt["sp"])
            for k2 in ("pe", "act", "dve", "pool"):
                self.known[k][k2] = max(self.known[k].get(k2, 0), self.cnt[k2])
            for q, d in self.dq.items():
                for i in range(NDS):
                    uses = (d["n"] - i + NDS - 1) // NDS
                    if uses > 0:
                        self.known[k][("d", q, i)] = 16 * uses

    def finish(self):
        for q, d in self.dq.items():
            for i in range(min(NDS, d["n"])):
                last = (d["n"] - 1 - i) // NDS + 1 if d["n"] - 1 - i >= 0 else 0
                uses = (d["n"] - i + NDS - 1) // NDS
                if uses > 0:
                    self._wait("sp", ("d", q, i), 16 * uses)
        for k in ("pe", "act", "dve", "pool"):
            if self.cnt[k] > 0:
                self._wait("sp", k, self.cnt[k])


D = 1024; NE = 32; DE = 512; LN_EPS = 1e-5
ALPHA = float((2.0 * 2) ** 0.25)
NMEM = 256
RB_EXT = 1024


def host_consts(nblk):
    tabs = {}
    p = np.arange(128)
    g = 1.0 - np.exp2(-5.0 - np.arange(8, dtype=np.float64))
    lg = np.log(g)
    i = np.arange(128)[None, :]; j = np.arange(128)[:, None]
    dm = np.zeros((128, 8, 128))
    for h in range(8):
        m = np.exp(lg[h] * np.abs(i - j)) * ((j // 64) <= (i // 64))
        dm[:, h, :] = 0.125 * m
    tabs["DM"] = dm.reshape(128, 1024)
    qd = np.zeros((128, 4, 128))
    cd = np.zeros((128, 4))
    for c in range(4):
        for half in range(2):
            h = 2 * c + half
            qd[half * 64:(half + 1) * 64, c, :] = np.exp(lg[h] * (np.arange(128) + 1.0))[None, :]
            cd[half * 64:(half + 1) * 64, c] = np.exp(lg[h] * 128.0)
    tabs["QD"] = qd.reshape(128, 512)
    tabs["CD"] = cd
    kd = np.zeros((128, 8))
    for h in range(8):
        kd[:, h] = 0.125 * np.exp(lg[h] * (127.0 - np.arange(128)))
    tabs["KD"] = kd
    inv_freq = 1.0 / (10000.0 ** np.linspace(0.0, 1.0, 32, dtype=np.float32)).astype(np.float32)
    fq = (inv_freq.astype(np.float64) / (2 * np.pi))
    tabs["FQ"] = np.tile(np.concatenate([fq, fq])[None, :], (128, 1))
    tabs["IDENT"] = np.eye(128)
    tabs["ANTI"] = np.eye(128)[::-1].copy()
    tabs["USTRICT"] = (p[:, None] < p[None, :]).astype(np.float64)
    tabs["ONES"] = np.ones((128, 128))
    u32 = np.zeros((128, 32)); u32[:32, :] = (np.arange(32)[:, None] < np.arange(32)[None, :])
    tabs["U32"] = u32
    tabs["PIDX"] = p[:, None].astype(np.float64)
    ui32 = np.zeros((128, 32)); ui32[:32, :] = (np.arange(32)[:, None] <= np.arange(32)[None, :])
    tabs["UI32"] = ui32
    tabs["BLK"] = np.tile((256.0 * np.arange(nblk))[None, :], (128, 1))
    off = {}; cols = []; o = 0
    for k, v in tabs.items():
        v = np.asarray(v, dtype=np.float32)
        off[k] = (o, v.shape[1]); cols.append(v); o += v.shape[1]
    return np.ascontiguousarray(np.concatenate(cols, axis=1)), off


class Ctx:
    pass


def build(NSEQ, S, L=2, debug=None, stop=None):
    NTOK = NSEQ * S; NT = NTOK // 128; TS = S // 128
    NBLK = -(-(NTOK * 2 + NE * 255) // 256)
    PROWS = NBLK * 256
    consts_np, coff = host_consts(NBLK)
    NCONST = consts_np.shape[1]

    nc = bass.Bass("TRN2", target_bir_lowering=False)
    dbg = debug or ()

    def dram(name, shape, dt, kind="Internal"):
        if kind == "Internal" and name in dbg:
            kind = "ExternalOutput"
        return nc.dram_tensor(name, shape, dt, kind=kind).ap()

    I = {}
    I["x"] = dram("x", [NTOK, D], F32, "ExternalInput")
    I["mem"] = dram("mem", [NSEQ * NMEM, D], F32, "ExternalInput")
    I["pos"] = dram("pos", [128, NT], I32, "ExternalInput")
    I["consts"] = dram("consts", [128, NCONST], F32, "ExternalInput")
    I["lnall"] = dram("lnall", [2 + 6 * L, D], F32, "ExternalInput")
    I["w_in"] = dram("w_in", [L, D, 6656], F32, "ExternalInput")
    I["rb_ext"] = dram("rb_ext", [L, 8, RB_EXT], F32, "ExternalInput")
    I["w_proj_ret"] = dram("w_proj_ret", [L, D, D], F32, "ExternalInput")
    I["w_proj_att"] = dram("w_proj_att", [L, 512, D], F32, "ExternalInput")
    I["w_out"] = dram("w_out", [L, D, D], F32, "ExternalInput")
    I["w_q_mem"] = dram("w_q_mem", [L, D, D], F32, "ExternalInput")
    I["w_kv_mem"] = dram("w_kv_mem", [L, D, 2 * D], F32, "ExternalInput")
    I["w_o_mem"] = dram("w_o_mem", [L, D, D], F32, "ExternalInput")
    I["wr"] = dram("wr", [L, D, 36], F32, "ExternalInput")
    I["br"] = dram("br", [L, 36], F32, "ExternalInput")
    I["w_gate"] = dram("w_gate", [L * NE * 256, 2048], F32, "ExternalInput")
    I["w_up"] = dram("w_up", [L * NE * 256, 2048], F32, "ExternalInput")
    I["w_down"] = dram("w_down", [L * NE * 256, 2048], F32, "ExternalInput")
    OUT = dram("out", [NTOK, D], F32, "ExternalOutput")
    H = dram("H", [NTOK, D], F32); M1 = dram("M1", [NTOK, D], BF16)
    H1 = dram("H1", [NTOK, D], F32); H2 = dram("H2", [NTOK, D], F32)
    XS = dram("XS", [PROWS, D], BF16); YB = dram("YB", [PROWS, D], F32)
    B_H = [Buf("H%d" % t) for t in range(NT)]; B_M1 = [Buf("M1%d" % t) for t in range(NT)]
    B_H1 = [Buf("H1%d" % t) for t in range(NT)]; B_H2 = [Buf("H2%d" % t) for t in range(NT)]
    B_XS = Buf("XS"); B_YB = Buf("YB")
    DBGI = dram("DBGI", [128, 2 * NT + 2 * NBLK], I32) if "DBGI" in dbg else None
    DBGF = dram("DBGF", [128, 2 * NT + 64], F32) if "DBGI" in dbg else None

    with ExitStack() as es:
        S_ = Sched(nc, es)
        ctr = [0]

        def sbt(es_, shape, dt, name=None):
            ctr[0] += 1
            nm = "%s_%d" % (name or "t", ctr[0])
            return es_.enter_context(nc.sbuf_tensor(nm, shape, dt)), Buf(nm)

        def pst(es_, shape, dt, name=None):
            ctr[0] += 1
            nm = "%s_%d" % (name or "p", ctr[0])
            return es_.enter_context(nc.psum_tensor(nm, shape, dt)), Buf(nm)

        op = S_.op; dma = S_.dma

        cst, B_cst = sbt(es, [128, NCONST], F32, "cst")
        dma("sp", cst[:], I["consts"], writes=[B_cst])

        def ctab(name):
            o, w = coff[name]
            return cst[:, o:o + w]
        idb, B_idb = sbt(es, [128, 128], BF16, "idb")
        op("dve", lambda e: e.tensor_copy(idb[:], ctab("IDENT")), reads=[B_cst], writes=[B_idb])
        mhalf, B_mhalf = sbt(es, [128, 8], F32, "mhalf")
        op("pool", lambda e: e.memset(mhalf[:], -0.5), writes=[B_mhalf])
        posf, B_posf = sbt(es, [128, NT], F32, "posf")
        with ExitStack() as es0:
            posi, B_posi = sbt(es0, [128, NT], I32, "posi")
            dma("sp", posi[:], I["pos"], writes=[B_posi])
            op("dve", lambda e: e.tensor_copy(posf[:], posi[:]), reads=[B_posi], writes=[B_posf])
            S_.barrier()
        PT = [pst(es, [128, 8, 128], BF16, "PT") for _ in range(2)]
        PF = [pst(es, [128, 1024], F32, "PF") for _ in range(3)]
        rr = {"pt": 0, "pf": 0}

        def next_pt():
            rr["pt"] += 1
            return PT[rr["pt"] % 2]

        def next_pf():
            rr["pf"] += 1
            return PF[rr["pf"] % 3]

        MK1, B_MK1 = sbt(es, [128, NT, 32], BF16, "MK1")
        MK2, B_MK2 = sbt(es, [128, NT, 32], BF16, "MK2")
        W1, B_W1 = sbt(es, [128, NT], F32, "W1")
        W2, B_W2 = sbt(es, [128, NT], F32, "W2")
        DEST1, B_DEST1 = sbt(es, [128, NT], I32, "DEST1")
        DEST2, B_DEST2 = sbt(es, [128, NT], I32, "DEST2")
        WIDX, B_WIDX = sbt(es, [128, NBLK, 2], I32, "WIDX")
        REG_PROWS = nc.gpsimd.to_reg(PROWS - 1)
        REG_WROWS = nc.gpsimd.to_reg(L * NE * 256 - 1)
        onesb, B_onesb = sbt(es, [128, 128], BF16, "onesb")
        op("pool", lambda e: e.memset(onesb[:], 1.0), writes=[B_onesb])
        def load_w(es_, src_rows, K, ncols, name):
            w, B = sbt(es_, [128, K, ncols], BF16, name)
            for k in range(K):
                for c0 in range(0, ncols, 2048):
                    c1 = min(ncols, c0 + 2048)
                    dma("pool", w[:, k, c0:c1], src_rows[k * 128:(k + 1) * 128, c0:c1], writes=[B])
            return w, B

        def load_bcast(es_, row_ap, n, name):
            t, B = sbt(es_, [128, n], F32, name)
            dma("sp", t[:], row_ap.partition_broadcast(128), writes=[B])
            return t, B

        def transposes(src, B_src, n, dstT, B_dst, eng="dve", idt=None, B_id=None):
            pt, B_pt = next_pt()
            for k in range(n):
                op("pe", lambda e: e.transpose(pt[:, k, :], src[:, k * 128:(k + 1) * 128], idb[:]),
                   reads=[B_src, B_idb], writes=[B_pt], sig=(k == n - 1))
            if eng == "dve":
                op("dve", lambda e: e.tensor_copy(dstT[:, 0:n, :], pt[:, 0:n, :]), reads=[B_pt], writes=[B_dst])
            elif eng == "act":
                op("dve", lambda e: e.tensor_copy(dstT[:, 0:n, :], pt[:, 0:n, :]), reads=[B_pt], writes=[B_dst])
            return pt, B_pt

        def mm_tok(ps, B_ps, xT, B_xT, K, w, B_w, c0, ncols):
            ng = (ncols + 511) // 512
            for g_ in range(ng):
                a = g_ * 512; b = min(ncols, a + 512)
                for k in range(K):
                    op("pe", lambda e: e.matmul(ps[:, a:b], xT[:, k, :], w[:, k, c0 + a:c0 + b],
                                                start=(k == 0), stop=(k == K - 1)),
                       reads=[B_xT, B_w], writes=[B_ps], sig=(k == K - 1 and g_ == ng - 1))

        class LNState:
            pass

        def make_ln(es_):
            st = LNState()
            st.st, st.B_st = sbt(es_, [128, 2, 6], F32, "lnst")
            st.mv, st.B_mv = sbt(es_, [128, 2], F32, "lnmv")
            st.rs, st.B_rs = sbt(es_, [128, 1], F32, "lnrs")
            st.nm, st.B_nm = sbt(es_, [128, 1], F32, "lnnm")
            st.tmp, st.B_tmp = sbt(es_, [128, D], F32, "lntmp")
            return st

        def layernorm(st, src, B_src, dst, B_dst, g, B_g, b, B_b):
            for c in range(2):
                op("dve", lambda e: e.bn_stats(st.st[:, c, :], src[:, c * 512:(c + 1) * 512]), reads=[B_src], writes=[st.B_st])
            op("dve", lambda e: e.bn_aggr(st.mv[:], st.st[:].rearrange("p a b -> p (a b)")), reads=[st.B_st], writes=[st.B_mv])
            op("dve", lambda e: e.tensor_scalar(st.rs[:], st.mv[:, 1:2], LN_EPS, None, ALU.add), reads=[st.B_mv], writes=[st.B_rs])
            op("pool", lambda e: e.tensor_tensor(st.rs[:], st.rs[:], mhalf[:, 0:1], ALU.pow), reads=[st.B_rs, B_mhalf], writes=[st.B_rs])
            op("dve", lambda e: e.scalar_tensor_tensor(st.nm[:], st.mv[:, 0:1], -1.0, st.rs[:], ALU.mult, ALU.mult),
               reads=[st.B_mv, st.B_rs], writes=[st.B_nm])
            op("act", lambda e: e.activation(st.tmp[:], src[:], AF.Identity, bias=st.nm[:], scale=st.rs[:]),
               reads=[B_src, st.B_nm, st.B_rs], writes=[st.B_tmp])
            op("dve", lambda e: e.tensor_tensor(st.tmp[:], st.tmp[:], g, ALU.mult), reads=[st.B_tmp, B_g], writes=[st.B_tmp])
            op("dve", lambda e: e.tensor_tensor(dst[:], st.tmp[:], b, ALU.add), reads=[st.B_tmp, B_b], writes=[B_dst])


        def pass_P0():
            with ExitStack() as es1:
                lng, B_lng = load_bcast(es1, I["lnall"][0], D, "lng")
                lnb, B_lnb = load_bcast(es1, I["lnall"][1], D, "lnb")
                lnst = make_ln(es1)
                xt = [sbt(es1, [128, D], F32, "xt") for _ in range(2)]
                yt = [sbt(es1, [128, D], F32, "yt") for _ in range(2)]
                zt, B_zt = sbt(es1, [128, D], BF16, "zt")
                op("pool", lambda e: e.memset(zt[:], 0.0), writes=[B_zt])
                nz = PROWS // 128; zi = 0
                dma("sp", xt[0][0][:], I["x"][0:128, :], writes=[xt[0][1]])
                for t in range(NT):
                    while zi < nz * (t + 1) // NT:
                        dma("sp", XS[zi * 128:(zi + 1) * 128, :], zt[:], reads=[B_zt], writes=[B_XS])
                        zi += 1
                    if t + 1 < NT:
                        dma("sp", xt[(t + 1) % 2][0][:], I["x"][(t + 1) * 128:(t + 2) * 128, :], writes=[xt[(t + 1) % 2][1]])
                    x_, Bx = xt[t % 2]; y_, By = yt[t % 2]
                    layernorm(lnst, x_, Bx, y_, By, lng[:], B_lng, lnb[:], B_lnb)
                    dma("sp", H[t * 128:(t + 1) * 128, :], y_[:], reads=[By], writes=[B_H[t]])
                S_.barrier()

        def pass_R(l):
            with ExitStack() as es1:
                win = I["w_in"][l]
                wA, B_wA = load_w(es1, win[:, 0:3072], 8, 3072, "wA")
                wG, B_wG = load_w(es1, win[:, 4608:5632], 8, 1024, "wG")
                wP, B_wP = load_w(es1, I["w_proj_ret"][l], 8, 1024, "wP")
                cos2, B_cos2 = sbt(es1, [128, NT, 64], BF16, "cos2")
                sins, B_sins = sbt(es1, [128, NT, 64], BF16, "sins")
                GT = min(NT, 16)
                with ExitStack() as es2:
                    u, B_u = sbt(es2, [128, GT, 2, 64], F32, "u")
                    ki, B_ki = sbt(es2, [128, GT, 2, 64], I32, "ki")
                    kf, B_kf = sbt(es2, [128, GT, 2, 64], F32, "kf")
                    FQ = ctab("FQ")
                    uf = u[:].rearrange("p a b c -> p (a b c)"); kif = ki[:].rearrange("p a b c -> p (a b c)")
                    kff = kf[:].rearrange("p a b c -> p (a b c)")
                    for t0 in range(0, NT, GT):
                        for tt in range(GT):
                            op("dve", lambda e: e.tensor_scalar(u[:, tt, 0, :], FQ, posf[:, t0 + tt:t0 + tt + 1], None, ALU.mult),
                               reads=[B_cst, B_posf], writes=[B_u])
                        op("dve", lambda e: e.tensor_scalar(u[:, :, 1, :], u[:, :, 0, :], 0.25, None, ALU.add), reads=[B_u], writes=[B_u])
                        op("dve", lambda e: e.tensor_copy(kif, uf), reads=[B_u], writes=[B_ki])
                        op("dve", lambda e: e.tensor_copy(kff, kif), reads=[B_ki], writes=[B_kf])
                        op("dve", lambda e: e.tensor_tensor(uf, uf, kff, ALU.subtract), reads=[B_u, B_kf], writes=[B_u])
                        op("dve", lambda e: e.tensor_scalar(kff, uf, 0.5, None, ALU.is_gt), reads=[B_u], writes=[B_kf])
                        op("dve", lambda e: e.tensor_tensor(uf, uf, kff, ALU.subtract), reads=[B_u, B_kf], writes=[B_u])
                        op("dve", lambda e: e.tensor_scalar(kff, uf, -0.5, None, ALU.is_lt), reads=[B_u], writes=[B_kf])
                        op("dve", lambda e: e.tensor_tensor(uf, uf, kff, ALU.add), reads=[B_u, B_kf], writes=[B_u])
                        op("act", lambda e: e.activation(kff, uf, AF.Sin, scale=float(2 * np.pi)), reads=[B_u], writes=[B_kf])
                        op("dve", lambda e: e.tensor_copy(cos2[:, t0:t0 + GT, :], kf[:, :, 1, :]), reads=[B_kf], writes=[B_cos2])
                        op("dve", lambda e: e.tensor_scalar(sins[:, t0:t0 + GT, 0:32], kf[:, :, 0, 0:32], -1.0, None, ALU.mult), reads=[B_kf], writes=[B_sins])
                        op("dve", lambda e: e.tensor_copy(sins[:, t0:t0 + GT, 32:64], kf[:, :, 0, 32:64]), reads=[B_kf], writes=[B_sins])
                    S_.barrier()
                DM = ctab("DM").rearrange("p (h i) -> p h i", h=8)
                QD = ctab("QD").rearrange("p (c i) -> p c i", c=4)
                CDt = ctab("CD"); KD = ctab("KD")
                ht = [sbt(es1, [128, D], F32, "ht") for _ in range(2)]
                hb, B_hb = sbt(es1, [128, D], BF16, "hb")
                hT, B_hT = sbt(es1, [128, 8, 128], BF16, "hT")
                rA, B_rA = sbt(es1, [128, 16, 64], F32, "rA")
                rB, B_rB = sbt(es1, [128, 16, 64], F32, "rB")
                rots = [sbt(es1, [128, 16, 64], BF16, "rot") for _ in range(2)]
                kps = [sbt(es1, [128, 8, 64], BF16, "kp") for _ in range(2)]
                vrs = [sbt(es1, [128, D], BF16, "vr") for _ in range(2)]
                sgs = [sbt(es1, [128, D], BF16, "sg") for _ in range(2)]
                sgrs = [sbt(es1, [128, D], BF16, "sgr") for _ in range(2)]
                qkA, B_qkA = sbt(es1, [128, 8, 128], BF16, "qkA")
                kT = qkA[:, 4:8, :]; B_kT = B_qkA
                qTe, B_qTe = sbt(es1, [128, 4, 128], BF16, "qTe")
                qTo, B_qTo = sbt(es1, [128, 4, 128], BF16, "qTo")
                qpe, B_qpe = sbt(es1, [128, 4, 128], BF16, "qpe")
                qpo, B_qpo = sbt(es1, [128, 4, 128], BF16, "qpo")
                for (t_, B_) in ((qTe, B_qTe), (qTo, B_qTo), (qpe, B_qpe), (qpo, B_qpo)):
                    op("pool", lambda e: e.memset(t_[:], 0.0), writes=[B_])
                qsel = [(qTe, B_qTe), (qTo, B_qTo)]; qpsel = [(qpe, B_qpe), (qpo, B_qpo)]
                sTd, B_sTd = sbt(es1, [128, 8, 128], BF16, "sTd")
                state, B_state = sbt(es1, [128, 4, 128], F32, "state")
                stbf, B_stbf = sbt(es1, [128, 4, 128], BF16, "stbf")
                sq = rB[:].rearrange("p (a x) b -> p a (x b)", a=8); B_sq = B_rB
                dsb = rA[:].rearrange("p a b -> p (a b)"); B_dsb = B_rA
                st8, B_st8 = sbt(es1, [128, 8, 6], F32, "st8")
                mv8, B_mv8 = sbt(es1, [128, 8, 2], F32, "mv8")
                rs8, B_rs8 = sbt(es1, [128, 8], F32, "rs8")
                yr, B_yr = sbt(es1, [128, D], BF16, "yr")
                yrT, B_yrT = sbt(es1, [128, 8, 128], BF16, "yrT")
                m1 = [sbt(es1, [128, D], BF16, "m1")] * 2

                def loadh(t):
                    dma("sp", ht[t % 2][0][:], H[t * 128:(t + 1) * 128, :], reads=[B_H[t]], writes=[ht[t % 2][1]])

                def F1(t):
                    h_, B_h = ht[t % 2]
                    rot, B_rot = rots[t % 2]; kp, B_kp = kps[t % 2]; vr, B_vr = vrs[t % 2]
                    op("act", lambda e: e.copy(hb[:], h_[:]), reads=[B_h], writes=[B_hb])
                    transposes(hb, B_hb, 8, hT, B_hT, eng="dve")
                    pqk, B_pqk = next_pf()
                    mm_tok(pqk, B_pqk, hT, B_hT, 8, wA, B_wA, 0, 1024)
                    pv, B_pv = next_pf()
                    mm_tok(pv, B_pv, hT, B_hT, 8, wA, B_wA, 1024, 1024)
                    z3 = pqk[:].rearrange("p (a b) -> p a b", b=64)
                    cb = cos2[:, t, :].unsqueeze(1).broadcast_to([128, 16, 64])
                    op("dve", lambda e: e.tensor_tensor(rA[:], z3, cb, ALU.mult), reads=[B_pqk, B_cos2], writes=[B_rA])
                    nsb = sins[:, t, 0:32].unsqueeze(1).broadcast_to([128, 16, 32])
                    psb = sins[:, t, 32:64].unsqueeze(1).broadcast_to([128, 16, 32])
                    op("dve", lambda e: e.tensor_tensor(rB[:, :, 0:32], z3[:, :, 32:64], nsb, ALU.mult), reads=[B_pqk, B_sins], writes=[B_rB])
                    op("dve", lambda e: e.tensor_tensor(rB[:, :, 32:64], z3[:, :, 0:32], psb, ALU.mult), reads=[B_pqk, B_sins], writes=[B_rB])
                    op("dve", lambda e: e.tensor_tensor(rot[:], rA[:], rB[:], ALU.add), reads=[B_rA, B_rB], writes=[B_rot])
                    kdb = KD.unsqueeze(2).broadcast_to([128, 8, 64])
                    op("pool", lambda e: e.tensor_tensor(kp[:], rot[:, 8:16, :], kdb, ALU.mult), reads=[B_rot, B_cst], writes=[B_kp])
                    op("act", lambda e: e.copy(vr[:], pv[:]), reads=[B_pv], writes=[B_vr])

                def F2(t):
                    sg, B_sg = sgs[t % 2]; sgr, B_sgr = sgrs[t % 2]
                    pg, B_pg = next_pf()
                    mm_tok(pg, B_pg, hT, B_hT, 8, wA, B_wA, 2048, 1024)
                    op("act", lambda e: e.activation(sg[:], pg[:], AF.Sigmoid), reads=[B_pg], writes=[B_sg])
                    op("dve", lambda e: e.tensor_tensor(sg[:], pg[:], sg[:], ALU.mult), reads=[B_pg, B_sg], writes=[B_sg])
                    pgr, B_pgr = next_pf()
                    mm_tok(pgr, B_pgr, hT, B_hT, 8, wG, B_wG, 0, 1024)
                    op("act", lambda e: e.activation(sgr[:], pgr[:], AF.Sigmoid), reads=[B_pgr], writes=[B_sgr])

                def Bk1(t):
                    ts = t % TS
                    rot, B_rot = rots[t % 2]; kp, B_kp = kps[t % 2]; vr, B_vr = vrs[t % 2]
                    rotf = rot[:].rearrange("p a b -> p (a b)")
                    pt, B_pt = next_pt()
                    for k in range(8):
                        op("pe", lambda e: e.transpose(pt[:, k, :], rotf[:, k * 128:(k + 1) * 128], idb[:]),
                           reads=[B_rot, B_idb], writes=[B_pt], sig=(k == 7))
                    op("dve", lambda e: e.tensor_copy(qkA[:], pt[:]), reads=[B_pt], writes=[B_qkA])
                    op("act", lambda e: e.copy(qTe[0:64, :, :], qkA[0:64, 0:4, :]), reads=[B_qkA], writes=[B_qTe])
                    op("act", lambda e: e.copy(qTo[64:128, :, :], qkA[64:128, 0:4, :]), reads=[B_qkA], writes=[B_qTo])
                    op("pool", lambda e: e.tensor_tensor(qpe[0:64, :, :], qkA[0:64, 0:4, :], QD[0:64], ALU.mult), reads=[B_qkA, B_cst], writes=[B_qpe])
                    op("pool", lambda e: e.tensor_tensor(qpo[64:128, :, :], qkA[64:128, 0:4, :], QD[64:128], ALU.mult), reads=[B_qkA, B_cst], writes=[B_qpo])
                    psc, B_psc = next_pf()
                    psc3 = psc[:].rearrange("p (h i) -> p h i", h=8)
                    for h in range(8):
                        c = h // 2
                        qs_, B_qs = qsel[h % 2]
                        op("pe", lambda e: e.matmul(psc3[:, h, :], kT[:, c, :], qs_[:, c, :], start=True, stop=True),
                           reads=[B_kT, B_qs], writes=[B_psc], sig=(h == 7))
                    op("dve", lambda e: e.tensor_tensor(sTd[:], psc3, DM, ALU.mult), reads=[B_psc, B_cst], writes=[B_sTd])
                    po, B_po = next_pf()
                    po3 = po[:].rearrange("p (h i) -> p h i", h=8)
                    for h in range(8):
                        c = h // 2
                        op("pe", lambda e: e.matmul(po3[:, h, :], sTd[:, h, :], vr[:, h * 128:(h + 1) * 128], start=True, stop=(ts == 0)),
                           reads=[B_sTd, B_vr], writes=[B_po], sig=(ts == 0 and h == 7))
                        if ts > 0:
                            qp_, B_qp = qpsel[h % 2]
                            op("pe", lambda e: e.matmul(po3[:, h, :], qp_[:, c, :], stbf[:, c, :], start=False, stop=True),
                               reads=[B_qp, B_stbf], writes=[B_po], sig=(h == 7))
                    pds, B_pds = next_pf()
                    pds4 = pds[:].rearrange("p (a c e) -> p a c e", a=2, c=4)
                    kpf = kp[:].rearrange("p a b -> p (a b)")
                    for h in range(8):
                        c = h // 2; par = h % 2
                        op("pe", lambda e: e.matmul(pds4[:, par, c, :], kpf[:, c * 128:(c + 1) * 128], vr[:, h * 128:(h + 1) * 128], start=True, stop=True),
                           reads=[B_kp, B_vr], writes=[B_pds], sig=(h == 7))
                    op("act", lambda e: e.copy(dsb, pds[:]), reads=[B_pds], writes=[B_dsb])
                    dsb4 = dsb.rearrange("p (a c e) -> p a c e", a=2, c=4)
                    if ts == 0:
                        for par in range(2):
                            b0 = par * 64
                            op("dve", lambda e: e.tensor_copy(state[b0:b0 + 64, :, :], dsb4[b0:b0 + 64, par, :, :]), reads=[B_dsb], writes=[B_state])
                    else:
                        op("pool", lambda e: e.tensor_tensor(state[:], state[:], CDt.unsqueeze(2).broadcast_to([128, 4, 128]), ALU.mult),
                           reads=[B_state, B_cst], writes=[B_state])
                        for par in range(2):
                            b0 = par * 64
                            op("dve", lambda e: e.tensor_tensor(state[b0:b0 + 64, :, :], state[b0:b0 + 64, :, :], dsb4[b0:b0 + 64, par, :, :], ALU.add),
                               reads=[B_dsb, B_state], writes=[B_state])
                    op("act", lambda e: e.copy(stbf[:], state[:]), reads=[B_state], writes=[B_stbf])
                    op("act", lambda e: e.copy(sq.rearrange("p a b -> p (a b)"), po[:]), reads=[B_po], writes=[B_sq])
                    for h in range(8):
                        op("dve", lambda e: e.bn_stats(st8[:, h, :], sq[:, h, :]), reads=[B_sq], writes=[B_st8])
                    for h in range(8):
                        op("dve", lambda e: e.bn_aggr(mv8[:, h, :], st8[:, h, :]), reads=[B_st8], writes=[B_mv8])
                    op("dve", lambda e: e.tensor_scalar(rs8[:], mv8[:, :, 1], LN_EPS, None, ALU.add), reads=[B_mv8], writes=[B_rs8])
                    op("pool", lambda e: e.tensor_tensor(rs8[:], rs8[:], mhalf[:], ALU.pow), reads=[B_rs8, B_mhalf], writes=[B_rs8])
                    for h in range(8):
                        op("dve", lambda e: e.tensor_scalar(sq[:, h, :], sq[:, h, :], mv8[:, h, 0:1], rs8[:, h:h + 1], ALU.subtract, ALU.mult),
                           reads=[B_sq, B_mv8, B_rs8], writes=[B_sq])

                def Bk2(t):
                    sg, B_sg = sgs[t % 2]; sgr, B_sgr = sgrs[t % 2]
                    op("pool", lambda e: e.tensor_tensor(yr[:], sq.rearrange("p a b -> p (a b)"), sg[:], ALU.mult), reads=[B_sq, B_sg], writes=[B_yr])
                    transposes(yr, B_yr, 8, yrT, B_yrT, eng="dve")
                    pp, B_pp = next_pf()
                    mm_tok(pp, B_pp, yrT, B_yrT, 8, wP, B_wP, 0, 1024)
                    m1_, B_m1 = m1[t % 2]
                    op("dve", lambda e: e.tensor_tensor(m1_[:], pp[:], sgr[:], ALU.mult), reads=[B_pp, B_sgr], writes=[B_m1])
                    dma("sp", M1[t * 128:(t + 1) * 128, :], m1_[:], reads=[B_m1], writes=[B_M1[t]])

                loadh(0)
                if NT > 1:
                    loadh(1)
                F1(0); F2(0)
                for t in range(NT):
                    if t + 1 < NT:
                        F1(t + 1)
                    if t + 2 < NT:
                        loadh(t + 2)
                    Bk1(t)
                    if t + 1 < NT:
                        F2(t + 1)
                    Bk2(t)
                S_.barrier()

        def next_pf_ex(excl):
            while True:
                p = next_pf()
                if all(p[0] is not x[0] for x in excl):
                    return p

        def pass_B1(l):
            with ExitStack() as es1:
                win = I["w_in"][l]
                wB, B_wB = load_w(es1, win[:, 3072:4608], 8, 1536, "wB")
                wGa, B_wGa = load_w(es1, win[:, 5632:6656], 8, 1024, "wGa")
                wPa, B_wPa = load_w(es1, I["w_proj_att"][l], 4, 1024, "wPa")
                wO, B_wO = load_w(es1, I["w_out"][l], 8, 1024, "wO")
                g1, B_g1 = load_bcast(es1, I["lnall"][2 + 6 * l + 0], D, "g1")
                b1, B_b1 = load_bcast(es1, I["lnall"][2 + 6 * l + 1], D, "b1")
                lnst = make_ln(es1)
                btn, B_btn = sbt(es1, [128, 5, 8, 128], BF16, "btn")
                with ExitStack() as es2:
                    bt2, B_bt2 = sbt(es2, [128, 5, 8, 128], BF16, "bt2")
                    rbt = I["rb_ext"].tensor
                    for r in range(5):
                        src = bass.AP(tensor=rbt, offset=l * 8 * RB_EXT + 256 + (4 - r) * 128 - 127, ap=[[1, 128], [RB_EXT, 8], [1, 128]])
                        dma("pool", bt2[:, r, :, :], src, writes=[B_bt2])
                    op("pool", lambda e: e.memset(bt2[64:128, 0, :, 64:128], -30000.0), writes=[B_bt2])
                    op("pool", lambda e: e.memset(bt2[0:64, 4, :, 0:64], -30000.0), writes=[B_bt2])
                    antib, B_antib = sbt(es2, [128, 128], BF16, "antib")
                    op("dve", lambda e: e.tensor_copy(antib[:], ctab("ANTI")), reads=[B_cst], writes=[B_antib])
                    for r in range(5):
                        pf, B_pf = next_pf()
                        for hq in range(2):
                            op("pe", lambda e: e.matmul(pf[:, hq * 512:(hq + 1) * 512], antib[:], bt2[:, r, hq * 4:(hq + 1) * 4, :], start=True, stop=True),
                               reads=[B_antib, B_bt2], writes=[B_pf], sig=(hq == 1))
                        op("act", lambda e: e.copy(btn[:, r, :, :], pf[:].rearrange("p (h i) -> p h i", h=8)), reads=[B_pf], writes=[B_btn])
                    S_.barrier()
                ht = [sbt(es1, [128, D], F32, "ht") for _ in range(2)]
                m1 = [sbt(es1, [128, D], BF16, "m1") for _ in range(2)]
                hb, B_hb = sbt(es1, [128, D], BF16, "hb")
                hT, B_hT = sbt(es1, [128, 8, 128], BF16, "hT")
                qaA, B_qaA = sbt(es1, [128, 4, 128], BF16, "qaA")
                qkt, B_qkt = sbt(es1, [128, D], BF16, "qkt")
                sb5s = [sbt(es1, [128, 5, 128], F32, "sb5") for _ in range(2)]
                qasels = []
                for _ in range(2):
                    qaTe, B_qaTe = sbt(es1, [128, 4, 128], BF16, "qaTe")
                    qaTo, B_qaTo = sbt(es1, [128, 4, 128], BF16, "qaTo")
                    for (t_, B_) in ((qaTe, B_qaTe), (qaTo, B_qaTo)):
                        op("pool", lambda e: e.memset(t_[:], 0.0), writes=[B_])
                    qasels.append([(qaTe, B_qaTe), (qaTo, B_qaTo)])
                kring = [sbt(es1, [128, 4, 128], BF16, "kring") for _ in range(6)]
                vring = [sbt(es1, [128, 8, 65], BF16, "vring") for _ in range(6)]
                for v_, Bv in vring:
                    op("pool", lambda e: e.memset(v_[:, :, 64:65], 1.0), writes=[Bv])
                sgas = [sbt(es1, [128, D], BF16, "sga") for _ in range(2)]
                pTs = [sbt(es1, [128, 5, 128], BF16, "pT") for _ in range(2)]
                rden, B_rden = sbt(es1, [128, 8], F32, "rden")
                ya, B_ya = sbt(es1, [128, 8, 64], BF16, "ya")
                yaT, B_yaT = sbt(es1, [128, 4, 128], BF16, "yaT")
                t2, B_t2 = sbt(es1, [128, D], F32, "t2")
                posb = t2[:].rearrange("p (a b) -> p a b", a=8); B_posb = B_t2
                hs = t2; B_hs = B_t2
                mg, B_mg = sbt(es1, [128, D], BF16, "mg")
                mT, B_mT = sbt(es1, [128, 8, 128], BF16, "mT")
                rs_, B_rs = sbt(es1, [128, D], F32, "resid")
                h1 = [sbt(es1, [128, D], F32, "h1")] * 2

                def loads(t):
                    dma("sp", ht[t % 2][0][:], H[t * 128:(t + 1) * 128, :], reads=[B_H[t]], writes=[ht[t % 2][1]])
                    dma("sp", m1[t % 2][0][:], M1[t * 128:(t + 1) * 128, :], reads=[B_M1[t]], writes=[m1[t % 2][1]])
                def Fa(t):
                    ts = t % TS
                    h_, B_h = ht[t % 2]
                    vr_, B_vr = vring[ts % 6]
                    op("act", lambda e: e.copy(hb[:], h_[:]), reads=[B_h], writes=[B_hb])
                    transposes(hb, B_hb, 8, hT, B_hT, eng="dve")
                    pq, B_pq = next_pf()
                    mm_tok(pq, B_pq, hT, B_hT, 8, wB, B_wB, 0, 1024)
                    op("dve", lambda e: e.tensor_scalar(qkt[:, 0:512], pq[:, 0:512], 0.125, None, ALU.mult), reads=[B_pq], writes=[B_qkt])
                    op("act", lambda e: e.copy(qkt[:, 512:1024], pq[:, 512:1024]), reads=[B_pq], writes=[B_qkt])
                    pv, B_pv = next_pf()
                    mm_tok(pv, B_pv, hT, B_hT, 8, wB, B_wB, 1024, 512)
                    op("act", lambda e: e.copy(vr_[:, :, 0:64], pv[:, 0:512].rearrange("p (h d) -> p h d", h=8)), reads=[B_pv], writes=[B_vr])

                def Fb(t):
                    ts = t % TS
                    kr, B_kr = kring[ts % 6]
                    (qaTe, B_qaTe), (qaTo, B_qaTo) = qasels[t % 2]
                    sga, B_sga = sgas[t % 2]
                    ptq, B_ptq = next_pt()
                    for k in range(8):
                        op("pe", lambda e: e.transpose(ptq[:, k, :], qkt[:, k * 128:(k + 1) * 128], idb[:]),
                           reads=[B_qkt, B_idb], writes=[B_ptq], sig=(k == 7))
                    op("dve", lambda e: e.tensor_copy(qaA[:], ptq[:, 0:4, :]), reads=[B_ptq], writes=[B_qaA])
                    op("dve", lambda e: e.tensor_copy(kr[:], ptq[:, 4:8, :]), reads=[B_ptq], writes=[B_kr])
                    op("act", lambda e: e.copy(qaTe[0:64, :, :], qaA[0:64, :, :]), reads=[B_qaA], writes=[B_qaTe])
                    op("act", lambda e: e.copy(qaTo[64:128, :, :], qaA[64:128, :, :]), reads=[B_qaA], writes=[B_qaTo])
                    pg, B_pg = next_pf()
                    mm_tok(pg, B_pg, hT, B_hT, 8, wGa, B_wGa, 0, 1024)
                    op("act", lambda e: e.activation(sga[:], pg[:], AF.Sigmoid), reads=[B_pg], writes=[B_sga])

                loads(0)
                if NT > 1:
                    loads(1)
                Fa(0); Fb(0)
                for t in range(NT):
                    ts = t % TS
                    h_, B_h = ht[t % 2]; m1_, B_m1 = m1[t % 2]
                    qasel = qasels[t % 2]
                    sga, B_sga = sgas[t % 2]
                    po = next_pf()
                    po3 = po[0][:].rearrange("p (h i) -> p h i", h=8)
                    r_lo = max(0, 4 - ts)
                    def scores(h):
                        c = h // 2
                        ps, B_ps = next_pf_ex([po])
                        ps5 = ps[:, 0:640].rearrange("p (r i) -> p r i", r=5)
                        for r in range(r_lo, 5):
                            kt = ts - 4 + r
                            kk, B_kk = kring[kt % 6]
                            qa_, B_qa = qasel[h % 2]
                            op("pe", lambda e: e.matmul(ps5[:, r, :], kk[:, c, :], qa_[:, c, :], start=True, stop=True),
                               reads=[B_kk, B_qa], writes=[B_ps], sig=(r == 4))
                        return ps5, B_ps
                    pss = {0: scores(0)}
                    for h in range(8):
                        if h + 1 < 8:
                            pss[h + 1] = scores(h + 1)
                        ps5, B_ps = pss[h]
                        pT, B_pT = pTs[h % 2]
                        sb5, B_sb5 = sb5s[h % 2]
                        op("dve", lambda e: e.tensor_tensor(sb5[:, r_lo:5, :], ps5[:, r_lo:5, :], btn[:, r_lo:5, h, :], ALU.add),
                           reads=[B_ps, B_btn], writes=[B_sb5])
                        op("act", lambda e: e.activation(pT[:, r_lo:5, :], sb5[:, r_lo:5, :], AF.Exp), reads=[B_sb5], writes=[B_pT])
                        for r in range(r_lo, 5):
                            kt = ts - 4 + r
                            vv, B_vv = vring[kt % 6]
                            op("pe", lambda e: e.matmul(po3[:, h, 0:65], pT[:, r, :], vv[:, h, :], start=(r == r_lo), stop=(r == 4)),
                               reads=[B_pT, B_vv], writes=[po[1]], sig=(r == 4))
                    op("act", lambda e: e.copy(t2[:], po[0][:]), reads=[po[1]], writes=[B_posb])
                    op("dve", lambda e: e.reciprocal(rden[:], posb[:, :, 64]), reads=[B_posb], writes=[B_rden])
                    for h in range(8):
                        op("dve", lambda e: e.tensor_scalar(ya[:, h, :], posb[:, h, 0:64], rden[:, h:h + 1], None, ALU.mult),
                           reads=[B_posb, B_rden], writes=[B_ya])
                    if t + 1 < NT:
                        Fa(t + 1)
                    transposes(ya[:].rearrange("p a b -> p (a b)"), B_ya, 4, yaT, B_yaT, eng="act")
                    pp, B_pp = next_pf()
                    mm_tok(pp, B_pp, yaT, B_yaT, 4, wPa, B_wPa, 0, 1024)
                    op("dve", lambda e: e.tensor_tensor(t2[:], pp[:], sga[:], ALU.mult), reads=[B_pp, B_sga], writes=[B_t2])
                    op("dve", lambda e: e.tensor_tensor(mg[:], t2[:], m1_[:], ALU.add), reads=[B_t2, B_m1], writes=[B_mg])
                    if t + 1 < NT:
                        Fb(t + 1)
                    transposes(mg, B_mg, 8, mT, B_mT, eng="dve")
                    px, B_px = next_pf()
                    mm_tok(px, B_px, mT, B_mT, 8, wO, B_wO, 0, 1024)
                    op("act", lambda e: e.mul(hs[:], h_[:], ALPHA), reads=[B_h], writes=[B_hs])
                    op("dve", lambda e: e.tensor_tensor(rs_[:], px[:], hs[:], ALU.add), reads=[B_hs, B_px], writes=[B_rs])
                    if t + 2 < NT:
                        loads(t + 2)
                    h1_, B_h1 = h1[t % 2]
                    layernorm(lnst, rs_, B_rs, h1_, B_h1, g1[:], B_g1, b1[:], B_b1)
                    dma("sp", H1[t * 128:(t + 1) * 128, :], h1_[:], reads=[B_h1], writes=[B_H1[t]])
                S_.barrier()

        def pass_B2(l):
            with ExitStack() as es1:
                wQ, B_wQ = load_w(es1, I["w_q_mem"][l], 8, 1024, "wQ")
                wOm, B_wOm = load_w(es1, I["w_o_mem"][l], 8, 1024, "wOm")
                g2, B_g2 = load_bcast(es1, I["lnall"][2 + 6 * l + 2], D, "g2")
                b2, B_b2 = load_bcast(es1, I["lnall"][2 + 6 * l + 3], D, "b2")
                brt, B_brt = load_bcast(es1, I["br"][l], 36, "brt")
                wrt, B_wrt = sbt(es1, [128, 8, 36], F32, "wrt")
                dma("sp", wrt[:], I["wr"][l].rearrange("(k p) n -> p k n", p=128), writes=[B_wrt])
                lnst = make_ln(es1)
                kTs = [sbt(es1, [128, 8, NMEM], BF16, "kTs") for _ in range(NSEQ)]
                Vs = [sbt(es1, [128, 2, D], BF16, "Vs") for _ in range(NSEQ)]
                with ExitStack() as es2:
                    wkv, B_wkv = load_w(es2, I["w_kv_mem"][l], 8, 2048, "wkv")
                    memb, B_memb = sbt(es2, [128, 2, D], BF16, "memb")
                    memT, B_memT = sbt(es2, [128, 8, NMEM], BF16, "memT")
                    for s_ in range(NSEQ):
                        for mc in range(2):
                            dma("pool", memb[:, mc, :], I["mem"][s_ * NMEM + mc * 128:s_ * NMEM + (mc + 1) * 128, :], writes=[B_memb])
                        for mc in range(2):
                            pt, B_pt = next_pt()
                            for k in range(8):
                                op("pe", lambda e: e.transpose(pt[:, k, :], memb[:, mc, k * 128:(k + 1) * 128], idb[:]),
                                   reads=[B_memb, B_idb], writes=[B_pt], sig=(k == 7))
                            op("dve", lambda e: e.tensor_copy(memT[:, :, mc * 128:(mc + 1) * 128], pt[:]), reads=[B_pt], writes=[B_memT])
                        kT_, B_kT = kTs[s_]; V_, B_V = Vs[s_]
                        for half in range(2):
                            pf, B_pf = next_pf()
                            for c4 in range(4):
                                c = half * 4 + c4
                                for k in range(8):
                                    op("pe", lambda e: e.matmul(pf[:, c4 * 256:(c4 + 1) * 256], wkv[:, k, c * 128:(c + 1) * 128], memT[:, k, :],
                                                                start=(k == 0), stop=(k == 7)),
                                       reads=[B_wkv, B_memT], writes=[B_pf], sig=(k == 7 and c4 == 3))
                            op("dve", lambda e: e.tensor_scalar(kT_[:, half * 4:(half + 1) * 4, :], pf[:].rearrange("p (c m) -> p c m", c=4), 1.0 / 16, None, ALU.mult),
                               reads=[B_pf], writes=[B_kT])
                        for mc in range(2):
                            pf, B_pf = next_pf()
                            for g_ in range(2):
                                for k in range(8):
                                    op("pe", lambda e: e.matmul(pf[:, g_ * 512:(g_ + 1) * 512], memT[:, k, mc * 128:(mc + 1) * 128],
                                                                wkv[:, k, 1024 + g_ * 512:1024 + (g_ + 1) * 512], start=(k == 0), stop=(k == 7)),
                                       reads=[B_wkv, B_memT], writes=[B_pf], sig=(k == 7 and g_ == 1))
                            op("act", lambda e: e.copy(V_[:, mc, :], pf[:]), reads=[B_pf], writes=[B_V])
                    S_.barrier()
                ht = [sbt(es1, [128, D], F32, "h1t") for _ in range(2)]
                hb, B_hb = sbt(es1, [128, D], BF16, "hb")
                hT, B_hT = sbt(es1, [128, 8, 128], BF16, "hT")
                qmT, B_qmT = sbt(es1, [128, 8, 128], BF16, "qmT")
                qtok, B_qtok = sbt(es1, [128, D], BF16, "qtok")
                pm, B_pm = sbt(es1, [128, 8, 128], BF16, "pm")
                rden, B_rden = sbt(es1, [128, 4], F32, "rden")
                ob, B_ob = sbt(es1, [128, 4, 256], BF16, "ob")
                osb, B_osb = sbt(es1, [128, D], F32, "osb")
                hs, B_hs = sbt(es1, [128, D], F32, "hs")
                oT, B_oT = sbt(es1, [128, 8, 128], BF16, "oT")
                rs_, B_rs = sbt(es1, [128, D], F32, "resid")
                h2 = [sbt(es1, [128, D], F32, "h2") for _ in range(2)]
                h2T, B_h2T = sbt(es1, [128, 8, 128], F32, "h2T")
                sm, B_sm = sbt(es1, [128, 256], F32, "rsm")
                idf = ctab("IDENT")
                qmTs = [(qmT, B_qmT), sbt(es1, [128, 8, 128], BF16, "qmT2")]

                def loadh1(t):
                    dma("sp", ht[t % 2][0][:], H1[t * 128:(t + 1) * 128, :], reads=[B_H1[t]], writes=[ht[t % 2][1]])

                def Fa(t):
                    h_, B_h = ht[t % 2]
                    op("act", lambda e: e.copy(hb[:], h_[:]), reads=[B_h], writes=[B_hb])
                    transposes(hb, B_hb, 8, hT, B_hT, eng="dve")
                    pq, B_pq = next_pf()
                    mm_tok(pq, B_pq, hT, B_hT, 8, wQ, B_wQ, 0, 1024)
                    op("act", lambda e: e.copy(qtok[:], pq[:]), reads=[B_pq], writes=[B_qtok])

                def Fb(t):
                    q_, B_q = qmTs[t % 2]
                    transposes(qtok, B_qtok, 8, q_, B_q, eng="dve")

                loadh1(0)
                if NT > 1:
                    loadh1(1)
                Fa(0); Fb(0)
                for t in range(NT):
                    s_ = t // TS
                    kT_, B_kT = kTs[s_]; V_, B_V = Vs[s_]
                    h_, B_h = ht[t % 2]
                    qmT, B_qmT = qmTs[t % 2]
                    ps, B_ps = next_pf()
                    ps3 = ps[:].rearrange("p (a i) -> p a i", a=8)
                    for hh in range(4):
                        for mc in range(2):
                            for c2 in range(2):
                                c = hh * 2 + c2
                                op("pe", lambda e: e.matmul(ps3[:, hh * 2 + mc, :], kT_[:, c, mc * 128:(mc + 1) * 128], qmT[:, c, :], start=(c2 == 0), stop=(c2 == 1)),
                                   reads=[B_kT, B_qmT], writes=[B_ps], sig=(hh == 3 and mc == 1 and c2 == 1))
                    op("act", lambda e: e.activation(pm[:], ps3, AF.Exp), reads=[B_ps], writes=[B_pm])
                    po, B_po = next_pf()
                    pd, B_pd = next_pf()
                    for hh in range(4):
                        for mc in range(2):
                            op("pe", lambda e: e.matmul(po[:, hh * 256:(hh + 1) * 256], pm[:, hh * 2 + mc, :], V_[:, mc, hh * 256:(hh + 1) * 256], start=(mc == 0), stop=(mc == 1)),
                               reads=[B_pm, B_V], writes=[B_po], sig=(hh == 3 and mc == 1))
                    for hh in range(4):
                        for mc in range(2):
                            op("pe", lambda e: e.matmul(pd[:, hh:hh + 1], pm[:, hh * 2 + mc, :], onesb[:, 0:1], start=(mc == 0), stop=(mc == 1)),
                               reads=[B_pm, B_onesb], writes=[B_pd], sig=(hh == 3 and mc == 1))
                    op("act", lambda e: e.copy(osb[:], po[:]), reads=[B_po], writes=[B_osb])
                    op("act", lambda e: e.copy(rden[:], pd[:, 0:4]), reads=[B_pd], writes=[B_rden])
                    op("dve", lambda e: e.reciprocal(rden[:], rden[:]), reads=[B_rden], writes=[B_rden])
                    for hh in range(4):
                        op("dve", lambda e: e.tensor_scalar(ob[:, hh, :], osb[:, hh * 256:(hh + 1) * 256], rden[:, hh:hh + 1], None, ALU.mult),
                           reads=[B_osb, B_rden], writes=[B_ob])
                    if t + 1 < NT:
                        Fa(t + 1)
                    transposes(ob[:].rearrange("p a b -> p (a b)"), B_ob, 8, oT, B_oT, eng="act")
                    px, B_px = next_pf()
                    mm_tok(px, B_px, oT, B_oT, 8, wOm, B_wOm, 0, 1024)
                    op("act", lambda e: e.mul(hs[:], h_[:], ALPHA), reads=[B_h], writes=[B_hs])
                    op("dve", lambda e: e.tensor_tensor(rs_[:], px[:], hs[:], ALU.add), reads=[B_hs, B_px], writes=[B_rs])
                    h2_, B_h2 = h2[t % 2]
                    layernorm(lnst, rs_, B_rs, h2_, B_h2, g2[:], B_g2, b2[:], B_b2)
                    dma("sp", H2[t * 128:(t + 1) * 128, :], h2_[:], reads=[B_h2], writes=[B_H2[t]])
                    if t + 2 < NT:
                        loadh1(t + 2)
                    if t + 1 < NT:
                        Fb(t + 1)
                    pt_, B_ptf = next_pf()
                    pt3 = pt_[:].rearrange("p (c i) -> p c i", c=8)
                    for k in range(8):
                        op("pe", lambda e: e.transpose(pt3[:, k, :], h2_[:, k * 128:(k + 1) * 128], idf), reads=[B_h2, B_cst], writes=[B_ptf], sig=(k == 7))
                    op("act", lambda e: e.copy(h2T[:], pt3), reads=[B_ptf], writes=[B_h2T])
                    pl, B_pl = next_pf()
                    for k in range(8):
                        op("pe", lambda e: e.matmul(pl[:, 0:36], h2T[:, k, :], wrt[:, k, :], start=(k == 0), stop=(k == 7)),
                           reads=[B_h2T, B_wrt], writes=[B_pl], sig=(k == 7))
                    lg = sm[:, 0:36]; gmax = sm[:, 36:37]; gm = sm[:, 40:44]; ngmax = sm[:, 37:38]; gexp = sm[:, 44:48]
                    gsum = sm[:, 38:39]; gw = sm[:, 39:40]; pen = sm[:, 48:52]; em = sm[:, 64:96]; mk1 = sm[:, 96:128]
                    em2 = sm[:, 128:160]; mk2 = sm[:, 160:192]; m1v = sm[:, 52:53]; m2v = sm[:, 53:54]; dd = sm[:, 54:55]
                    e2 = sm[:, 55:56]; den = sm[:, 56:57]; w1 = sm[:, 57:58]; w2 = sm[:, 58:59]
                    R_ = [B_sm]
                    op("dve", lambda e: e.tensor_tensor(lg, pl[:, 0:36], brt[:], ALU.add), reads=[B_pl, B_brt], writes=R_)
                    op("dve", lambda e: e.tensor_reduce(gmax, lg[:, 0:4], AX.X, ALU.max), reads=R_, writes=R_)
                    op("dve", lambda e: e.tensor_scalar(gm, lg[:, 0:4], gmax, None, ALU.is_equal), reads=R_, writes=R_)
                    op("dve", lambda e: e.tensor_scalar(ngmax, gmax, -1.0, None, ALU.mult), reads=R_, writes=R_)
                    op("act", lambda e: e.activation(gexp, lg[:, 0:4], AF.Exp, bias=ngmax), reads=R_, writes=R_)
                    op("dve", lambda e: e.tensor_reduce(gsum, gexp, AX.X, ALU.add), reads=R_, writes=R_)
                    op("dve", lambda e: e.reciprocal(gw, gsum), reads=R_, writes=R_)
                    op("dve", lambda e: e.tensor_scalar(pen, gm, 1.0, 1e30, ALU.subtract, ALU.mult), reads=R_, writes=R_)
                    op("dve", lambda e: e.tensor_tensor(em.rearrange("p (g x) -> p g x", g=4), lg[:, 4:36].rearrange("p (g x) -> p g x", g=4),
                                                        pen.unsqueeze(2).broadcast_to([128, 4, 8]), ALU.add), reads=R_, writes=R_)
                    op("dve", lambda e: e.tensor_reduce(m1v, em, AX.X, ALU.max), reads=R_, writes=R_)
                    op("dve", lambda e: e.tensor_scalar(mk1, em, m1v, None, ALU.is_equal), reads=R_, writes=R_)
                    op("dve", lambda e: e.scalar_tensor_tensor(em2, mk1, -1e30, em, ALU.mult, ALU.add), reads=R_, writes=R_)
                    op("dve", lambda e: e.tensor_reduce(m2v, em2, AX.X, ALU.max), reads=R_, writes=R_)
                    op("dve", lambda e: e.tensor_scalar(mk2, em2, m2v, None, ALU.is_equal), reads=R_, writes=R_)
                    op("dve", lambda e: e.tensor_tensor(dd, m2v, m1v, ALU.subtract), reads=R_, writes=R_)
                    op("act", lambda e: e.activation(e2, dd, AF.Exp), reads=R_, writes=R_)
                    op("dve", lambda e: e.tensor_scalar(den, e2, 1.0, None, ALU.add), reads=R_, writes=R_)
                    op("dve", lambda e: e.reciprocal(w1, den), reads=R_, writes=R_)
                    op("dve", lambda e: e.tensor_tensor(w2, e2, w1, ALU.mult), reads=R_, writes=R_)
                    op("dve", lambda e: e.tensor_tensor(W1[:, t:t + 1], w1, gw, ALU.mult), reads=R_, writes=[B_W1])
                    op("dve", lambda e: e.tensor_tensor(W2[:, t:t + 1], w2, gw, ALU.mult), reads=R_, writes=[B_W2])
                    op("dve", lambda e: e.tensor_copy(MK1[:, t, :], mk1), reads=R_, writes=[B_MK1])
                    op("dve", lambda e: e.tensor_copy(MK2[:, t, :], mk2), reads=R_, writes=[B_MK2])
                S_.barrier()

        def pass_C0(l):
            with ExitStack() as es1:
                mk12, B_mk12 = sbt(es1, [128, NT, 32], BF16, "mk12")
                op("dve", lambda e: e.tensor_tensor(mk12[:], MK1[:], MK2[:], ALU.add), reads=[B_MK1, B_MK2], writes=[B_mk12])
                ustb, B_ustb = sbt(es1, [128, 128], BF16, "ustb")
                op("dve", lambda e: e.tensor_copy(ustb[:], ctab("USTRICT")), reads=[B_cst], writes=[B_ustb])
                pc, B_pc = next_pf()
                for t in range(NT):
                    op("pe", lambda e: e.matmul(pc[:, 0:32], onesb[:], mk12[:, t, :], start=(t == 0), stop=(t == NT - 1)),
                       reads=[B_mk12, B_onesb], writes=[B_pc], sig=(t == NT - 1))
                cs, B_cs = sbt(es1, [128, 32], F32, "cs")
                ci, B_ci = sbt(es1, [128, 3, 32], I32, "ci")
                op("act", lambda e: e.copy(cs[:], pc[:, 0:32]), reads=[B_pc], writes=[B_cs])
                op("dve", lambda e: e.tensor_scalar(cs[:], cs[:], 255.0, None, ALU.add), reads=[B_cs], writes=[B_cs])
                op("dve", lambda e: e.tensor_copy(ci[:, 0, :], cs[:]), reads=[B_cs], writes=[B_ci])
                op("dve", lambda e: e.tensor_scalar(ci[:, 1, :], ci[:, 0, :], 8, None, ALU.arith_shift_right), reads=[B_ci], writes=[B_ci])
                op("dve", lambda e: e.tensor_scalar(ci[:, 2, :], ci[:, 1, :], 8, None, ALU.logical_shift_left), reads=[B_ci], writes=[B_ci])
                pse, B_pse = sbt(es1, [128, 64], F32, "pse")
                cum = [sbt(es1, [128, 32], F32, "cum") for _ in range(2)]
                op("dve", lambda e: e.tensor_copy(cs[:], ci[:, 2, :]), reads=[B_ci], writes=[B_cs])
                op("dve", lambda e: e.tensor_copy(cum[0][0][:], cs[:]), reads=[B_cs], writes=[cum[0][1]])
                cur = 0
                for sh in (1, 2, 4, 8, 16):
                    a_, B_a = cum[cur]; b_, B_b = cum[1 - cur]
                    op("dve", lambda e: e.tensor_copy(b_[:, 0:sh], a_[:, 0:sh]), reads=[B_a], writes=[B_b])
                    op("dve", lambda e: e.tensor_tensor(b_[:, sh:32], a_[:, sh:32], a_[:, 0:32 - sh], ALU.add), reads=[B_a], writes=[B_b])
                    cur = 1 - cur
                inc_, B_inc = cum[cur]
                op("dve", lambda e: e.tensor_copy(pse[:, 32:64], inc_[:]), reads=[B_inc], writes=[B_pse])
                op("dve", lambda e: e.tensor_tensor(pse[:, 0:32], inc_[:], cs[:], ALU.subtract), reads=[B_inc, B_cs], writes=[B_pse])
                cmp_, B_cmp = sbt(es1, [128, NBLK, 32], F32, "cmp")
                bet, B_bet = sbt(es1, [128, NBLK], F32, "bet")
                BLK = ctab("BLK")
                op("dve", lambda e: e.tensor_tensor(cmp_[:], pse[:, 32:64].unsqueeze(1).broadcast_to([128, NBLK, 32]),
                                                    BLK.unsqueeze(2).broadcast_to([128, NBLK, 32]), ALU.is_le), reads=[B_pse, B_cst], writes=[B_cmp])
                op("dve", lambda e: e.tensor_reduce(bet[:], cmp_[:], AX.X, ALU.add), reads=[B_cmp], writes=[B_bet])
                flg, B_flg = sbt(es1, [128, NBLK], F32, "flg")
                op("dve", lambda e: e.tensor_scalar(flg[:], bet[:], 31.5, 1.0e6, ALU.is_gt, ALU.mult), reads=[B_bet], writes=[B_flg])
                op("dve", lambda e: e.tensor_scalar(bet[:], bet[:], 31.0, 256.0, ALU.min, ALU.mult), reads=[B_bet], writes=[B_bet])
                op("dve", lambda e: e.tensor_tensor(bet[:], bet[:], flg[:], ALU.add), reads=[B_bet, B_flg], writes=[B_bet])
                pb2, B_pb2 = sbt(es1, [128, 2], F32, "pb2")
                op("dve", lambda e: e.tensor_scalar(pb2[:, 0:1], ctab("PIDX"), 2.0, float(l * NE * 256), ALU.mult, ALU.add), reads=[B_cst], writes=[B_pb2])
                op("dve", lambda e: e.tensor_scalar(pb2[:, 1:2], pb2[:, 0:1], 1.0, None, ALU.add), reads=[B_pb2], writes=[B_pb2])
                wf, B_wf = sbt(es1, [128, NBLK, 2], F32, "wf")
                for c in range(2):
                    op("dve", lambda e: e.tensor_scalar(wf[:, :, c], bet[:], pb2[:, c:c + 1], None, ALU.add), reads=[B_bet, B_pb2], writes=[B_wf])
                op("dve", lambda e: e.tensor_copy(WIDX[:], wf[:]), reads=[B_wf], writes=[B_WIDX])
                mkc, B_mkc = sbt(es1, [128, 32], BF16, "mkc")
                op("pool", lambda e: e.memset(mkc[:], 0.0), writes=[B_mkc])
                dst, B_dst = sbt(es1, [128, 32], F32, "dst")
                tmp, B_tmp = sbt(es1, [128, 32], F32, "dtmp")
                dd, B_dd = sbt(es1, [128, 2], F32, "dd")
                ht = [sbt(es1, [128, D], F32, "h2t") for _ in range(2)]
                xb = [sbt(es1, [128, D], BF16, "xb") for _ in range(2)]
                dma("sp", ht[0][0][:], H2[0:128, :], reads=[B_H2[0]], writes=[ht[0][1]])
                for t in range(NT):
                    if t + 1 < NT:
                        dma("sp", ht[(t + 1) % 2][0][:], H2[(t + 1) * 128:(t + 2) * 128, :], reads=[B_H2[t + 1]], writes=[ht[(t + 1) % 2][1]])
                    pr, B_pr = next_pf()
                    op("pe", lambda e: e.matmul(pr[:, 0:32], ustb[:], mk12[:, t, :], start=True, stop=(t == 0)), reads=[B_ustb, B_mk12], writes=[B_pr], sig=(t == 0))
                    if t > 0:
                        op("pe", lambda e: e.matmul(pr[:, 0:32], onesb[:], mkc[:], start=False, stop=True), reads=[B_onesb, B_mkc], writes=[B_pr])
                    op("dve", lambda e: e.tensor_tensor(dst[:], pr[:, 0:32], pse[:, 0:32], ALU.add), reads=[B_pr, B_pse], writes=[B_dst])
                    op("dve", lambda e: e.tensor_tensor(tmp[:], dst[:], MK1[:, t, :], ALU.mult), reads=[B_dst, B_MK1], writes=[B_tmp])
                    op("dve", lambda e: e.tensor_reduce(dd[:, 0:1], tmp[:], AX.X, ALU.add), reads=[B_tmp], writes=[B_dd])
                    op("dve", lambda e: e.tensor_tensor(tmp[:], dst[:], MK2[:, t, :], ALU.mult), reads=[B_dst, B_MK2], writes=[B_tmp])
                    op("dve", lambda e: e.tensor_reduce(dd[:, 1:2], tmp[:], AX.X, ALU.add), reads=[B_tmp], writes=[B_dd])
                    op("dve", lambda e: e.tensor_copy(DEST1[:, t:t + 1], dd[:, 0:1]), reads=[B_dd], writes=[B_DEST1])
                    op("dve", lambda e: e.tensor_copy(DEST2[:, t:t + 1], dd[:, 1:2]), reads=[B_dd], writes=[B_DEST2])
                    op("dve", lambda e: e.tensor_tensor(mkc[:], mkc[:], mk12[:, t, :], ALU.add), reads=[B_mkc, B_mk12], writes=[B_mkc])
                    h_, B_h = ht[t % 2]; x_, B_x = xb[t % 2]
                    op("act", lambda e: e.copy(x_[:], h_[:]), reads=[B_h], writes=[B_x])
                    for DST, B_D in ((DEST1, B_DEST1), (DEST2, B_DEST2)):
                        dma("pool", None, None, reads=[B_x, B_D], writes=[B_XS],
                            fn=lambda e: e.indirect_dma_start(out=XS, out_offset=bass.IndirectOffsetOnAxis(ap=DST[:, t:t + 1], axis=0),
                                                              in_=x_[:], in_offset=None, bounds_check=REG_PROWS, oob_is_err=False))
                if "DBGI" in dbg and l == 0:
                    dma("sp", DBGI[:, 0:NT], DEST1[:], reads=[B_DEST1])
                    dma("sp", DBGI[:, NT:2 * NT], DEST2[:], reads=[B_DEST2])
                    dma("sp", DBGI[:, 2 * NT:2 * NT + 2 * NBLK], WIDX[:].rearrange("p a b -> p (a b)"), reads=[B_WIDX])
                    dma("sp", DBGF[:, 0:NT], W1[:], reads=[B_W1])
                    dma("sp", DBGF[:, NT:2 * NT], W2[:], reads=[B_W2])
                    dma("sp", DBGF[:, 2 * NT:2 * NT + 64], pse[:], reads=[B_pse])
                S_.barrier()

        def pass_D(l):
            with ExitStack() as es1:
                wg = [sbt(es1, [128, 4096], BF16, "wg") for _ in range(3)]
                wu = [sbt(es1, [128, 4096], BF16, "wu") for _ in range(3)]
                wd = [sbt(es1, [128, 4096], BF16, "wd") for _ in range(3)]
                xs = [sbt(es1, [128, 2, D], BF16, "xs") for _ in range(3)]
                xT = [sbt(es1, [128, 8, 256], BF16, "xT") for _ in range(2)]
                sgt, B_sgt = sbt(es1, [128, 1024], F32, "sgt")
                hTt, B_hTt = sbt(es1, [128, 4, 256], BF16, "hTt")
                yt = [sbt(es1, [128, D], F32, "yt") for _ in range(2)]

                def loadx(b):
                    dma("sp", xs[b % 3][0][:], XS[b * 256:(b + 1) * 256, :].rearrange("(s p) d -> p s d", p=128), reads=[B_XS], writes=[xs[b % 3][1]])

                def loadw(b):
                    for (wt, src) in ((wg, I["w_gate"]), (wu, I["w_up"]), (wd, I["w_down"])):
                        w_, B_w = wt[b % 3]
                        for c in range(2):
                            dma("pool", None, None, reads=[B_WIDX], writes=[B_w],
                                fn=lambda e: e.indirect_dma_start(out=w_[:, c * 2048:(c + 1) * 2048], out_offset=None, in_=src,
                                                                  in_offset=bass.IndirectOffsetOnAxis(ap=WIDX[:, b, c:c + 1], axis=0),
                                                                  bounds_check=REG_WROWS, oob_is_err=False))
                def xpose(b):
                    xs_, B_xs = xs[b % 3]; xT_, B_xT = xT[b % 2]
                    for sub in range(2):
                        pt, B_pt = next_pt()
                        xv = xs_[:, sub, :].rearrange("p (m j) -> p j m", j=8)
                        for j in range(8):
                            op("pe", lambda e: e.transpose(pt[:, j, :], xv[:, j, :], idb[:]), reads=[B_xs, B_idb], writes=[B_pt], sig=(j == 7))
                        op("dve", lambda e: e.tensor_copy(xT_[:, :, sub * 128:(sub + 1) * 128], pt[:]), reads=[B_pt], writes=[B_xT])
                loadx(0); loadw(0)
                if NBLK > 1:
                    loadx(1); loadw(1)
                xpose(0)
                for b in range(NBLK):
                    if b + 2 < NBLK:
                        loadx(b + 2)
                        loadw(b + 2)
                    xT_, B_xT = xT[b % 2]
                    wg_, B_wg = wg[b % 3]; wu_, B_wu = wu[b % 3]; wd_, B_wd = wd[b % 3]
                    pG, B_pG = next_pf(); pU, B_pU = next_pf()
                    for (pX, B_pX, w_, B_w) in ((pG, B_pG, wg_, B_wg), (pU, B_pU, wu_, B_wu)):
                        w4 = w_[:].rearrange("p (j m c) -> p j m c", j=8, c=4)
                        for cc in range(4):
                            for j in range(8):
                                op("pe", lambda e: e.matmul(pX[:, cc * 256:(cc + 1) * 256], w4[:, j, :, cc], xT_[:, j, :], start=(j == 0), stop=(j == 7)),
                                   reads=[B_w, B_xT], writes=[B_pX], sig=(j == 7 and cc == 3))
                    op("act", lambda e: e.activation(sgt[:], pG[:], AF.Sigmoid), reads=[B_pG], writes=[B_sgt])
                    op("dve", lambda e: e.tensor_tensor(sgt[:], pG[:], sgt[:], ALU.mult), reads=[B_pG, B_sgt], writes=[B_sgt])
                    op("dve", lambda e: e.tensor_tensor(hTt[:].rearrange("p a b -> p (a b)"), pU[:], sgt[:], ALU.mult), reads=[B_sgt, B_pU], writes=[B_hTt])
                    if b + 1 < NBLK:
                        xpose(b + 1)
                    for sub in range(2):
                        pY, B_pY = next_pf()
                        for g_ in range(2):
                            for cc in range(4):
                                op("pe", lambda e: e.matmul(pY[:, g_ * 512:(g_ + 1) * 512], hTt[:, cc, sub * 128:(sub + 1) * 128],
                                                            wd_[:, cc * 1024 + g_ * 512:cc * 1024 + (g_ + 1) * 512], start=(cc == 0), stop=(cc == 3)),
                                   reads=[B_hTt, B_wd], writes=[B_pY], sig=(cc == 3 and g_ == 1))
                        y_, B_y = yt[sub]
                        op("act", lambda e: e.copy(y_[:], pY[:]), reads=[B_pY], writes=[B_y])
                        r0 = b * 256 + sub * 128
                        dma("sp", YB[r0:r0 + 128, :], y_[:], reads=[B_y], writes=[B_YB])
                S_.barrier()

        def pass_E(l, last):
            with ExitStack() as es1:
                g3, B_g3 = load_bcast(es1, I["lnall"][2 + 6 * l + 4], D, "g3")
                b3, B_b3 = load_bcast(es1, I["lnall"][2 + 6 * l + 5], D, "b3")
                lnst = make_ln(es1)
                ht = [sbt(es1, [128, D], F32, "h2t") for _ in range(2)]
                y1 = [sbt(es1, [128, D], F32, "y1") for _ in range(2)]
                y2 = [sbt(es1, [128, D], F32, "y2") for _ in range(2)]
                acc, B_acc = sbt(es1, [128, D], F32, "acc")
                ot = [sbt(es1, [128, D], F32, "ot") for _ in range(2)]

                def loads(t):
                    dma("sp", ht[t % 2][0][:], H2[t * 128:(t + 1) * 128, :], reads=[B_H2[t]], writes=[ht[t % 2][1]])
                    for (yy, DST, B_D) in ((y1, DEST1, B_DEST1), (y2, DEST2, B_DEST2)):
                        y_, B_y = yy[t % 2]
                        dma("pool", None, None, reads=[B_YB, B_D], writes=[B_y],
                            fn=lambda e: e.indirect_dma_start(out=y_[:], out_offset=None, in_=YB,
                                                              in_offset=bass.IndirectOffsetOnAxis(ap=DST[:, t:t + 1], axis=0),
                                                              bounds_check=REG_PROWS, oob_is_err=False))
                loads(0)
                for t in range(NT):
                    if t + 1 < NT:
                        loads(t + 1)
                    h_, B_h = ht[t % 2]; y1_, B_y1 = y1[t % 2]; y2_, B_y2 = y2[t % 2]
                    op("act", lambda e: e.mul(acc[:], h_[:], ALPHA), reads=[B_h], writes=[B_acc])
                    op("dve", lambda e: e.scalar_tensor_tensor(acc[:], y1_[:], W1[:, t:t + 1], acc[:], ALU.mult, ALU.add), reads=[B_y1, B_W1, B_acc], writes=[B_acc])
                    op("dve", lambda e: e.scalar_tensor_tensor(acc[:], y2_[:], W2[:, t:t + 1], acc[:], ALU.mult, ALU.add), reads=[B_y2, B_W2, B_acc], writes=[B_acc])
                    o_, B_o = ot[t % 2]
                    layernorm(lnst, acc, B_acc, o_, B_o, g3[:], B_g3, b3[:], B_b3)
                    if last:
                        dma("sp", OUT[t * 128:(t + 1) * 128, :], o_[:], reads=[B_o])
                    else:
                        dma("sp", H[t * 128:(t + 1) * 128, :], o_[:], reads=[B_o], writes=[B_H[t]])
                S_.barrier()

        pass_P0()
        if stop != "P0":
            for l in range(L):
                pass_R(l)
                if stop == "R":
                    break
                pass_B1(l)
                if stop == "B1":
                    break
                pass_B2(l)
                if stop == "B2":
                    break
                pass_C0(l)
                if stop == "C0":
                    break
                pass_D(l)
                if stop == "D":
                    break
                pass_E(l, l == L - 1 or stop == "L0")
                if stop == "L0":
                    break
        S_.finish()
    return nc, consts_np


def prep_core_inputs(inp, seqs, consts_np):
    L = inp["w_in"].shape[0]
    S = inp["x"].shape[1]
    f = lambda a: np.ascontiguousarray(a)
    m = {}
    m["x"] = f(inp["x"][seqs].reshape(-1, D))
    m["mem"] = f(inp["mem"][seqs].reshape(-1, D))
    pos = inp["positions"][seqs].reshape(-1).astype(np.int32)
    m["pos"] = f(pos.reshape(-1, 128).T)
    m["consts"] = consts_np
    rows = [inp["ln_in_g"], inp["ln_in_b"]]
    for l in range(L):
        rows += [inp["ln1_g"][l], inp["ln1_b"][l], inp["ln2_g"][l], inp["ln2_b"][l], inp["ln3_g"][l], inp["ln3_b"][l]]
    m["lnall"] = f(np.stack(rows).astype(np.float32))
    m["w_in"] = inp["w_in"]
    idx = np.clip(np.arange(RB_EXT), 0, 512)
    m["rb_ext"] = f(inp["rel_bias"][:, :, idx])
    for k in ("w_proj_ret", "w_proj_att", "w_out", "w_q_mem", "w_kv_mem", "w_o_mem"):
        m[k] = inp[k]
    m["wr"] = f(np.concatenate([inp["w_group"], inp["w_route"]], axis=2))
    m["br"] = f(np.concatenate([inp["b_group"], inp["b_route"].reshape(L, 32)], axis=1))
    m["w_gate"] = inp["w_gate"].reshape(L * NE * 256, 2048)
    m["w_up"] = inp["w_up"].reshape(L * NE * 256, 2048)
    m["w_down"] = inp["w_down"].reshape(L * NE * 256, 2048)
    return m


def kernel(**inputs):
    inp = {k: np.asarray(v) for k, v in inputs.items()}
    nc, consts_np = build(2, 4096, L=2)
    in_maps = [prep_core_inputs(inp, [2 * c, 2 * c + 1], consts_np) for c in range(8)]
    res = run_bass_kernel_spmd(nc, in_maps, core_ids=list(range(8)))
    out = np.stack([np.asarray(res.results[c]["out"]) for c in range(8)])
    return np.ascontiguousarray(out.reshape(16, 4096, D).astype(np.float32))
```
